# Optimizing a Trainium2 kernel written in Bass

```python
import math
import jax, jax.numpy as jnp
from jax import lax
import numpy as np

D_MODEL = 1024
BATCH = 2
SEQ = 16384
DEPTH = 2

N_META = 16
N_MIXERS = 2
N_A_LAYERS = (DEPTH + 1) // 2
N_B_LAYERS = DEPTH // 2

DA_HEADS = 8
DA_HEAD_DIM = D_MODEL // (2 * DA_HEADS)
DA_V_DIM = 2 * DA_HEAD_DIM
Q_BLOCK = 128
SUBLN_EPS = 1e-5

RW_HEAD_DIM = 64
RW_HEADS = D_MODEL // RW_HEAD_DIM
RW_DECAY_LORA = 64
RW_AAA_LORA = 64
RW_GATE_LORA = 160
RW_N_MIX = 6
RW_LNX_EPS = 64e-5

D_FF = 2816
N_EXPERTS = 8
TOP_K = 2
D_FF_EXPERT = 3584

LN_EPS = 1e-5
DEEPNORM_ALPHA = (2.0 * DEPTH) ** 0.25
DEEPNORM_BETA = (8.0 * DEPTH) ** -0.25

kernel_name = 'hybrid_diffattn_rwkv7_moe_encoder'


def layer_norm(x, g, b):
    xf = x.astype(jnp.float32)
    mu = jnp.mean(xf, -1, keepdims=True)
    var = jnp.mean(jnp.square(xf - mu), -1, keepdims=True)
    return ((xf - mu) * lax.rsqrt(var + LN_EPS) * g + b).astype(x.dtype)


def diff_attention(x, w_in, w_o, lq1, lk1, lq2, lk2, subln_g, layer_idx):
    B, L, D = x.shape
    H, Dh, Dv = DA_HEADS, DA_HEAD_DIM, DA_V_DIM
    Lp = -(-L // Q_BLOCK) * Q_BLOCK
    nblk = Lp // Q_BLOCK
    qkv = jnp.einsum('bld,de->ble', x, w_in)
    qkv = jnp.pad(qkv, ((0, 0), (0, Lp - L), (0, 0)))
    q, k, v = jnp.split(qkv, 3, axis=-1)
    q = q.reshape(B, Lp, H, 2, Dh)
    k = k.reshape(B, Lp, H, 2, Dh)
    vf = v.reshape(B, Lp, H, Dv).astype(jnp.float32)
    lam_init = 0.8 - 0.6 * math.exp(-0.3 * layer_idx)
    lam = (jnp.exp(jnp.sum(lq1 * lk1).astype(jnp.float32))
           - jnp.exp(jnp.sum(lq2 * lk2).astype(jnp.float32)) + lam_init)
    slopes = 2.0 ** (-(8.0 / H) * jnp.arange(1, H + 1, dtype=jnp.float32))
    kpos = jnp.arange(Lp, dtype=jnp.int32)
    kvalid = kpos < L
    scale = Dh ** -0.5
    qb = q.reshape(B, nblk, Q_BLOCK, H, 2, Dh).transpose(1, 0, 2, 3, 4, 5)
    starts = jnp.arange(nblk, dtype=jnp.int32) * Q_BLOCK

    def block(args):
        qblk, start = args
        s = jnp.einsum('bqhcd,bkhcd->bhcqk', qblk, k).astype(jnp.float32) * scale
        qpos = start + jnp.arange(Q_BLOCK, dtype=jnp.int32)
        dist = jnp.abs(qpos[:, None] - kpos[None, :]).astype(jnp.float32)
        s = s - slopes[None, :, None, None, None] * dist
        s = jnp.where(kvalid, s, -jnp.inf)
        p = jax.nn.softmax(s, axis=-1)
        a = p[:, :, 0] - lam * p[:, :, 1]
        return jnp.einsum('bhqk,bkhe->bqhe', a, vf)

    o = lax.map(block, (qb, starts))
    o = o.transpose(1, 0, 2, 3, 4).reshape(B, Lp, H, Dv)[:, :L]
    o = o * lax.rsqrt(jnp.mean(o * o, -1, keepdims=True) + SUBLN_EPS) * subln_g * (1.0 - lam_init)
    o = o.reshape(B, L, H * Dv).astype(x.dtype)
    return jnp.einsum('ble,ed->bld', o, w_o)


def _wkv7_step(S, inp):
    r, w, k, v, kk, a = inp
    sa = jnp.einsum('bhvk,bhk->bhv', S, kk)
    S = S * w[:, :, None, :] - sa[..., None] * (kk * a)[:, :, None, :] + v[..., None] * k[:, :, None, :]
    y = jnp.einsum('bhvk,bhk->bhv', S, r)
    return S, y


def rwkv7_time_mix(x, mu, w_rkv, w0, w1, w2, a0, a1, a2, g1, g2, k_k, k_a, r_k, lnx_g, lnx_b, w_o):
    B, L, D = x.shape
    H, N = RW_HEADS, RW_HEAD_DIM
    zero = jnp.zeros_like(x[:, :1])
    dx_prev = jnp.concatenate([zero, x[:, :-1]], 1) - x
    dx_next = jnp.concatenate([x[:, 1:], zero], 1) - x
    xs = x[None] + mu[0][:, None, None, :] * dx_prev[None] + mu[1][:, None, None, :] * dx_next[None]
    r, k, v = jnp.einsum('ibld,ide->ible', xs[:3], w_rkv)
    lw = jnp.einsum('nblr,nrd->nbld', jnp.tanh(jnp.einsum('bld,ndr->nblr', xs[3], w1)), w2)
    w_log = -jax.nn.softplus(-(w0[:, None, None, :] + lw).astype(jnp.float32)) - 0.5
    decay = jnp.exp(-jnp.exp(w_log))
    la = jnp.einsum('nblr,nrd->nbld', jnp.einsum('bld,ndr->nblr', xs[4], a1), a2)
    a = jax.nn.sigmoid((a0[:, None, None, :] + la).astype(jnp.float32))
    g = jnp.einsum('blr,rd->bld', jax.nn.sigmoid(jnp.einsum('bld,dr->blr', xs[5], g1)), g2)
    kf = k.astype(jnp.float32)
    kk = (kf * k_k).reshape(B, L, H, N)
    kk = kk / jnp.maximum(jnp.sqrt(jnp.sum(kk * kk, -1, keepdims=True)), 1e-12)
    kd = kf[None] * (1.0 + (a - 1.0) * k_a)

    def tm(t):
        return jnp.swapaxes(t.reshape(B, L, H, N), 0, 1).astype(jnp.float32)

    r_t, v_t, kk_t = tm(r), tm(v), jnp.swapaxes(kk, 0, 1)
    S0 = jnp.zeros((B, H, N, N), jnp.float32)
    _, y_fwd = lax.scan(_wkv7_step, S0, (r_t, tm(decay[0]), tm(kd[0]), v_t, kk_t, tm(a[0])))
    _, y_bwd = lax.scan(_wkv7_step, S0, (r_t, tm(decay[1]), tm(kd[1]), v_t, kk_t, tm(a[1])), reverse=True)
    y = jnp.swapaxes(y_fwd + y_bwd, 0, 1)
    ym = jnp.mean(y, -1, keepdims=True)
    yv = jnp.mean(jnp.square(y - ym), -1, keepdims=True)
    y = ((y - ym) * lax.rsqrt(yv + RW_LNX_EPS)).reshape(B, L, D) * lnx_g + lnx_b
    rh = r.reshape(B, L, H, N).astype(jnp.float32)
    kdh = (kd[0] + kd[1]).reshape(B, L, H, N)
    bonus = jnp.sum(rh * kdh * r_k, -1, keepdims=True) * v.reshape(B, L, H, N).astype(jnp.float32)
    out = ((y + bonus.reshape(B, L, D)) * g).astype(x.dtype)
    return jnp.einsum('bld,de->ble', out, w_o)


def swiglu(x, w_gate, w_up, w_down):
    return (jax.nn.silu(x @ w_gate) * (x @ w_up)) @ w_down


def moe_swiglu(x, w_router, b_router, w_gate, w_up, w_down):
    logits = (jnp.einsum('bld,de->ble', x, w_router) + b_router).astype(jnp.float32)
    probs = jax.nn.softmax(logits, -1)
    top_p, top_i = lax.top_k(probs, TOP_K)
    top_p = top_p / jnp.sum(top_p, -1, keepdims=True)
    gates = jnp.sum(jax.nn.one_hot(top_i, N_EXPERTS, dtype=jnp.float32) * top_p[..., None], -2)
    y = jnp.zeros(x.shape, jnp.float32)
    for e in range(N_EXPERTS):
        y = y + gates[..., e:e + 1] * swiglu(x, w_gate[e], w_up[e], w_down[e]).astype(jnp.float32)
    return y.astype(x.dtype)


def setup_inputs(seed: int = 0) -> dict:
    key = jax.random.key(seed)
    ks = iter(jax.random.split(key, 48))
    f32 = jnp.float32
    D, F, FE, E = D_MODEL, D_FF, D_FF_EXPERT, N_EXPERTS
    nA, nB = N_A_LAYERS, N_B_LAYERS

    def nrm(shape, scale):
        return jax.random.normal(next(ks), shape, f32) * scale

    inp = {}
    inp['x'] = nrm((BATCH, SEQ, D), 1.0)
    inp['meta'] = nrm((N_META, D), 1.0)
    inp['ln_g'] = 1.0 + nrm((DEPTH, 2, D), 0.02)
    inp['ln_b'] = nrm((DEPTH, 2, D), 0.02)
    inp['attn_w_in'] = nrm((nA, D, 3 * D), D ** -0.5)
    inp['attn_w_o'] = nrm((nA, D, D), D ** -0.5 * DEEPNORM_BETA)
    inp['attn_lam_q1'] = nrm((nA, DA_HEAD_DIM), 0.1)
    inp['attn_lam_k1'] = nrm((nA, DA_HEAD_DIM), 0.1)
    inp['attn_lam_q2'] = nrm((nA, DA_HEAD_DIM), 0.1)
    inp['attn_lam_k2'] = nrm((nA, DA_HEAD_DIM), 0.1)
    inp['attn_subln_g'] = 1.0 + nrm((nA, DA_V_DIM), 0.02)
    inp['ffn_w_gate'] = nrm((nA, D, F), D ** -0.5)
    inp['ffn_w_up'] = nrm((nA, D, F), D ** -0.5)
    inp['ffn_w_down'] = nrm((nA, F, D), F ** -0.5 * DEEPNORM_BETA)
    inp['rw_mu'] = jax.random.uniform(next(ks), (nB, 2, RW_N_MIX, D), f32, 0.0, 0.5)
    inp['rw_w_rkv'] = nrm((nB, 3, D, D), D ** -0.5)
    inp['rw_w0'] = -1.0 + nrm((nB, 2, D), 0.5)
    inp['rw_w1'] = nrm((nB, 2, D, RW_DECAY_LORA), D ** -0.5)
    inp['rw_w2'] = nrm((nB, 2, RW_DECAY_LORA, D), 0.5 * RW_DECAY_LORA ** -0.5)
    inp['rw_a0'] = nrm((nB, 2, D), 0.1)
    inp['rw_a1'] = nrm((nB, 2, D, RW_AAA_LORA), D ** -0.5)
    inp['rw_a2'] = nrm((nB, 2, RW_AAA_LORA, D), 0.5 * RW_AAA_LORA ** -0.5)
    inp['rw_g1'] = nrm((nB, D, RW_GATE_LORA), D ** -0.5)
    inp['rw_g2'] = nrm((nB, RW_GATE_LORA, D), RW_GATE_LORA ** -0.5)
    inp['rw_k_k'] = 0.85 + nrm((nB, D), 0.05)
    inp['rw_k_a'] = 1.0 + nrm((nB, D), 0.05)
    inp['rw_r_k'] = nrm((nB, RW_HEADS, RW_HEAD_DIM), 0.1)
    inp['rw_lnx_g'] = 1.0 + nrm((nB, D), 0.02)
    inp['rw_lnx_b'] = nrm((nB, D), 0.02)
    inp['rw_w_o'] = nrm((nB, D, D), D ** -0.5 * DEEPNORM_BETA)
    inp['moe_w_router'] = nrm((nB, D, E), D ** -0.5)
    inp['moe_b_router'] = nrm((nB, E), 0.01)
    inp['moe_w_gate'] = nrm((nB, E, D, FE), D ** -0.5)
    inp['moe_w_up'] = nrm((nB, E, D, FE), D ** -0.5)
    inp['moe_w_down'] = nrm((nB, E, FE, D), FE ** -0.5 * DEEPNORM_BETA)
    return inp


def reference(x, meta, ln_g, ln_b, attn_w_in, attn_w_o, attn_lam_q1, attn_lam_k1, attn_lam_q2,
              attn_lam_k2, attn_subln_g, ffn_w_gate, ffn_w_up, ffn_w_down, rw_mu, rw_w_rkv, rw_w0,
              rw_w1, rw_w2, rw_a0, rw_a1, rw_a2, rw_g1, rw_g2, rw_k_k, rw_k_a, rw_r_k, rw_lnx_g,
              rw_lnx_b, rw_w_o, moe_w_router, moe_b_router, moe_w_gate, moe_w_up, moe_w_down):
    B = x.shape[0]
    h = jnp.concatenate([jnp.broadcast_to(meta[None].astype(x.dtype), (B, N_META, D_MODEL)), x], 1)
    for i in range(DEPTH):
        j = i // N_MIXERS
        if i % N_MIXERS == 0:
            mix = diff_attention(h, attn_w_in[j], attn_w_o[j], attn_lam_q1[j], attn_lam_k1[j],
                                 attn_lam_q2[j], attn_lam_k2[j], attn_subln_g[j], i)
        else:
            mix = rwkv7_time_mix(h, rw_mu[j], rw_w_rkv[j], rw_w0[j], rw_w1[j], rw_w2[j], rw_a0[j],
                                 rw_a1[j], rw_a2[j], rw_g1[j], rw_g2[j], rw_k_k[j], rw_k_a[j],
                                 rw_r_k[j], rw_lnx_g[j], rw_lnx_b[j], rw_w_o[j])
        h = layer_norm(DEEPNORM_ALPHA * h + mix, ln_g[i, 0], ln_b[i, 0])
        if i % 2 == 0:
            ff = swiglu(h, ffn_w_gate[j], ffn_w_up[j], ffn_w_down[j])
        else:
            ff = moe_swiglu(h, moe_w_router[j], moe_b_router[j], moe_w_gate[j], moe_w_up[j], moe_w_down[j])
        h = layer_norm(DEEPNORM_ALPHA * h + ff, ln_g[i, 1], ln_b[i, 1])
    return h[:, N_META:]
```

```python
import numpy as np
from contextlib import ExitStack
import concourse.bass as bass
import concourse.mybir as mybir
from concourse.bass_utils import run_bass_kernel_spmd

F32 = mybir.dt.float32
BF16 = mybir.dt.bfloat16
ALU = mybir.AluOpType
AF = mybir.ActivationFunctionType
AX = mybir.AxisListType


class Sem:
    def __init__(self, h, is_dma=False):
        self.h = h
        self.cnt = 0
        self.is_dma = is_dma


class Buf:
    def __init__(self, name):
        self.name = name
        self.w = None
        self.r = []


class Prog:
    ENG = ("pe", "act", "dve", "pool", "sp")

    def __init__(self):
        self.nc = bass.Bass("TRN2", target_bir_lowering=False)
        self.es = ExitStack()
        nc = self.nc
        self.eng = {"pe": nc.tensor, "act": nc.scalar, "dve": nc.vector, "pool": nc.gpsimd, "sp": nc.sync}
        self.esem = {e: Sem(self.es.enter_context(nc.semaphore("sem_" + e))) for e in self.ENG}
        self.waited = {e: {} for e in self.ENG}
        self.nsem = 0
        self.dsems = []
        self.root_es = self.es
        self.ninst = 0

    def dram(self, name, shape, dt, kind):
        return self.nc.dram_tensor(name, list(shape), dt, kind=kind).ap()

    def _uniq(self, name):
        self.nalloc = getattr(self, "nalloc", 0) + 1
        return "%s_u%d" % (name, self.nalloc)

    def sb(self, name, shape, dt):
        return self.es.enter_context(self.nc.sbuf_tensor(self._uniq(name), list(shape), dt))

    def ps(self, name, shape, dt):
        return self.es.enter_context(self.nc.psum_tensor(self._uniq(name), list(shape), dt))

    def newsem(self, name=None):
        self.nsem += 1
        sm = self._mksem(name)
        sm.is_dma = True
        self.dsems.append(sm)
        return sm

    def _mksem(self, name):
        return Sem(self.root_es.enter_context(self.nc.semaphore(name or ("ds%d" % self.nsem))))

    def _wait(self, e, ev, raw=True):
        sem, val = ev
        if sem is self.esem[e] and (e == "pe" or not raw):
            return
        if sem.is_dma:
            val = sem.cnt
        if self.waited[e].get(sem, 0) >= val:
            return
        self.waited[e][sem] = val
        self.eng[e].wait_ge(sem.h, val)

    def _deps(self, e, reads, writes):
        for b in reads:
            if b.w is not None:
                self._wait(e, b.w)
        for b in writes:
            if b.w is not None:
                self._wait(e, b.w, raw=False)
            for ev in b.r:
                self._wait(e, ev, raw=False)

    def _commit(self, ev, reads, writes):
        for b in reads:
            b.r.append(ev)
            if len(b.r) > 64:
                b.r = b.r[-64:]
        for b in writes:
            b.w = ev
            b.r = []

    def op(self, e, fn, reads=(), writes=(), accum=False):
        self._deps(e, reads, writes)
        s = self.esem[e]
        inst = fn(self.eng[e])
        s.cnt += 1
        inst.then_inc(s.h, 1)
        self._commit((s, s.cnt), reads, writes)
        self.ninst += 1
        return inst

    def dma(self, q, out, in_, sem, reads=(), writes=(), **kw):
        if sem is None:
            b0 = writes[0]
            if getattr(b0, "dsem", None) is None:
                b0.dsem = self.newsem()
            sem = b0.dsem
        self._deps(q, reads, writes)
        inst = self.eng[q].dma_start(out=out, in_=in_, **kw)
        sem.cnt += 16
        inst.then_inc(sem.h, 16)
        self._commit((sem, sem.cnt), reads, writes)
        self.ninst += 1
        return inst

    def barrier(self):
        evs = [(s, s.cnt) for s in list(self.esem.values()) + self.dsems if s.cnt > 0]
        for e in self.ENG:
            for ev in evs:
                self._wait(e, ev)

    def scope(self):
        prog = self
        class _S:
            def __enter__(s2):
                s2.old = prog.es
                prog.es = ExitStack()
                return prog
            def __exit__(s2, *a):
                prog.es.close()
                prog.es = s2.old
                return False
        return _S()

    def finish(self, bufs, e="sp"):
        for b in bufs:
            if b.w is not None:
                self._wait(e, b.w)
            for ev in b.r:
                self._wait(e, ev)

    def close(self):
        self.es.close()
        return self.nc

import math
ALPHA = 4.0 ** 0.25
LN_EPS = 1e-5

class Common:
    def __init__(self, P):
        self.P = P

def load_w_bf16(P, name, w_ap, K, N, sem):
    kc = K // 128
    t = P.sb(name, [128, kc, N], BF16)
    B = Buf(name)
    for c in range(kc):
        P.dma("pool", t[:, c, :], w_ap[c * 128:(c + 1) * 128, :], sem, writes=[B])
    return t, B

def load_bcast(P, name, v_ap, N, sem):
    t = P.sb(name, [128, N], F32)
    B = Buf(name)
    P.dma("sp", t[:], v_ap.to_broadcast([128, N]), sem, writes=[B])
    return t, B

def layer_norm(P, x, Bx, g, Bg, b, Bb, scr, Bscr, out, Bout, obf=None, Bobf=None, D=1024):
    nh = D // 512
    st, mv = scr
    for i in range(nh):
        P.op("dve", lambda e, i=i: e.bn_stats(st[:, i * 6:(i + 1) * 6], x[:, i * 512:(i + 1) * 512]), reads=[Bx], writes=[Bscr])
    P.op("dve", lambda e: e.bn_aggr(mv[:, 0:2], st[:, 0:6 * nh]), reads=[Bscr], writes=[Bscr])
    P.op("act", lambda e: e.activation(mv[:, 2:3], mv[:, 1:2], AF.Ln, bias=LN_EPS, scale=1.0), reads=[Bscr], writes=[Bscr])
    P.op("act", lambda e: e.activation(mv[:, 3:4], mv[:, 2:3], AF.Exp, scale=-0.5), reads=[Bscr], writes=[Bscr])
    P.op("dve", lambda e: e.tensor_scalar(out[:, :], x[:, :], mv[:, 0:1], mv[:, 3:4], ALU.subtract, ALU.mult), reads=[Bx, Bscr], writes=[Bout])
    P.op("pool", lambda e: e.tensor_tensor(out[:, :], out[:, :], g[:, :], ALU.mult), reads=[Bout, Bg], writes=[Bout])
    P.op("pool", lambda e: e.tensor_tensor(out[:, :], out[:, :], b[:, :], ALU.add), reads=[Bout, Bb], writes=[Bout])
    if obf is not None:
        P.op("act", lambda e: e.activation(obf[:, :], out[:, :], AF.Identity), reads=[Bout], writes=[Bobf])

def transpose_tile(P, src_bf, Bsrc, ident, Bid, pt, Bpt, dst, Bdst, tslot, nchunk=8, evac="act"):
    for c in range(nchunk):
        P.op("pe", lambda e, c=c: e.transpose(pt[:, c, :], src_bf[:, c * 128:(c + 1) * 128], ident[:, :]), reads=[Bsrc, Bid], writes=[Bpt])
    if evac == "act":
        P.op("act", lambda e: e.activation(dst[:, 0:nchunk, tslot * 128:(tslot + 1) * 128], pt[:, 0:nchunk, :], AF.Identity), reads=[Bpt], writes=[Bdst])
    else:
        P.op("dve", lambda e: e.tensor_copy(dst[:, 0:nchunk, tslot * 128:(tslot + 1) * 128], pt[:, 0:nchunk, :]), reads=[Bpt], writes=[Bdst])

def build_phaseB(ntiles, F=2816, D=1024):
    P = Prog(); nc = P.nc
    T = ntiles * 128
    FC = F // 128
    o_d = P.dram("o", [T, D], F32, "ExternalInput")
    h0_d = P.dram("h0", [T, D], F32, "ExternalInput")
    wo_d = P.dram("wo", [D, D], F32, "ExternalInput")
    wg_d = P.dram("wg", [D, F], F32, "ExternalInput")
    wu_d = P.dram("wu", [D, F], F32, "ExternalInput")
    wd_d = P.dram("wd", [F, D], F32, "ExternalInput")
    lng_d = P.dram("lng", [2, D], F32, "ExternalInput")
    lnb_d = P.dram("lnb", [2, D], F32, "ExternalInput")
    id_d = P.dram("ident", [128, 128], F32, "ExternalInput")
    out_d = P.dram("h1", [T, D], F32, "ExternalOutput")
    wsem = P.newsem("wsem")
    wo, Bwo = load_w_bf16(P, "wo_sb", wo_d, D, D, wsem)
    wg, Bwg = load_w_bf16(P, "wg_sb", wg_d, D, F, wsem)
    wu, Bwu = load_w_bf16(P, "wu_sb", wu_d, D, F, wsem)
    wd, Bwd = load_w_bf16(P, "wd_sb", wd_d, F, D, wsem)
    csem = P.newsem("csem")
    g1, Bg1 = load_bcast(P, "g1", lng_d[0:1, :], D, csem)
    b1, Bb1 = load_bcast(P, "b1", lnb_d[0:1, :], D, csem)
    g2, Bg2 = load_bcast(P, "g2", lng_d[1:2, :], D, csem)
    b2, Bb2 = load_bcast(P, "b2", lnb_d[1:2, :], D, csem)
    ident = P.sb("ident_sb", [128, 128], BF16); Bid = Buf("ident")
    P.dma("pool", ident[:], id_d[:, :], csem, writes=[Bid])
    GT = 2
    W = GT * 128
    o32 = P.sb("o32", [128, D], F32); Bo32 = Buf("o32"); o32sem = P.newsem()
    obf = P.sb("obf", [128, D], BF16); Bobf = Buf("obf")
    res = [P.sb("res%d" % i, [128, D], F32) for i in range(GT)]; Bres = [Buf("res%d" % i) for i in range(GT)]
    ressem = [P.newsem() for i in range(GT)]
    hbf = P.sb("hbf", [128, D], BF16); Bhbf = Buf("hbf")
    xT = P.sb("xT", [128, 8, W], BF16); BxT = Buf("xT")
    hidT = P.sb("hidT", [128, FC, W], BF16); BhidT = Buf("hidT")
    sg = [P.sb("sg%d" % i, [128, W], F32) for i in range(2)]; Bsg = [Buf("sg%d" % i) for i in range(2)]
    st = P.sb("lnst", [128, 12], F32); mv = P.sb("lnmv", [128, 4], F32); Bscr = Buf("lnscr")
    outt = P.sb("outt", [128, D], F32); Boutt = Buf("outt"); outsem = P.newsem()
    pt = [P.ps("pt%d" % i, [128, 8, 128], BF16) for i in range(2)]; Bpt = [Buf("pt%d" % i) for i in range(2)]
    pm = [P.ps("pm%d" % i, [128, 512], F32) for i in range(2)]; Bpm = [Buf("pm%d" % i) for i in range(2)]
    pg = [P.ps("pg%d" % i, [128, 512], F32) for i in range(2)]; Bpg = [Buf("pg%d" % i) for i in range(2)]
    pu = [P.ps("pu%d" % i, [128, 512], F32) for i in range(2)]; Bpu = [Buf("pu%d" % i) for i in range(2)]
    Bout_d = Buf("out_d")
    ptc = 0
    ngroups = (ntiles + GT - 1) // GT
    for gi in range(ngroups):
        tiles = list(range(gi * GT, min(ntiles, (gi + 1) * GT)))
        w = len(tiles) * 128
        for j, t in enumerate(tiles):
            P.dma("sp", o32[:], o_d[t * 128:(t + 1) * 128, :], o32sem, writes=[Bo32])
            P.dma("sp", res[j][:], h0_d[t * 128:(t + 1) * 128, :], ressem[j], writes=[Bres[j]])
            P.op("act", lambda e: e.activation(obf[:, :], o32[:, :], AF.Identity), reads=[Bo32], writes=[Bobf])
            transpose_tile(P, obf, Bobf, ident, Bid, pt[ptc % 2], Bpt[ptc % 2], xT, BxT, j); ptc += 1
        for j, t in enumerate(tiles):
            for nh in range(2):
                for c in range(8):
                    P.op("pe", lambda e, c=c, nh=nh, j=j: e.matmul(pm[nh][:, :], xT[:, c, j * 128:(j + 1) * 128], wo[:, c, nh * 512:(nh + 1) * 512], start=(c == 0), stop=(c == 7)),
                         reads=[BxT, Bwo], writes=[Bpm[nh]])
                P.op("dve", lambda e, nh=nh, j=j: e.scalar_tensor_tensor(res[j][:, nh * 512:(nh + 1) * 512], res[j][:, nh * 512:(nh + 1) * 512], ALPHA, pm[nh][:, :], ALU.mult, ALU.add),
                     reads=[Bres[j], Bpm[nh]], writes=[Bres[j]])
            layer_norm(P, res[j], Bres[j], g1, Bg1, b1, Bb1, (st, mv), Bscr, res[j], Bres[j], hbf, Bhbf)
            transpose_tile(P, hbf, Bhbf, ident, Bid, pt[ptc % 2], Bpt[ptc % 2], xT, BxT, j); ptc += 1
        for fc in range(FC):
            k = fc % 2
            for c in range(8):
                P.op("pe", lambda e, c=c, fc=fc, k=k: e.matmul(pg[k][:, 0:w], wg[:, c, fc * 128:(fc + 1) * 128], xT[:, c, 0:w], start=(c == 0), stop=(c == 7)),
                     reads=[BxT, Bwg], writes=[Bpg[k]])
            for c in range(8):
                P.op("pe", lambda e, c=c, fc=fc, k=k: e.matmul(pu[k][:, 0:w], wu[:, c, fc * 128:(fc + 1) * 128], xT[:, c, 0:w], start=(c == 0), stop=(c == 7)),
                     reads=[BxT, Bwu], writes=[Bpu[k]])
            P.op("act", lambda e, k=k: e.activation(sg[k][:, 0:w], pg[k][:, 0:w], AF.Silu), reads=[Bpg[k]], writes=[Bsg[k]])
            P.op("dve", lambda e, k=k, fc=fc: e.tensor_tensor(hidT[:, fc, 0:w], sg[k][:, 0:w], pu[k][:, 0:w], ALU.mult), reads=[Bsg[k], Bpu[k]], writes=[BhidT])
        for j, t in enumerate(tiles):
            for nh in range(2):
                for fc in range(FC):
                    P.op("pe", lambda e, fc=fc, nh=nh, j=j: e.matmul(pm[nh][:, :], hidT[:, fc, j * 128:(j + 1) * 128], wd[:, fc, nh * 512:(nh + 1) * 512], start=(fc == 0), stop=(fc == FC - 1)),
                         reads=[BhidT, Bwd], writes=[Bpm[nh]])
                P.op("dve", lambda e, nh=nh, j=j: e.scalar_tensor_tensor(res[j][:, nh * 512:(nh + 1) * 512], res[j][:, nh * 512:(nh + 1) * 512], ALPHA, pm[nh][:, :], ALU.mult, ALU.add),
                     reads=[Bres[j], Bpm[nh]], writes=[Bres[j]])
            layer_norm(P, res[j], Bres[j], g2, Bg2, b2, Bb2, (st, mv), Bscr, outt, Boutt)
            P.dma("sp", out_d[t * 128:(t + 1) * 128, :], outt[:], outsem, reads=[Boutt], writes=[Bout_d])
    P.finish([Bout_d])
    print("phaseB insts", P.ninst)
    return P.close()


import math
SUBLN_EPS = 1e-5
NEG = -30000.0

def attn_consts(heads, nxt):
    T = (nxt + 1) * 128
    qaug = np.zeros((2, 5, 512), np.float32)
    u = np.arange(512)
    qaug[0] = np.stack([u // 16, u % 16, np.ones(512), np.ones(512), np.ones(512)])
    qaug[1] = np.stack([-(u // 16), -(u % 16), -np.ones(512), -np.ones(512), np.ones(512)])
    kaug = np.zeros((len(heads), 5, T), np.float32)
    v = np.arange(T) % 128
    for i, h in enumerate(heads):
        sl = 2.0 ** (-(h + 1))
        kaug[i, 0] = -16 * sl
        kaug[i, 1] = -sl
        kaug[i, 2] = 16 * sl * (v // 16)
        kaug[i, 3] = sl * (v % 16)
        kaug[i, 4, nxt * 128 + 16:] = NEG
    return qaug, kaug

def build_phaseA(NH, nxt, NCT=1024, D=1024):
    P = Prog(); nc = P.nc
    NT = nxt + 1
    T = NT * 128
    x_d = P.dram("xp", [T, D], F32, "ExternalInput")
    wq_d = P.dram("wq", [NH, D, 128], F32, "ExternalInput")
    wk_d = P.dram("wk", [NH, D, 128], F32, "ExternalInput")
    wv_d = P.dram("wv", [NH, D, 128], F32, "ExternalInput")
    lam_d = P.dram("lamv", [4, 64], F32, "ExternalInput")
    sg_d = P.dram("subg", [1, 128], F32, "ExternalInput")
    qaug_d = P.dram("qaug", [2, 5, 512], F32, "ExternalInput")
    kaug_d = P.dram("kaug", [NH, 5, T], F32, "ExternalInput")
    ctab_d = P.dram("ctab", [NH, 1, NCT], F32, "ExternalInput")
    id_d = P.dram("ident", [128, 128], F32, "ExternalInput")
    o_d = P.dram("o", [T, NH * 128], F32, "ExternalOutput")
    Bout_d = Buf("o_d")
    csem = None
    ident = P.sb("ident_sb", [128, 128], BF16); Bid = Buf("ident")
    P.dma("pool", ident[:], id_d[:, :], csem, writes=[Bid])
    lv = P.sb("lv", [128, 4, 64], F32); Blv = Buf("lv")
    for i in range(4):
        P.dma("sp", lv[:, i, :], lam_d[i:i + 1, :].to_broadcast([128, 64]), csem, writes=[Blv])
    lsc = P.sb("lsc", [128, 8], F32); Blsc = Buf("lsc")
    lt = P.sb("lt", [128, 2, 64], F32)
    P.op("dve", lambda e: e.tensor_tensor(lt[:, 0, :], lv[:, 0, :], lv[:, 1, :], ALU.mult), reads=[Blv], writes=[Blsc])
    P.op("dve", lambda e: e.tensor_tensor(lt[:, 1, :], lv[:, 2, :], lv[:, 3, :], ALU.mult), reads=[Blv], writes=[Blsc])
    P.op("dve", lambda e: e.reduce_sum(lsc[:, 0:1], lt[:, 0, :], AX.X), reads=[Blsc], writes=[Blsc])
    P.op("dve", lambda e: e.reduce_sum(lsc[:, 1:2], lt[:, 1, :], AX.X), reads=[Blsc], writes=[Blsc])
    P.op("act", lambda e: e.activation(lsc[:, 2:4], lsc[:, 0:2], AF.Exp), reads=[Blsc], writes=[Blsc])
    P.op("dve", lambda e: e.tensor_tensor(lsc[:, 4:5], lsc[:, 3:4], lsc[:, 2:3], ALU.subtract), reads=[Blsc], writes=[Blsc])
    P.op("dve", lambda e: e.tensor_scalar(lsc[:, 5:6], lsc[:, 4:5], -0.2, None, ALU.add), reads=[Blsc], writes=[Blsc])
    neglam = lsc[:, 5:6]
    gsc, Bgsc = load_bcast(P, "gsc", sg_d[0:1, :], 128, csem)
    P.op("dve", lambda e: e.tensor_scalar(gsc[:, :], gsc[:, :], 0.8, None, ALU.mult), reads=[Bgsc], writes=[Bgsc])
    QT = P.sb("QT", [128, T], BF16); BQT = Buf("QT")
    KT = [P.sb("KT%d" % c, [69, T], BF16) for c in range(2)]; BKT = [Buf("KT%d" % c) for c in range(2)]
    VA = P.sb("VA", [128, NT, 129], BF16); BVA = Buf("VA")
    P.op("pool", lambda e: e.memset(VA[:, :, 128:129], 1.0), writes=[BVA])
    ctab = P.sb("ctab_sb", [128, NCT], F32); Bctab = Buf("ctab")
    QA = [[P.sb("QA%d%d" % (c, lr), [69, 512], BF16) for lr in range(2)] for c in range(2)]
    BQA = [[Buf("QA%d%d" % (c, lr)) for lr in range(2)] for c in range(2)]
    for c in range(2):
        for lr in range(2):
            P.dma("pool", QA[c][lr][64:69, :], qaug_d[lr, :, :], csem, writes=[BQA[c][lr]])
    wq = P.sb("wq_sb", [128, 8, 128], BF16); wk = P.sb("wk_sb", [128, 8, 128], BF16); wv = P.sb("wv_sb", [128, 8, 128], BF16)
    Bw = Buf("w_head"); wsem = None
    ctab_vals = [dict() for _ in range(NH)]
    def ccol(hi, val):
        d = ctab_vals[hi]
        if val not in d:
            d[val] = len(d)
            assert len(d) <= NCT
        return d[val]
    base = lambda tile: (0 if tile == nxt else 16 + 128 * tile)
    for hi in range(NH):
        for (dst, src) in ((wq, wq_d), (wk, wk_d), (wv, wv_d)):
            for c in range(8):
                P.dma("pool", dst[:, c, :], src[hi, c * 128:(c + 1) * 128, :], wsem, writes=[Bw])
        P.dma("sp", ctab[:], ctab_d[hi, 0:1, :].to_broadcast([128, NCT]), csem, writes=[Bctab])
        for c in range(2):
            P.dma("pool", KT[c][64:69, :], kaug_d[hi, :, :], csem, writes=[BKT[c]])
        P.barrier()
        with P.scope():
            x32 = [P.sb("x32_%d" % i, [128, D], F32) for i in range(2)]; Bx32 = [Buf("x32") for i in range(2)]; xsem = [P.newsem() for i in range(2)]
            xbf = [P.sb("xbf_%d" % i, [128, D], BF16) for i in range(2)]; Bxbf = [Buf("xbf") for i in range(2)]
            xT = [P.sb("xT_%d" % i, [128, 8, 512], BF16) for i in range(2)]; BxT = [Buf("xT") for i in range(2)]
            pt = [P.ps("pt%d" % i, [128, 8, 128], BF16) for i in range(2)]; Bpt = [Buf("pt") for i in range(2)]
            pq = [P.ps("pq%d" % i, [128, 512], F32) for i in range(2)]; Bpq = [Buf("pq") for i in range(2)]
            pk = [P.ps("pk%d" % i, [128, 512], F32) for i in range(2)]; Bpk = [Buf("pk") for i in range(2)]
            pv = [P.ps("pv%d" % i, [128, 512], F32) for i in range(2)]; Bpv = [Buf("pv") for i in range(2)]
            tcnt = 0
            ngr = (NT + 3) // 4
            for gi in range(ngr):
                tiles = list(range(gi * 4, min(NT, gi * 4 + 4)))
                w = len(tiles) * 128
                k2 = gi % 2
                for j, t in enumerate(tiles):
                    s = tcnt % 2; tcnt += 1
                    P.dma("sp", x32[s][:], x_d[t * 128:(t + 1) * 128, :], xsem[s], writes=[Bx32[s]])
                    P.op("dve" if j % 2 else "pool", lambda e, s=s: e.tensor_copy(xbf[s][:, :], x32[s][:, :]), reads=[Bx32[s]], writes=[Bxbf[s]])
                    transpose_tile(P, xbf[s], Bxbf[s], ident, Bid, pt[s], Bpt[s], xT[k2], BxT[k2], j, evac="act" if j % 2 else "dve")
                tok0 = tiles[0] * 128
                for c in range(8):
                    P.op("pe", lambda e, c=c: e.matmul(pq[k2][:, 0:w], wq[:, c, :], xT[k2][:, c, 0:w], start=(c == 0), stop=(c == 7)), reads=[Bw, BxT[k2]], writes=[Bpq[k2]])
                P.op("act", lambda e: e.activation(QT[:, tok0:tok0 + w], pq[k2][:, 0:w], AF.Identity, scale=0.125), reads=[Bpq[k2]], writes=[BQT])
                for cc in range(2):
                    for c in range(8):
                        P.op("pe", lambda e, c=c, cc=cc: e.matmul(pk[k2][0:64, cc * 0 + 0:w] if False else pk[k2][0:64, 0:w], wk[:, c, cc * 64:(cc + 1) * 64], xT[k2][:, c, 0:w], start=(c == 0), stop=(c == 7)), reads=[Bw, BxT[k2]], writes=[Bpk[k2]])
                    P.op("dve", lambda e, cc=cc: e.tensor_copy(KT[cc][0:64, tok0:tok0 + w], pk[k2][0:64, 0:w]), reads=[Bpk[k2]], writes=[BKT[cc]])
                for j, t in enumerate(tiles):
                    for c in range(8):
                        P.op("pe", lambda e, c=c, j=j: e.matmul(pv[k2][:, j * 128:(j + 1) * 128], xT[k2][:, c, j * 128:(j + 1) * 128], wv[:, c, :], start=(c == 0), stop=(c == 7)), reads=[Bw, BxT[k2]], writes=[Bpv[k2]])
                P.op("act", lambda e: e.activation(VA[:, tiles[0]:tiles[0] + len(tiles), 0:128], pv[k2][:, 0:w].rearrange("p (t d) -> p t d", d=128), AF.Identity), reads=[Bpv[k2]], writes=[BVA])
        P.barrier()
        with P.scope():
            psS = [[P.ps("psS%d%d" % (c, i), [128, 512], F32) for i in range(2)] for c in range(2)]
            BpsS = [[Buf("psS") for i in range(2)] for c in range(2)]
            oz = [[P.ps("oz%d%d" % (c, i), [128, 512], F32) for i in range(2)] for c in range(2)]
            Boz = [Buf("oz%d" % c) for c in range(2)]
            PT = [[P.sb("PT%d%d" % (c, i), [128, 512], BF16) for i in range(3)] for c in range(2)]
            BPT = [[Buf("PT") for i in range(3)] for c in range(2)]
            srt = [P.sb("srt%d" % c, [128, 512], F32) for c in range(2)]; Bsrt = [Buf("srt") for c in range(2)]
            fz = P.sb("fz", [128, 16], F32); Bfz = Buf("fz")
            fo = [P.sb("fo%d" % i, [128, 128], F32) for i in range(2)]; Bfo = [Buf("fo") for i in range(2)]
            fo2 = P.sb("fo2", [128, 128], F32); Bfo2 = Buf("fo2")
            osb = [P.sb("osb%d" % i, [128, 128], F32) for i in range(2)]; Bosb = [Buf("osb") for i in range(2)]; osem = [P.newsem() for i in range(2)]
            qtiles = [(j * 4, 4) for j in range(nxt // 4)] + [(nxt, 1)]
            assert nxt % 4 == 0
            it = 0; fcnt = 0
            for (qt0, qn) in qtiles:
                Wq = qn * 128
                qbase = base(qt0)
                for c in range(2):
                    for lr in range(2):
                        P.op("pool" if lr else "dve", lambda e, c=c, lr=lr: e.tensor_copy(QA[c][lr][0:64, 0:Wq], QT[c * 64:(c + 1) * 64, qt0 * 128:qt0 * 128 + Wq]), reads=[BQT], writes=[BQA[c][lr]])
                order = list(range(NT))
                for ki, i in enumerate(order):
                    Dq = qbase - base(i)
                    if Dq >= 127: typ = 0
                    elif Dq + Wq - 1 <= 0: typ = 1
                    else: typ = 2
                    for c in range(2):
                        s2 = it % 2; s3 = it % 3
                        ps = psS[c][s2]; Bps = BpsS[c][s2]
                        pt_ = PT[c][s3]; Bp = BPT[c][s3]
                        if typ < 2:
                            P.op("pe", lambda e, c=c, i=i, typ=typ, ps=ps: e.matmul(ps[:, 0:Wq], KT[c][0:69, i * 128:(i + 1) * 128], QA[c][typ][0:69, 0:Wq], start=True, stop=True), reads=[BKT[c], BQA[c][typ]], writes=[Bps])
                            col = ccol(hi, -abs(Dq))
                            P.op("act", lambda e, ps=ps, pt_=pt_, col=col: e.activation(pt_[:, 0:Wq], ps[:, 0:Wq], AF.Exp, bias=ctab[:, col:col + 1], scale=1.0), reads=[Bps, Bctab], writes=[Bp])
                        else:
                            ps2 = psS[c][1 - s2]; Bps2 = BpsS[c][1 - s2]
                            P.op("pe", lambda e, c=c, i=i, ps=ps: e.matmul(ps[:, 0:Wq], KT[c][0:69, i * 128:(i + 1) * 128], QA[c][0][0:69, 0:Wq], start=True, stop=True), reads=[BKT[c], BQA[c][0]], writes=[Bps])
                            P.op("pe", lambda e, c=c, i=i, ps2=ps2: e.matmul(ps2[:, 0:Wq], KT[c][0:69, i * 128:(i + 1) * 128], QA[c][1][0:69, 0:Wq], start=True, stop=True), reads=[BKT[c], BQA[c][1]], writes=[Bps2])
                            colL = ccol(hi, -Dq); colR = ccol(hi, Dq)
                            P.op("act", lambda e, c=c, ps2=ps2, colR=colR: e.activation(srt[c][:, 0:Wq], ps2[:, 0:Wq], AF.Identity, bias=ctab[:, colR:colR + 1], scale=1.0), reads=[Bps2, Bctab], writes=[Bsrt[c]])
                            P.op("dve", lambda e, c=c, ps=ps, colL=colL: e.scalar_tensor_tensor(srt[c][:, 0:Wq], ps[:, 0:Wq], ctab[:, colL:colL + 1], srt[c][:, 0:Wq], ALU.add, ALU.min), reads=[Bps, Bsrt[c], Bctab], writes=[Bsrt[c]])
                            P.op("act", lambda e, c=c, pt_=pt_: e.activation(pt_[:, 0:Wq], srt[c][:, 0:Wq], AF.Exp), reads=[Bsrt[c]], writes=[Bp])
                        for sub in range(qn):
                            P.op("pe", lambda e, c=c, sub=sub, i=i, pt_=pt_, ki=ki: e.matmul(oz[c][sub // 2][:, (sub % 2) * 129:(sub % 2) * 129 + 129], pt_[:, sub * 128:(sub + 1) * 128], VA[:, i, :], start=(ki == 0 and sub % 2 == 0), stop=(ki == NT - 1), skip_group_check=True), reads=[Bp, BVA], writes=[Boz[c]])
                    it += 1
                for sub in range(qn):
                    f = fcnt % 2; fcnt += 1
                    o0 = oz[0][sub // 2][:, (sub % 2) * 129:(sub % 2) * 129 + 129]; o1 = oz[1][sub // 2][:, (sub % 2) * 129:(sub % 2) * 129 + 129]
                    P.op("dve", lambda e, o0=o0: e.reciprocal(fz[:, 0:1], o0[:, 128:129]), reads=[Boz[0]], writes=[Bfz])
                    P.op("dve", lambda e, o1=o1: e.reciprocal(fz[:, 1:2], o1[:, 128:129]), reads=[Boz[1]], writes=[Bfz])
                    P.op("dve", lambda e: e.tensor_tensor(fz[:, 2:3], fz[:, 1:2], neglam, ALU.mult), reads=[Bfz, Blsc], writes=[Bfz])
                    P.op("dve", lambda e, o0=o0, f=f: e.tensor_scalar(fo[f][:, :], o0[:, 0:128], fz[:, 0:1], None, ALU.mult), reads=[Boz[0], Bfz], writes=[Bfo[f]])
                    P.op("dve", lambda e, o1=o1, f=f: e.scalar_tensor_tensor(fo[f][:, :], o1[:, 0:128], fz[:, 2:3], fo[f][:, :], ALU.mult, ALU.add), reads=[Boz[1], Bfz, Bfo[f]], writes=[Bfo[f]])
                    P.op("act", lambda e, f=f: e.activation(fo2[:, :], fo[f][:, :], AF.Square, accum_out=fz[:, 3:4]), reads=[Bfo[f]], writes=[Bfo2, Bfz])
                    P.op("act", lambda e: e.activation(fz[:, 4:5], fz[:, 3:4], AF.Ln, bias=SUBLN_EPS, scale=1.0 / 128), reads=[Bfz], writes=[Bfz])
                    P.op("act", lambda e: e.activation(fz[:, 5:6], fz[:, 4:5], AF.Exp, scale=-0.5), reads=[Bfz], writes=[Bfz])
                    P.op("dve", lambda e, f=f: e.scalar_tensor_tensor(osb[f][:, :], fo[f][:, :], fz[:, 5:6], gsc[:, :], ALU.mult, ALU.mult), reads=[Bfo[f], Bfz, Bgsc], writes=[Bosb[f]])
                    tt = qt0 + sub
                    P.dma("sp", o_d[tt * 128:(tt + 1) * 128, hi * 128:(hi + 1) * 128], osb[f][:], osem[f], reads=[Bosb[f]], writes=[Bout_d])
        P.barrier()
    P.finish([Bout_d])
    print("phaseA insts", P.ninst)
    nc = P.close()
    return nc, ctab_vals

def ref_attn(xp, pos, valid, w_in, lam4, subg, heads):
    T = xp.shape[0]
    D = 1024
    outs = []
    lam = np.exp((lam4[0] * lam4[1]).sum()) - np.exp((lam4[2] * lam4[3]).sum()) + 0.2
    for h in heads:
        q = xp @ w_in[:, h * 128:(h + 1) * 128]
        k = xp @ w_in[:, D + h * 128:D + (h + 1) * 128]
        v = xp @ w_in[:, 2 * D + h * 128:2 * D + (h + 1) * 128]
        sl = 2.0 ** (-(h + 1))
        dist = np.abs(pos[:, None] - pos[None, :]).astype(np.float32)
        ps = []
        for c in range(2):
            s = q[:, c * 64:(c + 1) * 64] @ k[:, c * 64:(c + 1) * 64].T / 8 - sl * dist
            s = np.where(valid[None, :], s, -np.inf)
            s = s - s.max(-1, keepdims=True)
            p = np.exp(s); p /= p.sum(-1, keepdims=True)
            ps.append(p)
        a = ps[0] - lam * ps[1]
        o = a @ v
        o = o / np.sqrt((o * o).mean(-1, keepdims=True) + 1e-5) * subg * 0.8
        outs.append(o)
    return np.concatenate(outs, 1)


import math
STAGE = 9
SKIP = ''
C = 64
NEGH = -math.exp(-0.5)

def rw_consts():
    m = np.zeros((64, 128), np.float32)
    s = np.arange(64)[:, None]; t = np.arange(64)[None, :]
    m[:, 0:64] = (s < t); m[:, 64:128] = (s <= t)
    return m

def build_phaseC(nch, NHD=8, D=1024, GW=512):
    P = Prog(); nc = P.nc
    Tp = nch * C
    NCH = NHD * 64
    xT_d = P.dram("xT", [D, Tp + 2], F32, "ExternalInput")
    mu_d = P.dram("mu", [D, 12], F32, "ExternalInput")
    wr_d = P.dram("wr", [D, NCH], F32, "ExternalInput"); wk_d = P.dram("wk", [D, NCH], F32, "ExternalInput"); wv_d = P.dram("wv", [D, NCH], F32, "ExternalInput")
    w1_d = P.dram("w1", [D, 64], F32, "ExternalInput"); w2_d = P.dram("w2", [64, NCH], F32, "ExternalInput")
    a1_d = P.dram("a1", [D, 64], F32, "ExternalInput"); a2_d = P.dram("a2", [64, NCH], F32, "ExternalInput")
    g1_d = P.dram("g1", [D, 160], F32, "ExternalInput"); g2_d = P.dram("g2", [160, NCH], F32, "ExternalInput")
    chv_d = P.dram("chv", [64, NHD, 8], F32, "ExternalInput")
    msk_d = P.dram("msk", [64, 128], F32, "ExternalInput")
    seg_d = P.dram("seg", [1, GW], F32, "ExternalInput")
    id_d = P.dram("ident", [128, 128], F32, "ExternalInput")
    y_d = P.dram("y", [Tp, NCH], F32, "ExternalOutput")
    aux_d = P.dram("aux", [4, NCH, Tp], F32, "ExternalOutput")
    By = Buf("y_d"); Baux = Buf("aux_d")
    ident = P.sb("ident_sb", [128, 128], BF16); Bid = Buf("ident"); P.dma("pool", ident[:], id_d[:, :], None, writes=[Bid])
    identf = P.sb("identf", [128, 128], F32); Bidf = Buf("identf"); P.dma("sp", identf[:], id_d[:, :], None, writes=[Bidf])
    def wload(name, src, K, N):
        kc = (K + 127) // 128
        t = P.sb(name, [128, kc, N], BF16); B = Buf(name)
        for c in range(kc):
            rows = min(128, K - c * 128)
            P.dma("pool", t[0:rows, c, :], src[c * 128:c * 128 + rows, :], None, writes=[B])
        return t, B
    wr, Bwr = wload("wr", wr_d, D, NCH); wk, Bwk = wload("wk", wk_d, D, NCH); wv, Bwv = wload("wv", wv_d, D, NCH)
    w1, Bw1 = wload("w1", w1_d, D, 64); a1, Ba1 = wload("a1", a1_d, D, 64); g1, Bg1 = wload("g1", g1_d, D, 160)
    w2, Bw2 = wload("w2", w2_d, 64, NCH); a2, Ba2 = wload("a2", a2_d, 64, NCH); g2, Bg2 = wload("g2", g2_d, 160, NCH)
    mu = P.sb("mu", [128, 8, 12], F32); Bmu = Buf("mu")
    P.dma("sp", mu[:], mu_d.rearrange("(c p) m -> p c m", p=128), None, writes=[Bmu])
    muc = P.sb("muc", [128, 8, 6], F32)
    P.op("dve", lambda e: e.tensor_tensor(muc[:, :, :], mu[:, :, 0:6], mu[:, :, 6:12], ALU.add), reads=[Bmu], writes=[Bmu])
    P.op("dve", lambda e: e.tensor_scalar(muc[:, :, :], muc[:, :, :], -1.0, 1.0, ALU.mult, ALU.add), reads=[Bmu], writes=[Bmu])
    chv = P.sb("chv", [64, NHD, 8], F32); Bchv = Buf("chv"); P.dma("sp", chv[:], chv_d[:, :, :], None, writes=[Bchv])
    P.op("dve", lambda e: e.tensor_scalar(chv[:, :, 4:5], chv[:, :, 3:4], -1.0, 1.0, ALU.mult, ALU.add), reads=[Bchv], writes=[Bchv])
    msk = P.sb("msk", [64, 128], F32); Bmsk = Buf("msk"); P.dma("sp", msk[:], msk_d[:, :], None, writes=[Bmsk])
    seg = P.sb("seg", [64, GW], F32); Bseg = Buf("seg"); P.dma("sp", seg[:], seg_d[0:1, :].to_broadcast([64, GW]), None, writes=[Bseg])
    ones64 = P.sb("ones64", [64, 64], F32); Bones = Buf("ones"); P.op("pool", lambda e: e.memset(ones64[:, :], 1.0), writes=[Bones])
    ST = P.sb("ST", [64, NHD, 64], F32); STb = P.sb("STb", [64, NHD, 64], BF16); BST = [Buf("ST%d" % h) for h in range(NHD)]
    P.op("pool", lambda e: e.memset(ST[:, :, :], 0.0), writes=BST)
    P.op("pool", lambda e: e.memset(STb[:, :, :], 0.0), writes=BST)
    x32 = P.sb("x32", [128, 8, GW + 2], F32); Bx32 = Buf("x32")
    xs = [P.sb("xs%d" % i, [128, 8, GW], BF16) for i in range(6)]; Bxs = [Buf("xs%d" % i) for i in range(6)]
    hw = P.sb("hw", [64, GW], BF16); Bhw = Buf("hw"); ha = P.sb("ha", [64, GW], BF16); Bha = Buf("ha")
    hg = P.sb("hg", [128, 2, GW], BF16); Bhg = Buf("hg")
    names = "r k v a lw kk ss t1 t2 Lc KRk".split()
    F = {n: P.sb("f_" + n, [64, GW], F32) for n in ["r", "k", "v", "a", "lw", "kk", "t1", "t2", "Lc", "kd", "g"]}
    BF = {n: Buf("f_" + n) for n in F}
    KR = P.sb("KR", [64, GW // C, 2, C], BF16); BKR = Buf("KR")
    KB = P.sb("KB", [64, 2, GW], BF16); BKB = Buf("KB")
    HT = P.sb("HT", [64, 3, GW], BF16); BHT = Buf("HT")
    gC = P.sb("gC", [64, GW // C], F32); BgC = Buf("gC")
    TM = P.sb("TM", [64, 3, 64], BF16); BTM = Buf("TM")
    MA = P.sb("MA", [64, 128], BF16); BMA = Buf("MA")
    MB = P.sb("MB", [64, 128], BF16); BMB = Buf("MB")
    NT_ = P.sb("NTt", [64, 64], BF16); BNT = Buf("NT")
    X = [P.sb("X%d" % i, [64, 64], BF16) for i in range(2)]; XT = [P.sb("XT%d" % i, [64, 64], BF16) for i in range(2)]
    BX = [Buf("X") for i in range(2)]; BXT = [Buf("XT") for i in range(2)]
    R = P.sb("R", [64, 64], F32); Rb = P.sb("Rb", [64, 64], BF16); BR = Buf("R")
    Wt = P.sb("Wt", [64, 64], BF16); BWt = Buf("Wt")
    Ut = P.sb("Ut", [64, 64], BF16); BUt = Buf("Ut")
    ysb = P.sb("ysb", [64, GW // C, NHD, 64], F32); Bysb = Buf("ysb")
    ysem = P.newsem()
    pp = [P.ps("pp%d" % i, [128, 512], F32) for i in range(2)]; Bpp = [Buf("pp") for i in range(2)]
    pl = P.ps("pl", [128, 512], F32); Bpl = Buf("pl")
    pm = [P.ps("pmm%d" % i, [64, 512], F32) for i in range(2)]; Bpmm = [Buf("pmm") for i in range(2)]
    pt = P.ps("ptr", [64, 3, 128], BF16); Bptr = Buf("ptr")
    pw = P.ps("pw", [64, 512], F32); Bpw = Buf("pw")
    psq = P.ps("psq", [64, 512], F32); Bpsq = Buf("psq")
    ngr = (Tp + GW - 1) // GW
    ppi = 0
    for gi in range(ngr):
        t0 = gi * GW
        W = min(GW, Tp - t0)
        ncg = W // C
        for c in range(8):
            P.dma("sp", x32[:, c, 0:W + 2], xT_d[c * 128:(c + 1) * 128, t0:t0 + W + 2], None, writes=[Bx32])
        for i in range(6):
            for c in range(8):
                eng = "dve" if (i * 8 + c) % 2 == 0 else "pool"
                P.op("dve", lambda e, i=i, c=c: e.tensor_scalar(xs[i][:, c, 0:W], x32[:, c, 1:W + 1], muc[:, c, i:i + 1], None, ALU.mult), reads=[Bx32, Bmu], writes=[Bxs[i]])
                P.op("dve", lambda e, i=i, c=c: e.scalar_tensor_tensor(xs[i][:, c, 0:W], x32[:, c, 0:W], mu[:, c, i:i + 1], xs[i][:, c, 0:W], ALU.mult, ALU.add), reads=[Bx32, Bmu, Bxs[i]], writes=[Bxs[i]])
                P.op("dve", lambda e, i=i, c=c: e.scalar_tensor_tensor(xs[i][:, c, 0:W], x32[:, c, 2:W + 2], mu[:, c, 6 + i:7 + i], xs[i][:, c, 0:W], ALU.mult, ALU.add), reads=[Bx32, Bmu, Bxs[i]], writes=[Bxs[i]])
        for c in range(8):
            P.op("pe", lambda e, c=c: e.matmul(pl[0:64, 0:W], w1[:, c, :], xs[3][:, c, 0:W], start=(c == 0), stop=(c == 7)), reads=[Bw1, Bxs[3]], writes=[Bpl])
        P.op("act", lambda e: e.activation(hw[:, 0:W], pl[0:64, 0:W], AF.Tanh), reads=[Bpl], writes=[Bhw])
        for c in range(8):
            P.op("pe", lambda e, c=c: e.matmul(pl[0:64, 0:W], a1[:, c, :], xs[4][:, c, 0:W], start=(c == 0), stop=(c == 7)), reads=[Ba1, Bxs[4]], writes=[Bpl])
        P.op("act", lambda e: e.activation(ha[:, 0:W], pl[0:64, 0:W], AF.Identity), reads=[Bpl], writes=[Bha])
        for part, (lo, n) in enumerate(((0, 128), (128, 32))):
            for c in range(8):
                P.op("pe", lambda e, c=c, lo=lo, n=n: e.matmul(pl[0:n, 0:W], g1[:, c, lo:lo + n], xs[5][:, c, 0:W], start=(c == 0), stop=(c == 7)), reads=[Bg1, Bxs[5]], writes=[Bpl])
            P.op("act", lambda e, part=part, n=n: e.activation(hg[0:n, part, 0:W], pl[0:n, 0:W], AF.Sigmoid), reads=[Bpl], writes=[Bhg])
        for h in range(NHD):
            cs = slice(h * 64, (h + 1) * 64)
            cw0, ca0, ckk, cka, c1ka = [chv[:, h, i:i + 1] for i in range(5)]
            def proj(wt, Bw, xi, dst, func=AF.Identity, **kw):
                nonlocal ppi
                p = pp[ppi % 2]; Bp = Bpp[ppi % 2]; ppi += 1
                for c in range(8):
                    P.op("pe", lambda e, c=c: e.matmul(p[0:64, 0:W], wt[:, c, cs], xs[xi][:, c, 0:W], start=(c == 0), stop=(c == 7)), reads=[Bw, Bxs[xi]], writes=[Bp])
                P.op("act", lambda e: e.activation(F[dst][:, 0:W], p[0:64, 0:W], func, **kw), reads=[Bp], writes=[BF[dst]])
            proj(wr, Bwr, 0, "r"); proj(wk, Bwk, 1, "k"); proj(wv, Bwv, 2, "v")
            def lora(w2t, Bw2_, hsrc, Bh, dst, bias):
                nonlocal ppi
                p = pp[ppi % 2]; Bp = Bpp[ppi % 2]; ppi += 1
                P.op("pe", lambda e: e.matmul(p[0:64, 0:W], w2t[0:64, 0, cs], hsrc[0:64, 0:W], start=True, stop=True), reads=[Bw2_, Bh], writes=[Bp])
                P.op("act", lambda e: e.activation(F[dst][:, 0:W], p[0:64, 0:W], AF.Sigmoid, bias=bias, scale=1.0), reads=[Bp, Bchv], writes=[BF[dst]])
            lora(w2, Bw2, hw, Bhw, "lw", cw0)
            lora(a2, Ba2, ha, Bha, "a", ca0)
            p = pp[ppi % 2]; Bp = Bpp[ppi % 2]; ppi += 1
            P.op("pe", lambda e: e.matmul(p[0:64, 0:W], g2[:, 0, cs], hg[:, 0, 0:W], start=True, stop=False), reads=[Bg2, Bhg], writes=[Bp])
            P.op("pe", lambda e: e.matmul(p[0:64, 0:W], g2[0:32, 1, cs], hg[0:32, 1, 0:W], start=False, stop=True), reads=[Bg2, Bhg], writes=[Bp])
            P.op("act", lambda e: e.activation(F["g"][:, 0:W], p[0:64, 0:W], AF.Identity), reads=[Bp], writes=[BF["g"]])
            P.op("dve", lambda e: e.tensor_scalar(F["kk"][:, 0:W], F["k"][:, 0:W], ckk, None, ALU.mult), reads=[BF["k"], Bchv], writes=[BF["kk"]])
            P.op("dve", lambda e: e.tensor_tensor(F["t1"][:, 0:W], F["kk"][:, 0:W], F["kk"][:, 0:W], ALU.mult), reads=[BF["kk"]], writes=[BF["t1"]])
            if 'a' not in SKIP:
                P.op("pe", lambda e: e.matmul(psq[:, 0:W], ones64[:, :], F["t1"][:, 0:W], start=True, stop=True), reads=[Bones, BF["t1"]], writes=[Bpsq])
            P.op("act", lambda e: e.activation(F["t2"][:, 0:W], psq[:, 0:W], AF.Ln, bias=1e-24, scale=1.0), reads=[Bpsq], writes=[BF["t2"]])
            P.op("act", lambda e: e.activation(F["t2"][:, 0:W], F["t2"][:, 0:W], AF.Exp, scale=-0.5), reads=[BF["t2"]], writes=[BF["t2"]])
            P.op("dve", lambda e: e.tensor_tensor(F["kk"][:, 0:W], F["kk"][:, 0:W], F["t2"][:, 0:W], ALU.mult), reads=[BF["kk"], BF["t2"]], writes=[BF["kk"]])
            P.op("dve", lambda e: e.tensor_scalar(F["t1"][:, 0:W], F["a"][:, 0:W], cka, c1ka, ALU.mult, ALU.add), reads=[BF["a"], Bchv], writes=[BF["t1"]])
            P.op("dve", lambda e: e.tensor_tensor(F["kd"][:, 0:W], F["k"][:, 0:W], F["t1"][:, 0:W], ALU.mult), reads=[BF["k"], BF["t1"]], writes=[BF["kd"]])
            P.op("pool", lambda e: e.tensor_tensor(F["a"][:, 0:W], F["a"][:, 0:W], F["kk"][:, 0:W], ALU.mult), reads=[BF["a"], BF["kk"]], writes=[BF["a"]])
            P.op("pool", lambda e: e.tensor_scalar(F["lw"][:, 0:W], F["lw"][:, 0:W], NEGH, None, ALU.mult), reads=[BF["lw"]], writes=[BF["lw"]])
            if 'c' not in SKIP:
              P.op("dve", lambda e: e.tensor_tensor_scan(F["Lc"][:, 0:W], seg[:, 0:W], F["lw"][:, 0:W], 0.0, ALU.mult, ALU.add), reads=[Bseg, BF["lw"]], writes=[BF["Lc"]])
            Lc3 = F["Lc"][:, 0:W].rearrange("p (n c) -> p n c", c=C)
            P.op("act", lambda e: e.activation(gC[:, 0:ncg], Lc3[:, :, C - 1], AF.Exp), reads=[BF["Lc"]], writes=[BgC])
            P.op("act", lambda e: e.activation(F["t1"][:, 0:W], F["Lc"][:, 0:W], AF.Exp), reads=[BF["Lc"]], writes=[BF["t1"]])
            P.op("dve", lambda e: e.tensor_tensor(KR[:, 0:ncg, 1, :], F["r"][:, 0:W].rearrange("p (n c) -> p n c", c=C), F["t1"][:, 0:W].rearrange("p (n c) -> p n c", c=C), ALU.mult), reads=[BF["r"], BF["t1"]], writes=[BKR])
            P.op("pool", lambda e: e.tensor_tensor(F["t2"][:, 0:W], F["Lc"][:, 0:W], F["lw"][:, 0:W], ALU.subtract), reads=[BF["Lc"], BF["lw"]], writes=[BF["t2"]])
            P.op("act", lambda e: e.activation(F["t2"][:, 0:W], F["t2"][:, 0:W], AF.Exp), reads=[BF["t2"]], writes=[BF["t2"]])
            P.op("dve", lambda e: e.tensor_tensor(KR[:, 0:ncg, 0, :], F["kk"][:, 0:W].rearrange("p (n c) -> p n c", c=C), F["t2"][:, 0:W].rearrange("p (n c) -> p n c", c=C), ALU.mult), reads=[BF["kk"], BF["t2"]], writes=[BKR])
            P.op("act", lambda e: e.activation(F["t1"][:, 0:W], F["Lc"][:, 0:W], AF.Exp, scale=-1.0), reads=[BF["Lc"]], writes=[BF["t1"]])
            P.op("dve", lambda e: e.tensor_tensor(KB[:, 0, 0:W], F["kd"][:, 0:W], F["t1"][:, 0:W], ALU.mult), reads=[BF["kd"], BF["t1"]], writes=[BKB])
            P.op("pool", lambda e: e.tensor_tensor(KB[:, 1, 0:W], F["a"][:, 0:W], F["t1"][:, 0:W], ALU.mult), reads=[BF["a"], BF["t1"]], writes=[BKB])
            if 'b' not in SKIP:
              P.op("dve", lambda e: e.tensor_tensor(F["t2"][:, 0:W].rearrange("p (n c) -> p n c", c=C), Lc3[:, :, C - 1:C].to_broadcast([64, ncg, C]), Lc3, ALU.subtract), reads=[BF["Lc"]], writes=[BF["t2"]])
            P.op("act", lambda e: e.activation(F["t2"][:, 0:W], F["t2"][:, 0:W], AF.Exp), reads=[BF["t2"]], writes=[BF["t2"]])
            P.op("dve", lambda e: e.tensor_tensor(HT[:, 0, 0:W], F["kd"][:, 0:W], F["t2"][:, 0:W], ALU.mult), reads=[BF["kd"], BF["t2"]], writes=[BHT])
            P.op("dve", lambda e: e.scalar_tensor_tensor(HT[:, 1, 0:W], F["a"][:, 0:W], -1.0, F["t2"][:, 0:W], ALU.mult, ALU.mult), reads=[BF["a"], BF["t2"]], writes=[BHT])
            P.op("pool", lambda e: e.tensor_copy(HT[:, 2, 0:W], F["v"][:, 0:W]), reads=[BF["v"]], writes=[BHT])
            for ai, nm in enumerate(("r", "kd", "v", "g")):
                P.dma("sp", aux_d[ai, h * 64:(h + 1) * 64, t0:t0 + W], F[nm][:, 0:W], None, reads=[BF[nm]], writes=[Baux])
            for ci in range(ncg if STAGE >= 2 else 0):
                cc = slice(ci * C, (ci + 1) * C)
                for q in range(3):
                    P.op("pe", lambda e, q=q: e.transpose(pt[:, q, 0:64], HT[:, q, cc], ident[0:64, 0:64]), reads=[BHT, Bid], writes=[Bptr])
                P.op("dve", lambda e: e.tensor_copy(TM[:, :, :], pt[:, :, 0:64]), reads=[Bptr], writes=[BTM])
                if STAGE < 3: continue
                P.op("pe", lambda e: e.matmul(pm[0][:, 0:128], KB[:, 0, cc], KR[:, ci, :, :], start=True, stop=True), reads=[BKB, BKR], writes=[Bpmm[0]])
                P.op("pe", lambda e: e.matmul(pm[1][:, 0:128], KB[:, 1, cc], KR[:, ci, :, :], start=True, stop=True), reads=[BKB, BKR], writes=[Bpmm[1]])
                P.op("pe", lambda e: e.matmul(pm[1][:, 128:192], KR[:, ci, 0, :], KB[:, 1, cc], start=False, stop=True, skip_group_check=True), reads=[BKB, BKR], writes=[Bpmm[1]])
                P.op("dve", lambda e: e.tensor_tensor(MA[:, :], pm[0][:, 0:128], msk[:, :], ALU.mult), reads=[Bpmm[0], Bmsk], writes=[BMA])
                P.op("dve", lambda e: e.tensor_tensor(MB[:, 0:64], pm[1][:, 0:64], msk[:, 0:64], ALU.mult), reads=[Bpmm[1], Bmsk], writes=[BMB])
                P.op("dve", lambda e: e.scalar_tensor_tensor(MB[:, 64:128], pm[1][:, 64:128], -1.0, msk[:, 64:128], ALU.mult, ALU.mult), reads=[Bpmm[1], Bmsk], writes=[BMB])
                P.op("pool", lambda e: e.tensor_scalar(Wt[:, :], msk[:, 64:128], -1.0, 1.0, ALU.mult, ALU.add), reads=[Bmsk], writes=[BWt])
                P.op("dve", lambda e: e.tensor_tensor(NT_[:, :], pm[1][:, 128:192], Wt[:, :], ALU.mult), reads=[Bpmm[1], BWt], writes=[BNT])
                if STAGE < 4: continue
                P.op("dve", lambda e: e.tensor_tensor(R[:, :], identf[0:64, 0:64], MB[:, 0:64], ALU.subtract), reads=[Bidf, BMB], writes=[BR])
                P.op("act", lambda e: e.activation(Rb[:, :], R[:, :], AF.Identity), reads=[BR], writes=[BR])
                curX, curXT, BcX, BcXT = MB[:, 0:64], NT_[:, :], BMB, BNT
                for lvl in range(5):
                    k2 = lvl % 2
                    P.op("pe", lambda e, curX=curX, curXT=curXT: e.matmul(pw[:, 0:64], curXT, curX, start=True, stop=True), reads=[BcX, BcXT], writes=[Bpw])
                    P.op("pe", lambda e, curX=curX, curXT=curXT: e.matmul(pw[:, 64:128], curX, curXT, start=False, stop=True, skip_group_check=True), reads=[BcX, BcXT], writes=[Bpw])
                    P.op("dve", lambda e, k2=k2: e.tensor_copy(X[k2][:, :], pw[:, 0:64]), reads=[Bpw], writes=[BX[k2]])
                    P.op("dve", lambda e, k2=k2: e.tensor_copy(XT[k2][:, :], pw[:, 64:128]), reads=[Bpw], writes=[BXT[k2]])
                    P.op("pe", lambda e, k2=k2: e.matmul(pw[:, 128:192], XT[k2][:, :], Rb[:, :], start=False, stop=True, skip_group_check=True), reads=[BXT[k2], BR], writes=[Bpw])
                    P.op("dve", lambda e: e.tensor_tensor(R[:, :], R[:, :], pw[:, 128:192], ALU.add), reads=[BR, Bpw], writes=[BR])
                    P.op("act", lambda e: e.activation(Rb[:, :], R[:, :], AF.Identity), reads=[BR], writes=[BR])
                    curX, curXT, BcX, BcXT = X[k2][:, :], XT[k2][:, :], BX[k2], BXT[k2]
                if STAGE < 5: continue
                P.op("pe", lambda e: e.matmul(pw[:, 192:256], KR[:, ci, 0, :], STb[:, h, :], start=False, stop=False, skip_group_check=True), reads=[BKR, BST[h]], writes=[Bpw])
                P.op("pe", lambda e: e.matmul(pw[:, 192:256], MA[:, 0:64], TM[:, 2, :], start=False, stop=True, skip_group_check=True), reads=[BMA, BTM], writes=[Bpw])
                P.op("dve", lambda e: e.tensor_copy(Wt[:, :], pw[:, 192:256]), reads=[Bpw], writes=[BWt])
                P.op("pe", lambda e: e.matmul(pw[:, 256:320], Rb[:, :], Wt[:, :], start=False, stop=True, skip_group_check=True), reads=[BR, BWt], writes=[Bpw])
                P.op("dve", lambda e: e.tensor_copy(Ut[:, :], pw[:, 256:320]), reads=[Bpw], writes=[BUt])
                P.op("pe", lambda e: e.matmul(pw[:, 320:384], KR[:, ci, 1, :], STb[:, h, :], start=False, stop=False, skip_group_check=True), reads=[BKR, BST[h]], writes=[Bpw])
                P.op("pe", lambda e: e.matmul(pw[:, 320:384], MA[:, 64:128], TM[:, 2, :], start=False, stop=False, skip_group_check=True), reads=[BMA, BTM], writes=[Bpw])
                P.op("pe", lambda e: e.matmul(pw[:, 320:384], MB[:, 64:128], Ut[:, :], start=False, stop=True, skip_group_check=True), reads=[BMB, BUt], writes=[Bpw])
                P.op("act", lambda e: e.activation(ysb[:, ci, h, :], pw[:, 320:384], AF.Identity), reads=[Bpw], writes=[Bysb])
                P.op("pe", lambda e: e.matmul(pw[:, 384:448], TM[:, 0, :], TM[:, 2, :], start=False, stop=False, skip_group_check=True), reads=[BTM], writes=[Bpw])
                P.op("pe", lambda e: e.matmul(pw[:, 384:448], TM[:, 1, :], Ut[:, :], start=False, stop=True, skip_group_check=True), reads=[BTM, BUt], writes=[Bpw])
                P.op("dve", lambda e: e.scalar_tensor_tensor(ST[:, h, :], ST[:, h, :], gC[:, ci:ci + 1], pw[:, 384:448], ALU.mult, ALU.add), reads=[BST[h], BgC, Bpw], writes=[BST[h]])
                P.op("act", lambda e: e.activation(STb[:, h, :], ST[:, h, :], AF.Identity), reads=[BST[h]], writes=[BST[h]])
        for ci in range(ncg if STAGE >= 5 else 0):
            P.dma("sp", y_d[t0 + ci * C:t0 + (ci + 1) * C, :], ysb[:, ci, :, :], ysem, reads=[Bysb], writes=[By])
    P.finish([By, Baux])
    print("phaseC insts", P.ninst)
    return P.close()

def ref_dir(xs6, Wd, nh):
    T = xs6.shape[1]
    r = xs6[0] @ Wd["wr"]; k = xs6[1] @ Wd["wk"]; v = xs6[2] @ Wd["wv"]
    lw = np.tanh(xs6[3] @ Wd["w1"]) @ Wd["w2"]
    z = Wd["w0"] + lw
    w_log = -np.log1p(np.exp(-z)) - 0.5
    decay = np.exp(-np.exp(w_log))
    a = 1 / (1 + np.exp(-(Wd["a0"] + (xs6[4] @ Wd["a1"]) @ Wd["a2"])))
    g = (1 / (1 + np.exp(-(xs6[5] @ Wd["g1"])))) @ Wd["g2"]
    kk = (k * Wd["k_k"]).reshape(T, nh, 64)
    kk = kk / np.maximum(np.sqrt((kk * kk).sum(-1, keepdims=True)), 1e-12)
    kd = k * (1 + (a - 1) * Wd["k_a"])
    rh = r.reshape(T, nh, 64); wh = decay.reshape(T, nh, 64); kdh = kd.reshape(T, nh, 64); vh = v.reshape(T, nh, 64); ah = a.reshape(T, nh, 64)
    S = np.zeros((nh, 64, 64)); ys = np.zeros((T, nh, 64))
    for t in range(T):
        sa = np.einsum('hvk,hk->hv', S, kk[t])
        S = S * wh[t][:, None, :] - sa[:, :, None] * (kk[t] * ah[t])[:, None, :] + vh[t][:, :, None] * kdh[t][:, None, :]
        ys[t] = np.einsum('hvk,hk->hv', S, rh[t])
    return ys.reshape(T, nh * 64), r, kd, v, g


LNX_EPS = 64e-5

def build_phaseD(ntiles, FE=3584, E=8, D=1024, FW=256, PART=8):
    P = Prog(); nc = P.nc
    T = ntiles * 128
    NHh = D // 64
    names = ["yf", "yb", "r", "kdf", "kdb", "v", "g", "h1"]
    ind = {n: P.dram(n, [T, D], F32, "ExternalInput") for n in names}
    wo_d = P.dram("wo", [D, D], F32, "ExternalInput")
    vec_d = P.dram("vecs", [7, D], F32, "ExternalInput")
    wr_d = P.dram("wrt", [E, D], F32, "ExternalInput")
    br_d = P.dram("brt", [1, E], F32, "ExternalInput")
    wg_d = P.dram("wg", [E, D, FE], F32, "ExternalInput")
    wu_d = P.dram("wu", [E, D, FE], F32, "ExternalInput")
    wd_d = P.dram("wd", [E, FE, D], F32, "ExternalInput")
    id_d = P.dram("ident", [128, 128], F32, "ExternalInput")
    out_d = P.dram("out", [T, D], F32, "ExternalOutput")
    Bout_d = Buf("out_d")
    ident = P.sb("ident_sb", [128, 128], BF16); Bid = Buf("ident"); P.dma("pool", ident[:], id_d[:, :], None, writes=[Bid])
    wo, Bwo = load_w_bf16(P, "wo_sb", wo_d, D, D, None)
    vb = []; Bvb = []
    for i in range(7):
        t, B = load_bcast(P, "vec%d" % i, vec_d[i:i + 1, :], D, None); vb.append(t); Bvb.append(B)
    wrb = P.sb("wrb", [128, E, D], F32); Bwrb = Buf("wrb")
    for e_ in range(E):
        P.dma("sp", wrb[:, e_, :], wr_d[e_:e_ + 1, :].to_broadcast([128, D]), None, writes=[Bwrb])
    brb, Bbrb = load_bcast(P, "brb", br_d[0:1, :], E, None)
    halves = [list(range(a, min(ntiles, a + PART))) for a in range(0, ntiles, PART)]
    maxh = max(len(h) for h in halves)
    hT = P.sb("hT", [128, 8, maxh * 128], BF16); BhT = Buf("hT")
    yacc = P.sb("yacc", [128, maxh, D], F32); Byacc = [Buf("yacc%d" % i) for i in range(maxh)]
    gates = P.sb("gates", [128, maxh, E], F32); Bgates = Buf("gates")
    st = P.sb("lnst", [128, 12], F32); mv = P.sb("lnmv", [128, 4], F32); Bscr = Buf("lnscr")
    pt = [P.ps("pt%d" % i, [128, 8, 128], BF16) for i in range(2)]; Bpt = [Buf("pt") for i in range(2)]
    pm = [P.ps("pm%d" % i, [128, 512], F32) for i in range(2)]; Bpm = [Buf("pm") for i in range(2)]
    pg = [P.ps("pg%d" % i, [128, 512], F32) for i in range(2)]; Bpg = [Buf("pg") for i in range(2)]
    pu = [P.ps("pu%d" % i, [128, 512], F32) for i in range(2)]; Bpu = [Buf("pu") for i in range(2)]
    NFC = FW // 128
    outt = P.sb("outt", [128, D], F32); Boutt = Buf("outt"); outsem = P.newsem()
    ptc = 0; wcc = 0; hc = 0
    for half in halves:
        if not half: continue
        with P.scope():
            tin = {n: P.sb("in_" + n, [128, D], F32) for n in names}; Bin = {n: Buf("in_" + n) for n in names}
            s16 = P.sb("s16", [128, 8, NHh], F32); Bs16 = Buf("s16")
            tmp = P.sb("tmp", [128, D], F32); Btmp = Buf("tmp")
            obf = P.sb("obf", [128, D], BF16); Bobf = Buf("obf")
            oT = P.sb("oT", [128, 8, 128], BF16); BoT = Buf("oT")
            hbf = P.sb("hbf", [128, D], BF16); Bhbf = Buf("hbf")
            lg = P.sb("lg", [128, 4, E], F32); Blg = Buf("lg")
            for j, t in enumerate(half):
                for n in names:
                    P.dma("sp", tin[n][:], ind[n][t * 128:(t + 1) * 128, :], None, writes=[Bin[n]])
                y = tin["yf"]; By_ = Bin["yf"]
                v3 = lambda a: a[:, :].rearrange("p (h c) -> p h c", c=64)
                P.op("dve", lambda e: e.tensor_tensor(y[:, :], y[:, :], tin["yb"][:, :], ALU.add), reads=[By_, Bin["yb"]], writes=[By_])
                P.op("dve", lambda e: e.tensor_reduce(s16[:, 0, :], v3(y), AX.X, ALU.add), reads=[By_], writes=[Bs16])
                P.op("pool", lambda e: e.tensor_tensor(tmp[:, :], y[:, :], y[:, :], ALU.mult), reads=[By_], writes=[Btmp])
                P.op("dve", lambda e: e.tensor_reduce(s16[:, 1, :], v3(tmp), AX.X, ALU.add), reads=[Btmp], writes=[Bs16])
                P.op("dve", lambda e: e.tensor_scalar(s16[:, 0, :], s16[:, 0, :], 1.0 / 64, None, ALU.mult), reads=[Bs16], writes=[Bs16])
                P.op("dve", lambda e: e.tensor_tensor(s16[:, 2, :], s16[:, 0, :], s16[:, 0, :], ALU.mult), reads=[Bs16], writes=[Bs16])
                P.op("dve", lambda e: e.scalar_tensor_tensor(s16[:, 3, :], s16[:, 1, :], 1.0 / 64, s16[:, 2, :], ALU.mult, ALU.subtract), reads=[Bs16], writes=[Bs16])
                P.op("act", lambda e: e.activation(s16[:, 4, :], s16[:, 3, :], AF.Ln, bias=LNX_EPS, scale=1.0), reads=[Bs16], writes=[Bs16])
                P.op("act", lambda e: e.activation(s16[:, 5, :], s16[:, 4, :], AF.Exp, scale=-0.5), reads=[Bs16], writes=[Bs16])
                P.op("dve", lambda e: e.tensor_tensor(v3(y), v3(y), s16[:, 0, :].to_broadcast([128, NHh, 64]) if False else s16[:, 0:1, :].rearrange("p o h -> p h o").to_broadcast([128, NHh, 64]), ALU.subtract), reads=[By_, Bs16], writes=[By_])
                P.op("dve", lambda e: e.tensor_tensor(v3(y), v3(y), s16[:, 5:6, :].rearrange("p o h -> p h o").to_broadcast([128, NHh, 64]), ALU.mult), reads=[By_, Bs16], writes=[By_])
                P.op("pool", lambda e: e.tensor_tensor(y[:, :], y[:, :], vb[0][:, :], ALU.mult), reads=[By_, Bvb[0]], writes=[By_])
                P.op("pool", lambda e: e.tensor_tensor(y[:, :], y[:, :], vb[1][:, :], ALU.add), reads=[By_, Bvb[1]], writes=[By_])
                kd = tin["kdf"]
                P.op("dve", lambda e: e.tensor_tensor(kd[:, :], kd[:, :], tin["kdb"][:, :], ALU.add), reads=[Bin["kdf"], Bin["kdb"]], writes=[Bin["kdf"]])
                P.op("pool", lambda e: e.tensor_tensor(kd[:, :], kd[:, :], vb[2][:, :], ALU.mult), reads=[Bin["kdf"], Bvb[2]], writes=[Bin["kdf"]])
                P.op("dve", lambda e: e.tensor_tensor(kd[:, :], kd[:, :], tin["r"][:, :], ALU.mult), reads=[Bin["kdf"], Bin["r"]], writes=[Bin["kdf"]])
                P.op("dve", lambda e: e.tensor_reduce(s16[:, 6, :], v3(kd), AX.X, ALU.add), reads=[Bin["kdf"]], writes=[Bs16])
                P.op("dve", lambda e: e.tensor_tensor(v3(tmp), v3(tin["v"]), s16[:, 6:7, :].rearrange("p o h -> p h o").to_broadcast([128, NHh, 64]), ALU.mult), reads=[Bin["v"], Bs16], writes=[Btmp])
                P.op("dve", lambda e: e.tensor_tensor(y[:, :], y[:, :], tmp[:, :], ALU.add), reads=[By_, Btmp], writes=[By_])
                P.op("dve", lambda e: e.tensor_tensor(obf[:, :], y[:, :], tin["g"][:, :], ALU.mult), reads=[By_, Bin["g"]], writes=[Bobf])
                transpose_tile(P, obf, Bobf, ident, Bid, pt[ptc % 2], Bpt[ptc % 2], oT, BoT, 0); ptc += 1
                res = tin["h1"]; Bres = Bin["h1"]
                for nh in range(2):
                    for c in range(8):
                        P.op("pe", lambda e, c=c, nh=nh: e.matmul(pm[nh][:, :], oT[:, c, :], wo[:, c, nh * 512:(nh + 1) * 512], start=(c == 0), stop=(c == 7)), reads=[BoT, Bwo], writes=[Bpm[nh]])
                    P.op("dve", lambda e, nh=nh: e.scalar_tensor_tensor(res[:, nh * 512:(nh + 1) * 512], res[:, nh * 512:(nh + 1) * 512], ALPHA, pm[nh][:, :], ALU.mult, ALU.add), reads=[Bres, Bpm[nh]], writes=[Bres])
                layer_norm(P, res, Bres, vb[3], Bvb[3], vb[4], Bvb[4], (st, mv), Bscr, res, Bres, hbf, Bhbf)
                transpose_tile(P, hbf, Bhbf, ident, Bid, pt[ptc % 2], Bpt[ptc % 2], hT, BhT, j); ptc += 1
                P.op("act", lambda e, j=j: e.activation(yacc[:, j, :], res[:, :], AF.Identity, scale=ALPHA), reads=[Bres], writes=[Byacc[j]])
                for e_ in range(E):
                    P.op("pool" if e_ % 2 else "dve", lambda e, e_=e_: e.tensor_tensor(tmp[:, :], res[:, :], wrb[:, e_, :], ALU.mult), reads=[Bres, Bwrb], writes=[Btmp])
                    P.op("dve", lambda e, e_=e_: e.reduce_sum(lg[:, 0, e_:e_ + 1], tmp[:, :], AX.X), reads=[Btmp], writes=[Blg])
                P.op("dve", lambda e: e.tensor_tensor(lg[:, 0, :], lg[:, 0, :], brb[:, :], ALU.add), reads=[Blg, Bbrb], writes=[Blg])
                P.op("dve", lambda e: e.reduce_max(lg[:, 1, 0:1], lg[:, 0, :], AX.X), reads=[Blg], writes=[Blg])
                P.op("dve", lambda e: e.tensor_scalar(lg[:, 2, :], lg[:, 0, :], lg[:, 1, 0:1], -1e30, ALU.is_equal, ALU.mult), reads=[Blg], writes=[Blg])
                P.op("dve", lambda e: e.tensor_tensor(lg[:, 2, :], lg[:, 2, :], lg[:, 0, :], ALU.add), reads=[Blg], writes=[Blg])
                P.op("dve", lambda e: e.reduce_max(lg[:, 1, 1:2], lg[:, 2, :], AX.X), reads=[Blg], writes=[Blg])
                P.op("dve", lambda e: e.tensor_scalar(lg[:, 2, :], lg[:, 0, :], lg[:, 1, 1:2], None, ALU.is_ge), reads=[Blg], writes=[Blg])
                P.op("dve", lambda e: e.tensor_scalar(lg[:, 3, :], lg[:, 0, :], lg[:, 1, 0:1], None, ALU.subtract), reads=[Blg], writes=[Blg])
                P.op("act", lambda e: e.activation(lg[:, 3, :], lg[:, 3, :], AF.Exp), reads=[Blg], writes=[Blg])
                P.op("dve", lambda e: e.tensor_tensor(lg[:, 3, :], lg[:, 3, :], lg[:, 2, :], ALU.mult), reads=[Blg], writes=[Blg])
                P.op("dve", lambda e: e.reduce_sum(lg[:, 1, 2:3], lg[:, 3, :], AX.X), reads=[Blg], writes=[Blg])
                P.op("dve", lambda e: e.reciprocal(lg[:, 1, 3:4], lg[:, 1, 2:3]), reads=[Blg], writes=[Blg])
                P.op("dve", lambda e, j=j: e.tensor_scalar(gates[:, j, :], lg[:, 3, :], lg[:, 1, 3:4], None, ALU.mult), reads=[Blg], writes=[Bgates])
        P.barrier()
        nT = len(half) * 128
        _sc = P.scope(); _sc.__enter__()
        wgc = [P.sb("wgc%d" % i, [128, 8, FW], BF16) for i in range(2)]; wuc = [P.sb("wuc%d" % i, [128, 8, FW], BF16) for i in range(2)]
        wdc = [P.sb("wdc%d" % i, [128, NFC, D], BF16) for i in range(2)]
        Bwc = [Buf("wc%d" % i) for i in range(2)]
        hid = [P.sb("hid%d" % i, [128, NFC, 512], BF16) for i in range(2)]; Bhid = [Buf("hid") for i in range(2)]
        sg = [P.sb("sg%d" % i, [128, 512], F32) for i in range(2)]; Bsg = [Buf("sg") for i in range(2)]
        tgroups = [(a, min(512, nT - a)) for a in range(0, nT, 512)]
        for e_ in range(E):
            for fg in range(FE // FW):
                k = wcc % 2; wcc += 1
                f0 = fg * FW
                for c in range(8):
                    P.dma("pool", wgc[k][:, c, :], wg_d[e_, c * 128:(c + 1) * 128, f0:f0 + FW], None, writes=[Bwc[k]])
                    P.dma("pool", wuc[k][:, c, :], wu_d[e_, c * 128:(c + 1) * 128, f0:f0 + FW], None, writes=[Bwc[k]])
                for fc in range(NFC):
                    P.dma("pool", wdc[k][:, fc, :], wd_d[e_, f0 + fc * 128:f0 + (fc + 1) * 128, :], None, writes=[Bwc[k]])
                for (a, w) in tgroups:
                    hk = hc % 2; hc += 1
                    for fc in range(NFC):
                        for c in range(8):
                            P.op("pe", lambda e, c=c, fc=fc: e.matmul(pg[hk][:, 0:w], wgc[k][:, c, fc * 128:(fc + 1) * 128], hT[:, c, a:a + w], start=(c == 0), stop=(c == 7)), reads=[Bwc[k], BhT], writes=[Bpg[hk]])
                        for c in range(8):
                            P.op("pe", lambda e, c=c, fc=fc: e.matmul(pu[hk][:, 0:w], wuc[k][:, c, fc * 128:(fc + 1) * 128], hT[:, c, a:a + w], start=(c == 0), stop=(c == 7)), reads=[Bwc[k], BhT], writes=[Bpu[hk]])
                        P.op("act", lambda e: e.activation(sg[hk][:, 0:w], pg[hk][:, 0:w], AF.Silu), reads=[Bpg[hk]], writes=[Bsg[hk]])
                        P.op("dve", lambda e, fc=fc: e.tensor_tensor(hid[hk][:, fc, 0:w], sg[hk][:, 0:w], pu[hk][:, 0:w], ALU.mult), reads=[Bsg[hk], Bpu[hk]], writes=[Bhid[hk]])
                    for jj in range(w // 128):
                        j = a // 128 + jj
                        for nh in range(2):
                            for fc in range(NFC):
                                P.op("pe", lambda e, fc=fc, nh=nh, jj=jj: e.matmul(pm[nh][:, :], hid[hk][:, fc, jj * 128:(jj + 1) * 128], wdc[k][:, fc, nh * 512:(nh + 1) * 512], start=(fc == 0), stop=(fc == NFC - 1)), reads=[Bhid[hk], Bwc[k]], writes=[Bpm[nh]])
                            P.op("dve" if nh else "pool", lambda e, nh=nh, j=j: e.scalar_tensor_tensor(yacc[:, j, nh * 512:(nh + 1) * 512], pm[nh][:, :], gates[:, j, e_:e_ + 1], yacc[:, j, nh * 512:(nh + 1) * 512], ALU.mult, ALU.add) if nh else e.tensor_copy(sg[hk][:, :], sg[hk][:, :]), reads=[Bpm[nh], Bgates, Byacc[j]], writes=[Byacc[j]]) if False else \
                            P.op("dve", lambda e, nh=nh, j=j: e.scalar_tensor_tensor(yacc[:, j, nh * 512:(nh + 1) * 512], pm[nh][:, :], gates[:, j, e_:e_ + 1], yacc[:, j, nh * 512:(nh + 1) * 512], ALU.mult, ALU.add), reads=[Bpm[nh], Bgates, Byacc[j]], writes=[Byacc[j]])
        P.barrier()
        _sc.__exit__(None, None, None)
        for j, t in enumerate(half):
            layer_norm(P, yacc[:, j, :], Byacc[j], vb[5], Bvb[5], vb[6], Bvb[6], (st, mv), Bscr, outt, Boutt)
            P.dma("sp", out_d[t * 128:(t + 1) * 128, :], outt[:], outsem, reads=[Boutt], writes=[Bout_d])
        P.barrier()
    P.finish([Bout_d])
    print("phaseD insts", P.ninst)
    return P.close()


_CACHE = {}

def _run(nc, maps):
    res = run_bass_kernel_spmd(nc, maps, core_ids=list(range(8)))
    return res.results

def kernel(**inp):
    f32 = np.float32
    g = lambda k: np.asarray(inp[k], dtype=f32)
    x = g("x"); meta = g("meta")
    Bn, S, D = x.shape
    NM = meta.shape[0]; L = S + NM
    ident = np.eye(128, dtype=f32)
    nxt = S // 128
    w_in = g("attn_w_in")[0]
    lamv = np.stack([g("attn_lam_q1")[0], g("attn_lam_k1")[0], g("attn_lam_q2")[0], g("attn_lam_k2")[0]])
    subg = g("attn_subln_g")[0][None, :]
    ncA, mults = build_phaseA(2, nxt)
    maps = []
    for c in range(8):
        b = c // 4; heads = [2 * (c % 4), 2 * (c % 4) + 1]
        xp = np.concatenate([x[b], meta, np.zeros((128 - NM, D), f32)], 0)
        qaug, kaug = attn_consts(heads, nxt)
        tab = np.zeros((2, 1, 1024), f32)
        for hi, h in enumerate(heads):
            sl = 2.0 ** (-(h + 1))
            for m, k in mults[hi].items():
                tab[hi, 0, k] = sl * m
        hw = lambda off: np.ascontiguousarray(np.stack([w_in[:, off + h * 128: off + (h + 1) * 128] for h in heads]))
        maps.append(dict(xp=xp, wq=hw(0), wk=hw(D), wv=hw(2 * D), lamv=lamv, subg=subg, qaug=qaug, kaug=kaug, ctab=tab, ident=ident))
    resA = _run(ncA, maps)
    o_full = np.zeros((Bn, (nxt + 1) * 128, D), f32)
    for c in range(8):
        b = c // 4; h0 = 2 * (c % 4)
        o_full[b][:, h0 * 128:(h0 + 2) * 128] = resA[c]["o"]
    del resA, maps
    TQ = S // 4; MQ = NM // 4
    ntB = TQ // 128 + 1
    ncB = build_phaseB(ntB)
    maps = []
    for c in range(8):
        b = c // 4; q = c % 4
        o_rows = np.zeros((ntB * 128, D), f32); h_rows = np.zeros((ntB * 128, D), f32)
        o_rows[:TQ] = o_full[b][q * TQ:(q + 1) * TQ]; o_rows[TQ:TQ + MQ] = o_full[b][S + q * MQ:S + (q + 1) * MQ]
        h_rows[:TQ] = x[b][q * TQ:(q + 1) * TQ]; h_rows[TQ:TQ + MQ] = meta[q * MQ:(q + 1) * MQ]
        maps.append(dict(o=o_rows, h0=h_rows, wo=g("attn_w_o")[0], wg=g("ffn_w_gate")[0], wu=g("ffn_w_up")[0], wd=g("ffn_w_down")[0],
                         lng=g("ln_g")[0], lnb=g("ln_b")[0], ident=ident))
    resB = _run(ncB, maps)
    seq = np.zeros((Bn, L, D), f32)
    for c in range(8):
        b = c // 4; q = c % 4
        seq[b][NM + q * TQ:NM + (q + 1) * TQ] = resB[c]["h1"][:TQ]
        seq[b][q * MQ:(q + 1) * MQ] = resB[c]["h1"][TQ:TQ + MQ]
    del resB, maps, o_full
    nch = (L + 63) // 64; Tp = nch * 64
    ncC = build_phaseC(nch, 8)
    mu = g("rw_mu")[0]
    seg = np.ones((1, 512), f32); seg[0, ::64] = 0
    maps = []
    for c in range(8):
        b = c // 4; d = (c % 4) // 2; hg = c % 2
        cs = slice(hg * 512, (hg + 1) * 512)
        sq = seq[b] if d == 0 else seq[b][::-1]
        xpad = np.zeros((Tp + 2, D), f32); xpad[1:1 + L] = sq
        mu_in = np.concatenate([mu[d], mu[1 - d]], 0)
        chv = np.zeros((64, 8, 8), f32)
        for i, v in enumerate((g("rw_w0")[0][d], g("rw_a0")[0][d], g("rw_k_k")[0], g("rw_k_a")[0])):
            chv[:, :, i] = v[cs].reshape(8, 64).T
        wrkv = g("rw_w_rkv")[0]
        maps.append(dict(xT=np.ascontiguousarray(xpad.T), mu=np.ascontiguousarray(mu_in.T),
                         wr=np.ascontiguousarray(wrkv[0][:, cs]), wk=np.ascontiguousarray(wrkv[1][:, cs]), wv=np.ascontiguousarray(wrkv[2][:, cs]),
                         w1=g("rw_w1")[0][d], w2=np.ascontiguousarray(g("rw_w2")[0][d][:, cs]),
                         a1=g("rw_a1")[0][d], a2=np.ascontiguousarray(g("rw_a2")[0][d][:, cs]),
                         g1=g("rw_g1")[0], g2=np.ascontiguousarray(g("rw_g2")[0][:, cs]),
                         chv=chv, msk=rw_consts(), seg=seg, ident=ident))
    resC = _run(ncC, maps)
    Y = np.zeros((Bn, 2, L, D), f32); AUX = np.zeros((Bn, 2, 4, L, D), f32)
    for c in range(8):
        b = c // 4; d = (c % 4) // 2; hg = c % 2
        cs = slice(hg * 512, (hg + 1) * 512)
        yy = resC[c]["y"][:L]; ax = resC[c]["aux"][:, :, :L]
        if d == 1:
            yy = yy[::-1]; ax = ax[:, :, ::-1]
        Y[b, d][:, cs] = yy
        for i in range(4):
            AUX[b, d, i][:, cs] = ax[i].T
    del resC, maps
    ncD = build_phaseD(TQ // 128)
    vecs = np.stack([g("rw_lnx_g")[0], g("rw_lnx_b")[0], g("rw_r_k")[0].reshape(D), g("ln_g")[1, 0], g("ln_b")[1, 0], g("ln_g")[1, 1], g("ln_b")[1, 1]])
    wrt = np.ascontiguousarray(g("moe_w_router")[0].T); brt = g("moe_b_router")[0][None, :]
    wgE = g("moe_w_gate")[0]; wuE = g("moe_w_up")[0]; wdE = g("moe_w_down")[0]; woR = g("rw_w_o")[0]
    maps = []
    for c in range(8):
        b = c // 4; q = c % 4
        rows = slice(NM + q * TQ, NM + (q + 1) * TQ)
        cp = lambda a: np.ascontiguousarray(a[rows])
        maps.append(dict(yf=cp(Y[b, 0]), yb=cp(Y[b, 1]), r=cp(AUX[b, 0, 0]), kdf=cp(AUX[b, 0, 1]), kdb=cp(AUX[b, 1, 1]), v=cp(AUX[b, 0, 2]), g=cp(AUX[b, 0, 3]),
                         h1=cp(seq[b]), wo=woR, vecs=vecs, wrt=wrt, brt=brt, wg=wgE, wu=wuE, wd=wdE, ident=ident))
    resD = _run(ncD, maps)
    out = np.zeros((Bn, S, D), f32)
    for c in range(8):
        b = c // 4; q = c % 4
        out[b, q * TQ:(q + 1) * TQ] = resD[c]["out"]
    return out
```

```python
import numpy as np
from contextlib import ExitStack
import concourse.bass as bass
import concourse.mybir as mybir
from concourse.bass_utils import run_bass_kernel_spmd

F32 = mybir.dt.float32
BF16 = mybir.dt.bfloat16
ALU = mybir.AluOpType
AF = mybir.ActivationFunctionType
AX = mybir.AxisListType


NUM_DEVICES = None


class Sem:
    def __init__(self, h, is_dma=False):
        self.h = h
        self.cnt = 0
        self.is_dma = is_dma


class Buf:
    def __init__(self, name):
        self.name = name
        self.w = None
        self.r = []


class Prog:
    ENG = ("pe", "act", "dve", "pool", "sp")

    def __init__(self):
        self.nc = bass.Bass("TRN2", target_bir_lowering=False, num_devices=NUM_DEVICES)
        self.es = ExitStack()
        nc = self.nc
        self.eng = {"pe": nc.tensor, "act": nc.scalar, "dve": nc.vector, "pool": nc.gpsimd, "sp": nc.sync}
        self.esem = {e: Sem(self.es.enter_context(nc.semaphore("sem_" + e))) for e in self.ENG}
        self.waited = {e: {} for e in self.ENG}
        self.nsem = 0
        self.dsems = []
        self.root_es = self.es
        self.ninst = 0

    def dram(self, name, shape, dt, kind):
        return self.nc.dram_tensor(name, list(shape), dt, kind=kind).ap()

    def _uniq(self, name):
        self.nalloc = getattr(self, "nalloc", 0) + 1
        return "%s_u%d" % (name, self.nalloc)

    def sb(self, name, shape, dt):
        return self.es.enter_context(self.nc.sbuf_tensor(self._uniq(name), list(shape), dt))

    def ps(self, name, shape, dt):
        return self.es.enter_context(self.nc.psum_tensor(self._uniq(name), list(shape), dt))

    def newsem(self, name=None):
        self.nsem += 1
        sm = self._mksem(name)
        sm.is_dma = True
        self.dsems.append(sm)
        return sm

    def _mksem(self, name):
        return Sem(self.root_es.enter_context(self.nc.semaphore(name or ("ds%d" % self.nsem))))

    def _wait(self, e, ev, raw=True):
        sem, val = ev
        if sem is self.esem[e] and (e == "pe" or not raw):
            return
        if sem.is_dma:
            val = sem.cnt
        if self.waited[e].get(sem, 0) >= val:
            return
        self.waited[e][sem] = val
        self.eng[e].wait_ge(sem.h, val)

    def _deps(self, e, reads, writes):
        for b in reads:
            if b.w is not None:
                self._wait(e, b.w)
        for b in writes:
            if b.w is not None:
                self._wait(e, b.w, raw=False)
            for ev in b.r:
                self._wait(e, ev, raw=False)

    def _commit(self, ev, reads, writes):
        for b in reads:
            b.r.append(ev)
            if len(b.r) > 64:
                b.r = b.r[-64:]
        for b in writes:
            b.w = ev
            b.r = []

    def op(self, e, fn, reads=(), writes=(), accum=False):
        self._deps(e, reads, writes)
        s = self.esem[e]
        inst = fn(self.eng[e])
        s.cnt += 1
        inst.then_inc(s.h, 1)
        self._commit((s, s.cnt), reads, writes)
        self.ninst += 1
        return inst

    def dma(self, q, out, in_, sem, reads=(), writes=(), **kw):
        if sem is None:
            b0 = writes[0]
            if getattr(b0, "dsem", None) is None:
                b0.dsem = self.newsem()
            sem = b0.dsem
        self._deps(q, reads, writes)
        inst = self.eng[q].dma_start(out=out, in_=in_, **kw)
        sem.cnt += 16
        inst.then_inc(sem.h, 16)
        self._commit((sem, sem.cnt), reads, writes)
        self.ninst += 1
        return inst

    def barrier(self):
        evs = [(s, s.cnt) for s in list(self.esem.values()) + self.dsems if s.cnt > 0]
        for e in self.ENG:
            for ev in evs:
                self._wait(e, ev)

    def scope(self):
        prog = self
        class _S:
            def __enter__(s2):
                s2.old = prog.es
                prog.es = ExitStack()
                return prog
            def __exit__(s2, *a):
                prog.es.close()
                prog.es = s2.old
                return False
        return _S()

    def finish(self, bufs, e="sp"):
        for b in bufs:
            if b.w is not None:
                self._wait(e, b.w)
            for ev in b.r:
                self._wait(e, ev)

    def close(self):
        self.es.close()
        return self.nc

import math
ALPHA = 4.0 ** 0.25
LN_EPS = 1e-5

class Common:
    def __init__(self, P):
        self.P = P

def load_w_bf16(P, name, w_ap, K, N, sem):
    kc = K // 128
    t = P.sb(name, [128, kc, N], BF16)
    B = Buf(name)
    for c in range(kc):
        P.dma("pool", t[:, c, :], w_ap[c * 128:(c + 1) * 128, :], sem, writes=[B])
    return t, B

def load_bcast(P, name, v_ap, N, sem):
    t = P.sb(name, [128, N], F32)
    B = Buf(name)
    P.dma("sp", t[:], v_ap.to_broadcast([128, N]), sem, writes=[B])
    return t, B

def layer_norm(P, x, Bx, g, Bg, b, Bb, scr, Bscr, out, Bout, obf=None, Bobf=None, D=1024):
    nh = D // 512
    st, mv = scr
    for i in range(nh):
        P.op("dve", lambda e, i=i: e.bn_stats(st[:, i * 6:(i + 1) * 6], x[:, i * 512:(i + 1) * 512]), reads=[Bx], writes=[Bscr])
    P.op("dve", lambda e: e.bn_aggr(mv[:, 0:2], st[:, 0:6 * nh]), reads=[Bscr], writes=[Bscr])
    P.op("act", lambda e: e.activation(mv[:, 2:3], mv[:, 1:2], AF.Ln, bias=LN_EPS, scale=1.0), reads=[Bscr], writes=[Bscr])
    P.op("act", lambda e: e.activation(mv[:, 3:4], mv[:, 2:3], AF.Exp, scale=-0.5), reads=[Bscr], writes=[Bscr])
    P.op("dve", lambda e: e.tensor_scalar(out[:, :], x[:, :], mv[:, 0:1], mv[:, 3:4], ALU.subtract, ALU.mult), reads=[Bx, Bscr], writes=[Bout])
    P.op("pool", lambda e: e.tensor_tensor(out[:, :], out[:, :], g[:, :], ALU.mult), reads=[Bout, Bg], writes=[Bout])
    P.op("pool", lambda e: e.tensor_tensor(out[:, :], out[:, :], b[:, :], ALU.add), reads=[Bout, Bb], writes=[Bout])
    if obf is not None:
        P.op("act", lambda e: e.activation(obf[:, :], out[:, :], AF.Identity), reads=[Bout], writes=[Bobf])

def transpose_tile(P, src_bf, Bsrc, ident, Bid, pt, Bpt, dst, Bdst, tslot, nchunk=8, evac="act"):
    for c in range(nchunk):
        P.op("pe", lambda e, c=c: e.transpose(pt[:, c, :], src_bf[:, c * 128:(c + 1) * 128], ident[:, :]), reads=[Bsrc, Bid], writes=[Bpt])
    if evac == "act":
        P.op("act", lambda e: e.activation(dst[:, 0:nchunk, tslot * 128:(tslot + 1) * 128], pt[:, 0:nchunk, :], AF.Identity), reads=[Bpt], writes=[Bdst])
    else:
        P.op("dve", lambda e: e.tensor_copy(dst[:, 0:nchunk, tslot * 128:(tslot + 1) * 128], pt[:, 0:nchunk, :]), reads=[Bpt], writes=[Bdst])

def build_phaseB(ntiles, F=2816, D=1024):
    P = Prog(); nc = P.nc
    T = ntiles * 128
    FC = F // 128
    o_d = P.dram("o", [T, D], F32, "ExternalInput")
    h0_d = P.dram("h0", [T, D], F32, "ExternalInput")
    wo_d = P.dram("wo", [D, D], F32, "ExternalInput")
    wg_d = P.dram("wg", [D, F], F32, "ExternalInput")
    wu_d = P.dram("wu", [D, F], F32, "ExternalInput")
    wd_d = P.dram("wd", [F, D], F32, "ExternalInput")
    lng_d = P.dram("lng", [2, D], F32, "ExternalInput")
    lnb_d = P.dram("lnb", [2, D], F32, "ExternalInput")
    id_d = P.dram("ident", [128, 128], F32, "ExternalInput")
    out_d = P.dram("h1", [T, D], F32, "ExternalOutput")
    wsem = P.newsem("wsem")
    wo, Bwo = load_w_bf16(P, "wo_sb", wo_d, D, D, wsem)
    wg, Bwg = load_w_bf16(P, "wg_sb", wg_d, D, F, wsem)
    wu, Bwu = load_w_bf16(P, "wu_sb", wu_d, D, F, wsem)
    wd, Bwd = load_w_bf16(P, "wd_sb", wd_d, F, D, wsem)
    csem = P.newsem("csem")
    g1, Bg1 = load_bcast(P, "g1", lng_d[0:1, :], D, csem)
    b1, Bb1 = load_bcast(P, "b1", lnb_d[0:1, :], D, csem)
    g2, Bg2 = load_bcast(P, "g2", lng_d[1:2, :], D, csem)
    b2, Bb2 = load_bcast(P, "b2", lnb_d[1:2, :], D, csem)
    ident = P.sb("ident_sb", [128, 128], BF16); Bid = Buf("ident")
    P.dma("pool", ident[:], id_d[:, :], csem, writes=[Bid])
    GT = 2
    W = GT * 128
    o32 = P.sb("o32", [128, D], F32); Bo32 = Buf("o32"); o32sem = P.newsem()
    obf = P.sb("obf", [128, D], BF16); Bobf = Buf("obf")
    res = [P.sb("res%d" % i, [128, D], F32) for i in range(GT)]; Bres = [Buf("res%d" % i) for i in range(GT)]
    ressem = [P.newsem() for i in range(GT)]
    hbf = P.sb("hbf", [128, D], BF16); Bhbf = Buf("hbf")
    xT = P.sb("xT", [128, 8, W], BF16); BxT = Buf("xT")
    hidT = P.sb("hidT", [128, FC, W], BF16); BhidT = Buf("hidT")
    sg = [P.sb("sg%d" % i, [128, W], F32) for i in range(2)]; Bsg = [Buf("sg%d" % i) for i in range(2)]
    st = P.sb("lnst", [128, 12], F32); mv = P.sb("lnmv", [128, 4], F32); Bscr = Buf("lnscr")
    outt = P.sb("outt", [128, D], F32); Boutt = Buf("outt"); outsem = P.newsem()
    pt = [P.ps("pt%d" % i, [128, 8, 128], BF16) for i in range(2)]; Bpt = [Buf("pt%d" % i) for i in range(2)]
    pm = [P.ps("pm%d" % i, [128, 512], F32) for i in range(2)]; Bpm = [Buf("pm%d" % i) for i in range(2)]
    pg = [P.ps("pg%d" % i, [128, 512], F32) for i in range(2)]; Bpg = [Buf("pg%d" % i) for i in range(2)]
    pu = [P.ps("pu%d" % i, [128, 512], F32) for i in range(2)]; Bpu = [Buf("pu%d" % i) for i in range(2)]
    Bout_d = Buf("out_d")
    ptc = 0
    ngroups = (ntiles + GT - 1) // GT
    for gi in range(ngroups):
        tiles = list(range(gi * GT, min(ntiles, (gi + 1) * GT)))
        w = len(tiles) * 128
        for j, t in enumerate(tiles):
            P.dma("sp", o32[:], o_d[t * 128:(t + 1) * 128, :], o32sem, writes=[Bo32])
            P.dma("sp", res[j][:], h0_d[t * 128:(t + 1) * 128, :], ressem[j], writes=[Bres[j]])
            P.op("act", lambda e: e.activation(obf[:, :], o32[:, :], AF.Identity), reads=[Bo32], writes=[Bobf])
            transpose_tile(P, obf, Bobf, ident, Bid, pt[ptc % 2], Bpt[ptc % 2], xT, BxT, j); ptc += 1
        for j, t in enumerate(tiles):
            for nh in range(2):
                for c in range(8):
                    P.op("pe", lambda e, c=c, nh=nh, j=j: e.matmul(pm[nh][:, :], xT[:, c, j * 128:(j + 1) * 128], wo[:, c, nh * 512:(nh + 1) * 512], start=(c == 0), stop=(c == 7)),
                         reads=[BxT, Bwo], writes=[Bpm[nh]])
                P.op("dve", lambda e, nh=nh, j=j: e.scalar_tensor_tensor(res[j][:, nh * 512:(nh + 1) * 512], res[j][:, nh * 512:(nh + 1) * 512], ALPHA, pm[nh][:, :], ALU.mult, ALU.add),
                     reads=[Bres[j], Bpm[nh]], writes=[Bres[j]])
            layer_norm(P, res[j], Bres[j], g1, Bg1, b1, Bb1, (st, mv), Bscr, res[j], Bres[j], hbf, Bhbf)
            transpose_tile(P, hbf, Bhbf, ident, Bid, pt[ptc % 2], Bpt[ptc % 2], xT, BxT, j); ptc += 1
        for fc in range(FC):
            k = fc % 2
            for c in range(8):
                P.op("pe", lambda e, c=c, fc=fc, k=k: e.matmul(pg[k][:, 0:w], wg[:, c, fc * 128:(fc + 1) * 128], xT[:, c, 0:w], start=(c == 0), stop=(c == 7)),
                     reads=[BxT, Bwg], writes=[Bpg[k]])
            for c in range(8):
                P.op("pe", lambda e, c=c, fc=fc, k=k: e.matmul(pu[k][:, 0:w], wu[:, c, fc * 128:(fc + 1) * 128], xT[:, c, 0:w], start=(c == 0), stop=(c == 7)),
                     reads=[BxT, Bwu], writes=[Bpu[k]])
            P.op("act", lambda e, k=k: e.activation(sg[k][:, 0:w], pg[k][:, 0:w], AF.Silu), reads=[Bpg[k]], writes=[Bsg[k]])
            P.op("dve", lambda e, k=k, fc=fc: e.tensor_tensor(hidT[:, fc, 0:w], sg[k][:, 0:w], pu[k][:, 0:w], ALU.mult), reads=[Bsg[k], Bpu[k]], writes=[BhidT])
        for j, t in enumerate(tiles):
            for nh in range(2):
                for fc in range(FC):
                    P.op("pe", lambda e, fc=fc, nh=nh, j=j: e.matmul(pm[nh][:, :], hidT[:, fc, j * 128:(j + 1) * 128], wd[:, fc, nh * 512:(nh + 1) * 512], start=(fc == 0), stop=(fc == FC - 1)),
                         reads=[BhidT, Bwd], writes=[Bpm[nh]])
                P.op("dve", lambda e, nh=nh, j=j: e.scalar_tensor_tensor(res[j][:, nh * 512:(nh + 1) * 512], res[j][:, nh * 512:(nh + 1) * 512], ALPHA, pm[nh][:, :], ALU.mult, ALU.add),
                     reads=[Bres[j], Bpm[nh]], writes=[Bres[j]])
            layer_norm(P, res[j], Bres[j], g2, Bg2, b2, Bb2, (st, mv), Bscr, outt, Boutt)
            P.dma("sp", out_d[t * 128:(t + 1) * 128, :], outt[:], outsem, reads=[Boutt], writes=[Bout_d])
    P.finish([Bout_d])
    print("phaseB insts", P.ninst)
    return P.close()


import math
SUBLN_EPS = 1e-5
NEG = -30000.0

def attn_consts(heads, nxt):
    T = (nxt + 1) * 128
    qaug = np.zeros((2, 5, 512), np.float32)
    u = np.arange(512)
    qaug[0] = np.stack([u // 16, u % 16, np.ones(512), np.ones(512), np.ones(512)])
    qaug[1] = np.stack([-(u // 16), -(u % 16), -np.ones(512), -np.ones(512), np.ones(512)])
    kaug = np.zeros((len(heads), 5, T), np.float32)
    v = np.arange(T) % 128
    for i, h in enumerate(heads):
        sl = 2.0 ** (-(h + 1))
        kaug[i, 0] = -16 * sl
        kaug[i, 1] = -sl
        kaug[i, 2] = 16 * sl * (v // 16)
        kaug[i, 3] = sl * (v % 16)
        kaug[i, 4, nxt * 128 + 16:] = NEG
    return qaug, kaug

def build_phaseA(NH, nxt, NCT=1024, D=1024):
    P = Prog(); nc = P.nc
    NT = nxt + 1
    T = NT * 128
    x_d = P.dram("xp", [T, D], F32, "ExternalInput")
    wq_d = P.dram("wq", [NH, D, 128], F32, "ExternalInput")
    wk_d = P.dram("wk", [NH, D, 128], F32, "ExternalInput")
    wv_d = P.dram("wv", [NH, D, 128], F32, "ExternalInput")
    lam_d = P.dram("lamv", [4, 64], F32, "ExternalInput")
    sg_d = P.dram("subg", [1, 128], F32, "ExternalInput")
    qaug_d = P.dram("qaug", [2, 5, 512], F32, "ExternalInput")
    kaug_d = P.dram("kaug", [NH, 5, T], F32, "ExternalInput")
    ctab_d = P.dram("ctab", [NH, 1, NCT], F32, "ExternalInput")
    id_d = P.dram("ident", [128, 128], F32, "ExternalInput")
    o_d = P.dram("o", [T, NH * 128], F32, "ExternalOutput")
    Bout_d = Buf("o_d")
    csem = None
    ident = P.sb("ident_sb", [128, 128], BF16); Bid = Buf("ident")
    P.dma("pool", ident[:], id_d[:, :], csem, writes=[Bid])
    lv = P.sb("lv", [128, 4, 64], F32); Blv = Buf("lv")
    for i in range(4):
        P.dma("sp", lv[:, i, :], lam_d[i:i + 1, :].to_broadcast([128, 64]), csem, writes=[Blv])
    lsc = P.sb("lsc", [128, 8], F32); Blsc = Buf("lsc")
    lt = P.sb("lt", [128, 2, 64], F32)
    P.op("dve", lambda e: e.tensor_tensor(lt[:, 0, :], lv[:, 0, :], lv[:, 1, :], ALU.mult), reads=[Blv], writes=[Blsc])
    P.op("dve", lambda e: e.tensor_tensor(lt[:, 1, :], lv[:, 2, :], lv[:, 3, :], ALU.mult), reads=[Blv], writes=[Blsc])
    P.op("dve", lambda e: e.reduce_sum(lsc[:, 0:1], lt[:, 0, :], AX.X), reads=[Blsc], writes=[Blsc])
    P.op("dve", lambda e: e.reduce_sum(lsc[:, 1:2], lt[:, 1, :], AX.X), reads=[Blsc], writes=[Blsc])
    P.op("act", lambda e: e.activation(lsc[:, 2:4], lsc[:, 0:2], AF.Exp), reads=[Blsc], writes=[Blsc])
    P.op("dve", lambda e: e.tensor_tensor(lsc[:, 4:5], lsc[:, 3:4], lsc[:, 2:3], ALU.subtract), reads=[Blsc], writes=[Blsc])
    P.op("dve", lambda e: e.tensor_scalar(lsc[:, 5:6], lsc[:, 4:5], -0.2, None, ALU.add), reads=[Blsc], writes=[Blsc])
    neglam = lsc[:, 5:6]
    gsc, Bgsc = load_bcast(P, "gsc", sg_d[0:1, :], 128, csem)
    P.op("dve", lambda e: e.tensor_scalar(gsc[:, :], gsc[:, :], 0.8, None, ALU.mult), reads=[Bgsc], writes=[Bgsc])
    QT = P.sb("QT", [128, T], BF16); BQT = Buf("QT")
    KT = [P.sb("KT%d" % c, [69, T], BF16) for c in range(2)]; BKT = [Buf("KT%d" % c) for c in range(2)]
    VA = P.sb("VA", [128, NT, 129], BF16); BVA = Buf("VA")
    P.op("pool", lambda e: e.memset(VA[:, :, 128:129], 1.0), writes=[BVA])
    ctab = P.sb("ctab_sb", [128, NCT], F32); Bctab = Buf("ctab")
    QA = [[P.sb("QA%d%d" % (c, lr), [69, 512], BF16) for lr in range(2)] for c in range(2)]
    BQA = [[Buf("QA%d%d" % (c, lr)) for lr in range(2)] for c in range(2)]
    for c in range(2):
        for lr in range(2):
            P.dma("pool", QA[c][lr][64:69, :], qaug_d[lr, :, :], csem, writes=[BQA[c][lr]])
    wq = P.sb("wq_sb", [128, 8, 128], BF16); wk = P.sb("wk_sb", [128, 8, 128], BF16); wv = P.sb("wv_sb", [128, 8, 128], BF16)
    Bw = Buf("w_head"); wsem = None
    ctab_vals = [dict() for _ in range(NH)]
    def ccol(hi, val):
        d = ctab_vals[hi]
        if val not in d:
            d[val] = len(d)
            assert len(d) <= NCT
        return d[val]
    base = lambda tile: (0 if tile == nxt else 16 + 128 * tile)
    for hi in range(NH):
        for (dst, src) in ((wq, wq_d), (wk, wk_d), (wv, wv_d)):
            for c in range(8):
                P.dma("pool", dst[:, c, :], src[hi, c * 128:(c + 1) * 128, :], wsem, writes=[Bw])
        P.dma("sp", ctab[:], ctab_d[hi, 0:1, :].to_broadcast([128, NCT]), csem, writes=[Bctab])
        for c in range(2):
            P.dma("pool", KT[c][64:69, :], kaug_d[hi, :, :], csem, writes=[BKT[c]])
        P.barrier()
        with P.scope():
            x32 = [P.sb("x32_%d" % i, [128, D], F32) for i in range(2)]; Bx32 = [Buf("x32") for i in range(2)]; xsem = [P.newsem() for i in range(2)]
            xbf = [P.sb("xbf_%d" % i, [128, D], BF16) for i in range(2)]; Bxbf = [Buf("xbf") for i in range(2)]
            xT = [P.sb("xT_%d" % i, [128, 8, 512], BF16) for i in range(2)]; BxT = [Buf("xT") for i in range(2)]
            pt = [P.ps("pt%d" % i, [128, 8, 128], BF16) for i in range(2)]; Bpt = [Buf("pt") for i in range(2)]
            pq = [P.ps("pq%d" % i, [128, 512], F32) for i in range(2)]; Bpq = [Buf("pq") for i in range(2)]
            pk = [P.ps("pk%d" % i, [128, 512], F32) for i in range(2)]; Bpk = [Buf("pk") for i in range(2)]
            pv = [P.ps("pv%d" % i, [128, 512], F32) for i in range(2)]; Bpv = [Buf("pv") for i in range(2)]
            tcnt = 0
            ngr = (NT + 3) // 4
            for gi in range(ngr):
                tiles = list(range(gi * 4, min(NT, gi * 4 + 4)))
                w = len(tiles) * 128
                k2 = gi % 2
                for j, t in enumerate(tiles):
                    s = tcnt % 2; tcnt += 1
                    P.dma("sp", x32[s][:], x_d[t * 128:(t + 1) * 128, :], xsem[s], writes=[Bx32[s]])
                    P.op("dve" if j % 2 else "pool", lambda e, s=s: e.tensor_copy(xbf[s][:, :], x32[s][:, :]), reads=[Bx32[s]], writes=[Bxbf[s]])
                    transpose_tile(P, xbf[s], Bxbf[s], ident, Bid, pt[s], Bpt[s], xT[k2], BxT[k2], j, evac="act" if j % 2 else "dve")
                tok0 = tiles[0] * 128
                for c in range(8):
                    P.op("pe", lambda e, c=c: e.matmul(pq[k2][:, 0:w], wq[:, c, :], xT[k2][:, c, 0:w], start=(c == 0), stop=(c == 7)), reads=[Bw, BxT[k2]], writes=[Bpq[k2]])
                P.op("act", lambda e: e.activation(QT[:, tok0:tok0 + w], pq[k2][:, 0:w], AF.Identity, scale=0.125), reads=[Bpq[k2]], writes=[BQT])
                for cc in range(2):
                    for c in range(8):
                        P.op("pe", lambda e, c=c, cc=cc: e.matmul(pk[k2][0:64, cc * 0 + 0:w] if False else pk[k2][0:64, 0:w], wk[:, c, cc * 64:(cc + 1) * 64], xT[k2][:, c, 0:w], start=(c == 0), stop=(c == 7)), reads=[Bw, BxT[k2]], writes=[Bpk[k2]])
                    P.op("dve", lambda e, cc=cc: e.tensor_copy(KT[cc][0:64, tok0:tok0 + w], pk[k2][0:64, 0:w]), reads=[Bpk[k2]], writes=[BKT[cc]])
                for j, t in enumerate(tiles):
                    for c in range(8):
                        P.op("pe", lambda e, c=c, j=j: e.matmul(pv[k2][:, j * 128:(j + 1) * 128], xT[k2][:, c, j * 128:(j + 1) * 128], wv[:, c, :], start=(c == 0), stop=(c == 7)), reads=[Bw, BxT[k2]], writes=[Bpv[k2]])
                P.op("act", lambda e: e.activation(VA[:, tiles[0]:tiles[0] + len(tiles), 0:128], pv[k2][:, 0:w].rearrange("p (t d) -> p t d", d=128), AF.Identity), reads=[Bpv[k2]], writes=[BVA])
        P.barrier()
        with P.scope():
            psS = [[P.ps("psS%d%d" % (c, i), [128, 512], F32) for i in range(2)] for c in range(2)]
            BpsS = [[Buf("psS") for i in range(2)] for c in range(2)]
            oz = [[P.ps("oz%d%d" % (c, i), [128, 512], F32) for i in range(2)] for c in range(2)]
            Boz = [Buf("oz%d" % c) for c in range(2)]
            PT = [[P.sb("PT%d%d" % (c, i), [128, 512], BF16) for i in range(3)] for c in range(2)]
            BPT = [[Buf("PT") for i in range(3)] for c in range(2)]
            srt = [P.sb("srt%d" % c, [128, 512], F32) for c in range(2)]; Bsrt = [Buf("srt") for c in range(2)]
            fz = P.sb("fz", [128, 16], F32); Bfz = Buf("fz")
            fo = [P.sb("fo%d" % i, [128, 128], F32) for i in range(2)]; Bfo = [Buf("fo") for i in range(2)]
            fo2 = P.sb("fo2", [128, 128], F32); Bfo2 = Buf("fo2")
            osb = [P.sb("osb%d" % i, [128, 128], F32) for i in range(2)]; Bosb = [Buf("osb") for i in range(2)]; osem = [P.newsem() for i in range(2)]
            qtiles = [(j * 4, 4) for j in range(nxt // 4)] + [(nxt, 1)]
            assert nxt % 4 == 0
            it = 0; fcnt = 0
            for (qt0, qn) in qtiles:
                Wq = qn * 128
                qbase = base(qt0)
                for c in range(2):
                    for lr in range(2):
                        P.op("pool" if lr else "dve", lambda e, c=c, lr=lr: e.tensor_copy(QA[c][lr][0:64, 0:Wq], QT[c * 64:(c + 1) * 64, qt0 * 128:qt0 * 128 + Wq]), reads=[BQT], writes=[BQA[c][lr]])
                order = list(range(NT))
                def mk(ki, i, it):
                    Dq = qbase - base(i)
                    if Dq >= 127: typ = 0
                    elif Dq + Wq - 1 <= 0: typ = 1
                    else: typ = 2
                    return dict(ki=ki, i=i, it=it, Dq=Dq, typ=typ)
                def issue_S(inf):
                    i, typ, it_ = inf["i"], inf["typ"], inf["it"]
                    for c in range(2):
                        s2 = it_ % 2
                        ps = psS[c][s2]; Bps = BpsS[c][s2]
                        if typ < 2:
                            P.op("pe", lambda e, c=c, i=i, typ=typ, ps=ps: e.matmul(ps[:, 0:Wq], KT[c][0:69, i * 128:(i + 1) * 128], QA[c][typ][0:69, 0:Wq], start=True, stop=True), reads=[BKT[c], BQA[c][typ]], writes=[Bps])
                        else:
                            ps2 = psS[c][1 - s2]; Bps2 = BpsS[c][1 - s2]
                            P.op("pe", lambda e, c=c, i=i, ps=ps: e.matmul(ps[:, 0:Wq], KT[c][0:69, i * 128:(i + 1) * 128], QA[c][0][0:69, 0:Wq], start=True, stop=True), reads=[BKT[c], BQA[c][0]], writes=[Bps])
                            P.op("pe", lambda e, c=c, i=i, ps2=ps2: e.matmul(ps2[:, 0:Wq], KT[c][0:69, i * 128:(i + 1) * 128], QA[c][1][0:69, 0:Wq], start=True, stop=True), reads=[BKT[c], BQA[c][1]], writes=[Bps2])
                def issue_rest(inf):
                    i, typ, it_, Dq, ki = inf["i"], inf["typ"], inf["it"], inf["Dq"], inf["ki"]
                    for c in range(2):
                        s2 = it_ % 2; s3 = it_ % 3
                        ps = psS[c][s2]; Bps = BpsS[c][s2]
                        pt_ = PT[c][s3]; Bp = BPT[c][s3]
                        if typ < 2:
                            col = ccol(hi, -abs(Dq))
                            P.op("act", lambda e, ps=ps, pt_=pt_, col=col: e.activation(pt_[:, 0:Wq], ps[:, 0:Wq], AF.Exp, bias=ctab[:, col:col + 1], scale=1.0), reads=[Bps, Bctab], writes=[Bp])
                        else:
                            ps2 = psS[c][1 - s2]; Bps2 = BpsS[c][1 - s2]
                            colL = ccol(hi, -Dq); colR = ccol(hi, Dq)
                            P.op("act", lambda e, c=c, ps2=ps2, colR=colR: e.activation(srt[c][:, 0:Wq], ps2[:, 0:Wq], AF.Identity, bias=ctab[:, colR:colR + 1], scale=1.0), reads=[Bps2, Bctab], writes=[Bsrt[c]])
                            P.op("dve", lambda e, c=c, ps=ps, colL=colL: e.scalar_tensor_tensor(srt[c][:, 0:Wq], ps[:, 0:Wq], ctab[:, colL:colL + 1], srt[c][:, 0:Wq], ALU.add, ALU.min), reads=[Bps, Bsrt[c], Bctab], writes=[Bsrt[c]])
                            P.op("act", lambda e, c=c, pt_=pt_: e.activation(pt_[:, 0:Wq], srt[c][:, 0:Wq], AF.Exp), reads=[Bsrt[c]], writes=[Bp])
                    for c in range(2):
                        s3 = it_ % 3
                        pt_ = PT[c][s3]; Bp = BPT[c][s3]
                        for sub in range(qn):
                            P.op("pe", lambda e, c=c, sub=sub, i=i, pt_=pt_, ki=ki: e.matmul(oz[c][sub // 2][:, (sub % 2) * 129:(sub % 2) * 129 + 129], pt_[:, sub * 128:(sub + 1) * 128], VA[:, i, :], start=(ki == 0 and sub % 2 == 0), stop=(ki == NT - 1), skip_group_check=True), reads=[Bp, BVA], writes=[Boz[c]])
                infos = []
                for ki, i in enumerate(order):
                    infos.append(mk(ki, i, it)); it += 1
                def can_ahead(a, b):
                    return a["typ"] < 2 and b["typ"] < 2
                issued = set()
                for n, inf in enumerate(infos):
                    if n not in issued:
                        issue_S(inf); issued.add(n)
                    if n + 1 < len(infos) and can_ahead(inf, infos[n + 1]):
                        issue_S(infos[n + 1]); issued.add(n + 1)
                    issue_rest(inf)
                for sub in range(qn):
                    f = fcnt % 2; fcnt += 1
                    o0 = oz[0][sub // 2][:, (sub % 2) * 129:(sub % 2) * 129 + 129]; o1 = oz[1][sub // 2][:, (sub % 2) * 129:(sub % 2) * 129 + 129]
                    P.op("dve", lambda e, o0=o0: e.reciprocal(fz[:, 0:1], o0[:, 128:129]), reads=[Boz[0]], writes=[Bfz])
                    P.op("dve", lambda e, o1=o1: e.reciprocal(fz[:, 1:2], o1[:, 128:129]), reads=[Boz[1]], writes=[Bfz])
                    P.op("dve", lambda e: e.tensor_tensor(fz[:, 2:3], fz[:, 1:2], neglam, ALU.mult), reads=[Bfz, Blsc], writes=[Bfz])
                    P.op("dve", lambda e, o0=o0, f=f: e.tensor_scalar(fo[f][:, :], o0[:, 0:128], fz[:, 0:1], None, ALU.mult), reads=[Boz[0], Bfz], writes=[Bfo[f]])
                    P.op("dve", lambda e, o1=o1, f=f: e.scalar_tensor_tensor(fo[f][:, :], o1[:, 0:128], fz[:, 2:3], fo[f][:, :], ALU.mult, ALU.add), reads=[Boz[1], Bfz, Bfo[f]], writes=[Bfo[f]])
                    P.op("act", lambda e, f=f: e.activation(fo2[:, :], fo[f][:, :], AF.Square, accum_out=fz[:, 3:4]), reads=[Bfo[f]], writes=[Bfo2, Bfz])
                    P.op("act", lambda e: e.activation(fz[:, 4:5], fz[:, 3:4], AF.Ln, bias=SUBLN_EPS, scale=1.0 / 128), reads=[Bfz], writes=[Bfz])
                    P.op("act", lambda e: e.activation(fz[:, 5:6], fz[:, 4:5], AF.Exp, scale=-0.5), reads=[Bfz], writes=[Bfz])
                    P.op("dve", lambda e, f=f: e.scalar_tensor_tensor(osb[f][:, :], fo[f][:, :], fz[:, 5:6], gsc[:, :], ALU.mult, ALU.mult), reads=[Bfo[f], Bfz, Bgsc], writes=[Bosb[f]])
                    tt = qt0 + sub
                    P.dma("sp", o_d[tt * 128:(tt + 1) * 128, hi * 128:(hi + 1) * 128], osb[f][:], osem[f], reads=[Bosb[f]], writes=[Bout_d])
        P.barrier()
    P.finish([Bout_d])
    print("phaseA insts", P.ninst)
    nc = P.close()
    return nc, ctab_vals

def ref_attn(xp, pos, valid, w_in, lam4, subg, heads):
    T = xp.shape[0]
    D = 1024
    outs = []
    lam = np.exp((lam4[0] * lam4[1]).sum()) - np.exp((lam4[2] * lam4[3]).sum()) + 0.2
    for h in heads:
        q = xp @ w_in[:, h * 128:(h + 1) * 128]
        k = xp @ w_in[:, D + h * 128:D + (h + 1) * 128]
        v = xp @ w_in[:, 2 * D + h * 128:2 * D + (h + 1) * 128]
        sl = 2.0 ** (-(h + 1))
        dist = np.abs(pos[:, None] - pos[None, :]).astype(np.float32)
        ps = []
        for c in range(2):
            s = q[:, c * 64:(c + 1) * 64] @ k[:, c * 64:(c + 1) * 64].T / 8 - sl * dist
            s = np.where(valid[None, :], s, -np.inf)
            s = s - s.max(-1, keepdims=True)
            p = np.exp(s); p /= p.sum(-1, keepdims=True)
            ps.append(p)
        a = ps[0] - lam * ps[1]
        o = a @ v
        o = o / np.sqrt((o * o).mean(-1, keepdims=True) + 1e-5) * subg * 0.8
        outs.append(o)
    return np.concatenate(outs, 1)


import math
STAGE = 9
SKIP = ''
C = 64
NEGH = -math.exp(-0.5)

def rw_consts():
    m = np.zeros((64, 128), np.float32)
    s = np.arange(64)[:, None]; t = np.arange(64)[None, :]
    m[:, 0:64] = (s < t); m[:, 64:128] = (s <= t)
    return m

def build_phaseC(nch, NHD=8, D=1024, GW=512):
    P = Prog(); nc = P.nc
    Tp = nch * C
    NCH = NHD * 64
    xT_d = P.dram("xT", [D, Tp + 2], F32, "ExternalInput")
    mu_d = P.dram("mu", [D, 12], F32, "ExternalInput")
    wr_d = P.dram("wr", [D, NCH], F32, "ExternalInput"); wk_d = P.dram("wk", [D, NCH], F32, "ExternalInput"); wv_d = P.dram("wv", [D, NCH], F32, "ExternalInput")
    w1_d = P.dram("w1", [D, 64], F32, "ExternalInput"); w2_d = P.dram("w2", [64, NCH], F32, "ExternalInput")
    a1_d = P.dram("a1", [D, 64], F32, "ExternalInput"); a2_d = P.dram("a2", [64, NCH], F32, "ExternalInput")
    g1_d = P.dram("g1", [D, 160], F32, "ExternalInput"); g2_d = P.dram("g2", [160, NCH], F32, "ExternalInput")
    chv_d = P.dram("chv", [64, NHD, 8], F32, "ExternalInput")
    msk_d = P.dram("msk", [64, 128], F32, "ExternalInput")
    seg_d = P.dram("seg", [1, GW], F32, "ExternalInput")
    id_d = P.dram("ident", [128, 128], F32, "ExternalInput")
    y_d = P.dram("y", [Tp, NCH], F32, "ExternalOutput")
    aux_d = P.dram("aux", [4, NCH, Tp], F32, "ExternalOutput")
    By = Buf("y_d"); Baux = Buf("aux_d")
    ident = P.sb("ident_sb", [128, 128], BF16); Bid = Buf("ident"); P.dma("pool", ident[:], id_d[:, :], None, writes=[Bid])
    identf = P.sb("identf", [128, 128], F32); Bidf = Buf("identf"); P.dma("sp", identf[:], id_d[:, :], None, writes=[Bidf])
    def wload(name, src, K, N):
        kc = (K + 127) // 128
        t = P.sb(name, [128, kc, N], BF16); B = Buf(name)
        for c in range(kc):
            rows = min(128, K - c * 128)
            P.dma("pool", t[0:rows, c, :], src[c * 128:c * 128 + rows, :], None, writes=[B])
        return t, B
    wr, Bwr = wload("wr", wr_d, D, NCH); wk, Bwk = wload("wk", wk_d, D, NCH); wv, Bwv = wload("wv", wv_d, D, NCH)
    w1, Bw1 = wload("w1", w1_d, D, 64); a1, Ba1 = wload("a1", a1_d, D, 64); g1, Bg1 = wload("g1", g1_d, D, 160)
    w2, Bw2 = wload("w2", w2_d, 64, NCH); a2, Ba2 = wload("a2", a2_d, 64, NCH); g2, Bg2 = wload("g2", g2_d, 160, NCH)
    mu = P.sb("mu", [128, 8, 12], F32); Bmu = Buf("mu")
    P.dma("sp", mu[:], mu_d.rearrange("(c p) m -> p c m", p=128), None, writes=[Bmu])
    muc = P.sb("muc", [128, 8, 6], F32)
    P.op("dve", lambda e: e.tensor_tensor(muc[:, :, :], mu[:, :, 0:6], mu[:, :, 6:12], ALU.add), reads=[Bmu], writes=[Bmu])
    P.op("dve", lambda e: e.tensor_scalar(muc[:, :, :], muc[:, :, :], -1.0, 1.0, ALU.mult, ALU.add), reads=[Bmu], writes=[Bmu])
    chv = P.sb("chv", [64, NHD, 8], F32); Bchv = Buf("chv"); P.dma("sp", chv[:], chv_d[:, :, :], None, writes=[Bchv])
    P.op("dve", lambda e: e.tensor_scalar(chv[:, :, 4:5], chv[:, :, 3:4], -1.0, 1.0, ALU.mult, ALU.add), reads=[Bchv], writes=[Bchv])
    msk = P.sb("msk", [64, 128], F32); Bmsk = Buf("msk"); P.dma("sp", msk[:], msk_d[:, :], None, writes=[Bmsk])
    seg = P.sb("seg", [64, GW], F32); Bseg = Buf("seg"); P.dma("sp", seg[:], seg_d[0:1, :].to_broadcast([64, GW]), None, writes=[Bseg])
    ones64 = P.sb("ones64", [64, 64], F32); Bones = Buf("ones"); P.op("pool", lambda e: e.memset(ones64[:, :], 1.0), writes=[Bones])
    ST = P.sb("ST", [64, NHD, 64], F32); STb = P.sb("STb", [64, NHD, 64], BF16); BST = [Buf("ST%d" % h) for h in range(NHD)]
    P.op("pool", lambda e: e.memset(ST[:, :, :], 0.0), writes=BST)
    P.op("pool", lambda e: e.memset(STb[:, :, :], 0.0), writes=BST)
    x32 = P.sb("x32", [128, 8, GW + 2], F32); Bx32 = Buf("x32")
    xs = [P.sb("xs%d" % i, [128, 8, GW], BF16) for i in range(6)]; Bxs = [Buf("xs%d" % i) for i in range(6)]
    hw = P.sb("hw", [64, GW], BF16); Bhw = Buf("hw"); ha = P.sb("ha", [64, GW], BF16); Bha = Buf("ha")
    hg = P.sb("hg", [128, 2, GW], BF16); Bhg = Buf("hg")
    names = "r k v a lw kk ss t1 t2 Lc KRk".split()
    F = {n: P.sb("f_" + n, [64, GW], F32) for n in ["r", "k", "v", "a", "lw", "kk", "t1", "t2", "Lc", "kd", "g"]}
    BF = {n: Buf("f_" + n) for n in F}
    HB = 4
    NS = 3
    KRl = [P.sb("KR%d" % i, [64, GW // C, 2, C], BF16) for i in range(HB)]; BKRl = [Buf("KR") for i in range(HB)]
    KBl = [P.sb("KB%d" % i, [64, 2, GW], BF16) for i in range(HB)]; BKBl = [Buf("KB") for i in range(HB)]
    HTl = [P.sb("HT%d" % i, [64, 3, GW], BF16) for i in range(HB)]; BHTl = [Buf("HT") for i in range(HB)]
    gCl = [P.sb("gC%d" % i, [64, GW // C], F32) for i in range(HB)]; BgCl = [Buf("gC") for i in range(HB)]
    mskT = P.sb("mskT", [64, 64], F32)
    P.op("pool", lambda e: e.tensor_scalar(mskT[:, :], msk[:, 64:128], -1.0, 1.0, ALU.mult, ALU.add), reads=[Bmsk], writes=[Bmsk])
    class Slot: pass
    slots = []
    for si in range(NS):
        sl = Slot()
        sl.TM = P.sb("TM%d" % si, [64, 3, 64], BF16); sl.BTM = Buf("TM")
        sl.MA = P.sb("MA%d" % si, [64, 128], BF16); sl.BMA = Buf("MA")
        sl.MB = P.sb("MB%d" % si, [64, 128], BF16); sl.BMB = Buf("MB")
        sl.NT = P.sb("NT%d" % si, [64, 64], BF16); sl.BNT = Buf("NT")
        sl.X = [P.sb("X%d_%d" % (si, i), [64, 64], BF16) for i in range(2)]; sl.XT = [P.sb("XT%d_%d" % (si, i), [64, 64], BF16) for i in range(2)]
        sl.BX = [Buf("X") for i in range(2)]; sl.BXT = [Buf("XT") for i in range(2)]
        sl.R = P.sb("R%d" % si, [64, 64], F32); sl.Rb = P.sb("Rb%d" % si, [64, 64], BF16); sl.BR = Buf("R")
        sl.Wt = P.sb("Wt%d" % si, [64, 64], BF16); sl.BWt = Buf("Wt")
        sl.Ut = P.sb("Ut%d" % si, [64, 64], BF16); sl.BUt = Buf("Ut")
        slots.append(sl)
    ysb = P.sb("ysb", [64, GW // C, NHD, 64], F32); Bysb = Buf("ysb")
    ysem = P.newsem()
    banks = [P.ps("bank%d" % i, [128, 512], F32) for i in range(7)]; Bbanks = [Buf("bank%d" % i) for i in range(7)]
    pt = P.ps("ptr", [64, 3, 128], BF16); Bptr = Buf("ptr")
    for si in range(NS):
        slots[si].pa = banks[2 * si]; slots[si].Bpa = Bbanks[2 * si]
        slots[si].pw = banks[2 * si + 1]; slots[si].Bpw = Bbanks[2 * si + 1]
    pp = [banks[0], banks[1]]; Bpp = [Bbanks[0], Bbanks[1]]
    pl = banks[2]; Bpl = Bbanks[2]
    psq = banks[3]; Bpsq = Bbanks[3]
    ngr = (Tp + GW - 1) // GW
    ppi = 0
    for gi in range(ngr):
        t0 = gi * GW
        W = min(GW, Tp - t0)
        ncg = W // C
        for c in range(8):
            P.dma("sp", x32[:, c, 0:W + 2], xT_d[c * 128:(c + 1) * 128, t0:t0 + W + 2], None, writes=[Bx32])
        for i in range(6):
            for c in range(8):
                eng = "dve" if (i * 8 + c) % 2 == 0 else "pool"
                P.op("dve", lambda e, i=i, c=c: e.tensor_scalar(xs[i][:, c, 0:W], x32[:, c, 1:W + 1], muc[:, c, i:i + 1], None, ALU.mult), reads=[Bx32, Bmu], writes=[Bxs[i]])
                P.op("dve", lambda e, i=i, c=c: e.scalar_tensor_tensor(xs[i][:, c, 0:W], x32[:, c, 0:W], mu[:, c, i:i + 1], xs[i][:, c, 0:W], ALU.mult, ALU.add), reads=[Bx32, Bmu, Bxs[i]], writes=[Bxs[i]])
                P.op("dve", lambda e, i=i, c=c: e.scalar_tensor_tensor(xs[i][:, c, 0:W], x32[:, c, 2:W + 2], mu[:, c, 6 + i:7 + i], xs[i][:, c, 0:W], ALU.mult, ALU.add), reads=[Bx32, Bmu, Bxs[i]], writes=[Bxs[i]])
        for c in range(8):
            P.op("pe", lambda e, c=c: e.matmul(pl[0:64, 0:W], w1[:, c, :], xs[3][:, c, 0:W], start=(c == 0), stop=(c == 7)), reads=[Bw1, Bxs[3]], writes=[Bpl])
        P.op("act", lambda e: e.activation(hw[:, 0:W], pl[0:64, 0:W], AF.Tanh), reads=[Bpl], writes=[Bhw])
        for c in range(8):
            P.op("pe", lambda e, c=c: e.matmul(pl[0:64, 0:W], a1[:, c, :], xs[4][:, c, 0:W], start=(c == 0), stop=(c == 7)), reads=[Ba1, Bxs[4]], writes=[Bpl])
        P.op("act", lambda e: e.activation(ha[:, 0:W], pl[0:64, 0:W], AF.Identity), reads=[Bpl], writes=[Bha])
        for part, (lo, n) in enumerate(((0, 128), (128, 32))):
            for c in range(8):
                P.op("pe", lambda e, c=c, lo=lo, n=n: e.matmul(pl[0:n, 0:W], g1[:, c, lo:lo + n], xs[5][:, c, 0:W], start=(c == 0), stop=(c == 7)), reads=[Bg1, Bxs[5]], writes=[Bpl])
            P.op("act", lambda e, part=part, n=n: e.activation(hg[0:n, part, 0:W], pl[0:n, 0:W], AF.Sigmoid), reads=[Bpl], writes=[Bhg])
        for h in range(NHD):
            hb = h % HB
            KR, BKR, KB, BKB, HT, BHT, gC, BgC = KRl[hb], BKRl[hb], KBl[hb], BKBl[hb], HTl[hb], BHTl[hb], gCl[hb], BgCl[hb]
            cs = slice(h * 64, (h + 1) * 64)
            cw0, ca0, ckk, cka, c1ka = [chv[:, h, i:i + 1] for i in range(5)]
            def proj(wt, Bw, xi, dst, func=AF.Identity, **kw):
                nonlocal ppi
                p = pp[ppi % 2]; Bp = Bpp[ppi % 2]; ppi += 1
                for c in range(8):
                    P.op("pe", lambda e, c=c: e.matmul(p[0:64, 0:W], wt[:, c, cs], xs[xi][:, c, 0:W], start=(c == 0), stop=(c == 7)), reads=[Bw, Bxs[xi]], writes=[Bp])
                P.op("act", lambda e: e.activation(F[dst][:, 0:W], p[0:64, 0:W], func, **kw), reads=[Bp], writes=[BF[dst]])
            proj(wr, Bwr, 0, "r"); proj(wk, Bwk, 1, "k"); proj(wv, Bwv, 2, "v")
            def lora(w2t, Bw2_, hsrc, Bh, dst, bias):
                nonlocal ppi
                p = pp[ppi % 2]; Bp = Bpp[ppi % 2]; ppi += 1
                P.op("pe", lambda e: e.matmul(p[0:64, 0:W], w2t[0:64, 0, cs], hsrc[0:64, 0:W], start=True, stop=True), reads=[Bw2_, Bh], writes=[Bp])
                P.op("act", lambda e: e.activation(F[dst][:, 0:W], p[0:64, 0:W], AF.Sigmoid, bias=bias, scale=1.0), reads=[Bp, Bchv], writes=[BF[dst]])
            lora(w2, Bw2, hw, Bhw, "lw", cw0)
            lora(a2, Ba2, ha, Bha, "a", ca0)
            p = pp[ppi % 2]; Bp = Bpp[ppi % 2]; ppi += 1
            P.op("pe", lambda e: e.matmul(p[0:64, 0:W], g2[:, 0, cs], hg[:, 0, 0:W], start=True, stop=False), reads=[Bg2, Bhg], writes=[Bp])
            P.op("pe", lambda e: e.matmul(p[0:64, 0:W], g2[0:32, 1, cs], hg[0:32, 1, 0:W], start=False, stop=True), reads=[Bg2, Bhg], writes=[Bp])
            P.op("act", lambda e: e.activation(F["g"][:, 0:W], p[0:64, 0:W], AF.Identity), reads=[Bp], writes=[BF["g"]])
            P.op("dve", lambda e: e.tensor_scalar(F["kk"][:, 0:W], F["k"][:, 0:W], ckk, None, ALU.mult), reads=[BF["k"], Bchv], writes=[BF["kk"]])
            P.op("dve", lambda e: e.tensor_tensor(F["t1"][:, 0:W], F["kk"][:, 0:W], F["kk"][:, 0:W], ALU.mult), reads=[BF["kk"]], writes=[BF["t1"]])
            if 'a' not in SKIP:
                P.op("pe", lambda e: e.matmul(psq[0:64, 0:W], ones64[:, :], F["t1"][:, 0:W], start=True, stop=True), reads=[Bones, BF["t1"]], writes=[Bpsq])
            P.op("act", lambda e: e.activation(F["t2"][:, 0:W], psq[0:64, 0:W], AF.Ln, bias=1e-24, scale=1.0), reads=[Bpsq], writes=[BF["t2"]])
            P.op("act", lambda e: e.activation(F["t2"][:, 0:W], F["t2"][:, 0:W], AF.Exp, scale=-0.5), reads=[BF["t2"]], writes=[BF["t2"]])
            P.op("dve", lambda e: e.tensor_tensor(F["kk"][:, 0:W], F["kk"][:, 0:W], F["t2"][:, 0:W], ALU.mult), reads=[BF["kk"], BF["t2"]], writes=[BF["kk"]])
            P.op("dve", lambda e: e.tensor_scalar(F["t1"][:, 0:W], F["a"][:, 0:W], cka, c1ka, ALU.mult, ALU.add), reads=[BF["a"], Bchv], writes=[BF["t1"]])
            P.op("dve", lambda e: e.tensor_tensor(F["kd"][:, 0:W], F["k"][:, 0:W], F["t1"][:, 0:W], ALU.mult), reads=[BF["k"], BF["t1"]], writes=[BF["kd"]])
            P.op("pool", lambda e: e.tensor_tensor(F["a"][:, 0:W], F["a"][:, 0:W], F["kk"][:, 0:W], ALU.mult), reads=[BF["a"], BF["kk"]], writes=[BF["a"]])
            P.op("pool", lambda e: e.tensor_scalar(F["lw"][:, 0:W], F["lw"][:, 0:W], NEGH, None, ALU.mult), reads=[BF["lw"]], writes=[BF["lw"]])
            if 'c' not in SKIP:
              P.op("dve", lambda e: e.tensor_tensor_scan(F["Lc"][:, 0:W], seg[:, 0:W], F["lw"][:, 0:W], 0.0, ALU.mult, ALU.add), reads=[Bseg, BF["lw"]], writes=[BF["Lc"]])
            Lc3 = F["Lc"][:, 0:W].rearrange("p (n c) -> p n c", c=C)
            P.op("act", lambda e: e.activation(gC[:, 0:ncg], Lc3[:, :, C - 1], AF.Exp), reads=[BF["Lc"]], writes=[BgC])
            P.op("act", lambda e: e.activation(F["t1"][:, 0:W], F["Lc"][:, 0:W], AF.Exp), reads=[BF["Lc"]], writes=[BF["t1"]])
            P.op("dve", lambda e: e.tensor_tensor(KR[:, 0:ncg, 1, :], F["r"][:, 0:W].rearrange("p (n c) -> p n c", c=C), F["t1"][:, 0:W].rearrange("p (n c) -> p n c", c=C), ALU.mult), reads=[BF["r"], BF["t1"]], writes=[BKR])
            P.op("pool", lambda e: e.tensor_tensor(F["t2"][:, 0:W], F["Lc"][:, 0:W], F["lw"][:, 0:W], ALU.subtract), reads=[BF["Lc"], BF["lw"]], writes=[BF["t2"]])
            P.op("act", lambda e: e.activation(F["t2"][:, 0:W], F["t2"][:, 0:W], AF.Exp), reads=[BF["t2"]], writes=[BF["t2"]])
            P.op("dve", lambda e: e.tensor_tensor(KR[:, 0:ncg, 0, :], F["kk"][:, 0:W].rearrange("p (n c) -> p n c", c=C), F["t2"][:, 0:W].rearrange("p (n c) -> p n c", c=C), ALU.mult), reads=[BF["kk"], BF["t2"]], writes=[BKR])
            P.op("act", lambda e: e.activation(F["t1"][:, 0:W], F["Lc"][:, 0:W], AF.Exp, scale=-1.0), reads=[BF["Lc"]], writes=[BF["t1"]])
            P.op("dve", lambda e: e.tensor_tensor(KB[:, 0, 0:W], F["kd"][:, 0:W], F["t1"][:, 0:W], ALU.mult), reads=[BF["kd"], BF["t1"]], writes=[BKB])
            P.op("pool", lambda e: e.tensor_tensor(KB[:, 1, 0:W], F["a"][:, 0:W], F["t1"][:, 0:W], ALU.mult), reads=[BF["a"], BF["t1"]], writes=[BKB])
            if 'b' not in SKIP:
              P.op("dve", lambda e: e.tensor_tensor(F["t2"][:, 0:W].rearrange("p (n c) -> p n c", c=C), Lc3[:, :, C - 1:C].to_broadcast([64, ncg, C]), Lc3, ALU.subtract), reads=[BF["Lc"]], writes=[BF["t2"]])
            P.op("act", lambda e: e.activation(F["t2"][:, 0:W], F["t2"][:, 0:W], AF.Exp), reads=[BF["t2"]], writes=[BF["t2"]])
            P.op("dve", lambda e: e.tensor_tensor(HT[:, 0, 0:W], F["kd"][:, 0:W], F["t2"][:, 0:W], ALU.mult), reads=[BF["kd"], BF["t2"]], writes=[BHT])
            P.op("dve", lambda e: e.scalar_tensor_tensor(HT[:, 1, 0:W], F["a"][:, 0:W], -1.0, F["t2"][:, 0:W], ALU.mult, ALU.mult), reads=[BF["a"], BF["t2"]], writes=[BHT])
            P.op("pool", lambda e: e.tensor_copy(HT[:, 2, 0:W], F["v"][:, 0:W]), reads=[BF["v"]], writes=[BHT])
            for ai, nm in enumerate(("r", "kd", "v", "g")):
                P.dma("sp", aux_d[ai, h * 64:(h + 1) * 64, t0:t0 + W], F[nm][:, 0:W], None, reads=[BF[nm]], writes=[Baux])
            if hb == HB - 1 or h == NHD - 1:
                batch = list(range(h - hb, h + 1))
                def unit(hh, ci, sl):
                    hb2 = hh % HB
                    KR, BKR, KB, BKB, HT, BHT, gC, BgC = KRl[hb2], BKRl[hb2], KBl[hb2], BKBl[hb2], HTl[hb2], BHTl[hb2], gCl[hb2], BgCl[hb2]
                    cc = slice(ci * C, (ci + 1) * C)
                    pa, Bpa, pw, Bpw = sl.pa, sl.Bpa, sl.pw, sl.Bpw
                    TM, BTM, MA, BMA, MB, BMB, NT_, BNT, R, Rb, BR, Wt, BWt, Ut, BUt = sl.TM, sl.BTM, sl.MA, sl.BMA, sl.MB, sl.BMB, sl.NT, sl.BNT, sl.R, sl.Rb, sl.BR, sl.Wt, sl.BWt, sl.Ut, sl.BUt
                    for q in range(3):
                        P.op("pe", lambda e, q=q: e.transpose(pt[:, q, 0:64], HT[:, q, cc], ident[0:64, 0:64]), reads=[BHT, Bid], writes=[Bptr])
                    P.op("dve", lambda e: e.tensor_copy(TM[:, :, :], pt[:, :, 0:64]), reads=[Bptr], writes=[BTM])
                    P.op("pe", lambda e: e.matmul(pa[0:64, 0:128], KB[:, 0, cc], KR[:, ci, :, :], start=True, stop=True), reads=[BKB, BKR], writes=[Bpa])
                    P.op("pe", lambda e: e.matmul(pa[0:64, 128:256], KB[:, 1, cc], KR[:, ci, :, :], start=False, stop=True, skip_group_check=True), reads=[BKB, BKR], writes=[Bpa])
                    P.op("pe", lambda e: e.matmul(pa[0:64, 256:320], KR[:, ci, 0, :], KB[:, 1, cc], start=False, stop=True, skip_group_check=True), reads=[BKB, BKR], writes=[Bpa])
                    yield
                    P.op("dve", lambda e: e.tensor_tensor(MA[:, :], pa[0:64, 0:128], msk[:, :], ALU.mult), reads=[Bpa, Bmsk], writes=[BMA])
                    P.op("dve", lambda e: e.tensor_tensor(MB[:, 0:64], pa[0:64, 128:192], msk[:, 0:64], ALU.mult), reads=[Bpa, Bmsk], writes=[BMB])
                    P.op("dve", lambda e: e.scalar_tensor_tensor(MB[:, 64:128], pa[0:64, 192:256], -1.0, msk[:, 64:128], ALU.mult, ALU.mult), reads=[Bpa, Bmsk], writes=[BMB])
                    P.op("dve", lambda e: e.tensor_tensor(NT_[:, :], pa[0:64, 256:320], mskT[:, :], ALU.mult), reads=[Bpa, Bmsk], writes=[BNT])
                    P.op("dve", lambda e: e.tensor_tensor(R[:, :], identf[0:64, 0:64], MB[:, 0:64], ALU.subtract), reads=[Bidf, BMB], writes=[BR])
                    P.op("act", lambda e: e.activation(Rb[:, :], R[:, :], AF.Identity), reads=[BR], writes=[BR])
                    yield
                    curX, curXT, BcX, BcXT = MB[:, 0:64], NT_[:, :], BMB, BNT
                    for lvl in range(5):
                        k2 = lvl % 2
                        P.op("pe", lambda e, curX=curX, curXT=curXT: e.matmul(pw[0:64, 0:64], curXT, curX, start=True, stop=True), reads=[BcX, BcXT], writes=[Bpw])
                        P.op("pe", lambda e, curX=curX, curXT=curXT: e.matmul(pw[0:64, 64:128], curX, curXT, start=False, stop=True, skip_group_check=True), reads=[BcX, BcXT], writes=[Bpw])
                        yield
                        P.op("dve", lambda e, k2=k2: e.tensor_copy(sl.X[k2][:, :], pw[0:64, 0:64]), reads=[Bpw], writes=[sl.BX[k2]])
                        P.op("dve", lambda e, k2=k2: e.tensor_copy(sl.XT[k2][:, :], pw[0:64, 64:128]), reads=[Bpw], writes=[sl.BXT[k2]])
                        P.op("pe", lambda e, k2=k2: e.matmul(pw[0:64, 128:192], sl.XT[k2][:, :], Rb[:, :], start=False, stop=True, skip_group_check=True), reads=[sl.BXT[k2], BR], writes=[Bpw])
                        yield
                        P.op("dve", lambda e: e.tensor_tensor(R[:, :], R[:, :], pw[0:64, 128:192], ALU.add), reads=[BR, Bpw], writes=[BR])
                        P.op("act", lambda e: e.activation(Rb[:, :], R[:, :], AF.Identity), reads=[BR], writes=[BR])
                        yield
                        curX, curXT, BcX, BcXT = sl.X[k2][:, :], sl.XT[k2][:, :], sl.BX[k2], sl.BXT[k2]
                    P.op("pe", lambda e: e.matmul(pw[0:64, 192:256], KR[:, ci, 0, :], STb[:, hh, :], start=False, stop=False, skip_group_check=True), reads=[BKR, BST[hh]], writes=[Bpw])
                    P.op("pe", lambda e: e.matmul(pw[0:64, 192:256], MA[:, 0:64], TM[:, 2, :], start=False, stop=True, skip_group_check=True), reads=[BMA, BTM], writes=[Bpw])
                    yield
                    P.op("dve", lambda e: e.tensor_copy(Wt[:, :], pw[0:64, 192:256]), reads=[Bpw], writes=[BWt])
                    P.op("pe", lambda e: e.matmul(pw[0:64, 256:320], Rb[:, :], Wt[:, :], start=False, stop=True, skip_group_check=True), reads=[BR, BWt], writes=[Bpw])
                    yield
                    P.op("dve", lambda e: e.tensor_copy(Ut[:, :], pw[0:64, 256:320]), reads=[Bpw], writes=[BUt])
                    P.op("pe", lambda e: e.matmul(pw[0:64, 320:384], KR[:, ci, 1, :], STb[:, hh, :], start=False, stop=False, skip_group_check=True), reads=[BKR, BST[hh]], writes=[Bpw])
                    P.op("pe", lambda e: e.matmul(pw[0:64, 320:384], MA[:, 64:128], TM[:, 2, :], start=False, stop=False, skip_group_check=True), reads=[BMA, BTM], writes=[Bpw])
                    P.op("pe", lambda e: e.matmul(pw[0:64, 320:384], MB[:, 64:128], Ut[:, :], start=False, stop=True, skip_group_check=True), reads=[BMB, BUt], writes=[Bpw])
                    P.op("pe", lambda e: e.matmul(pw[0:64, 384:448], TM[:, 0, :], TM[:, 2, :], start=False, stop=False, skip_group_check=True), reads=[BTM], writes=[Bpw])
                    P.op("pe", lambda e: e.matmul(pw[0:64, 384:448], TM[:, 1, :], Ut[:, :], start=False, stop=True, skip_group_check=True), reads=[BTM, BUt], writes=[Bpw])
                    yield
                    P.op("dve", lambda e: e.tensor_copy(ysb[:, ci, hh, :], pw[0:64, 320:384]), reads=[Bpw], writes=[Bysb])
                    P.op("dve", lambda e: e.scalar_tensor_tensor(ST[:, hh, :], ST[:, hh, :], gC[:, ci:ci + 1], pw[0:64, 384:448], ALU.mult, ALU.add), reads=[BST[hh], BgC, Bpw], writes=[BST[hh]])
                    P.op("act", lambda e: e.activation(STb[:, hh, :], ST[:, hh, :], AF.Identity), reads=[BST[hh]], writes=[BST[hh]])
                    yield
                todo = [(hh, ci) for ci in range(ncg) for hh in batch]
                active = []
                free = list(range(NS))
                while todo or active:
                    while todo and free:
                        hh, ci = todo.pop(0); si = free.pop(0)
                        active.append((unit(hh, ci, slots[si]), si))
                    nxt_active = []
                    for gen, si in active:
                        try:
                            next(gen); nxt_active.append((gen, si))
                        except StopIteration:
                            free.append(si)
                    active = nxt_active
        for ci in range(ncg):
            P.dma("sp", y_d[t0 + ci * C:t0 + (ci + 1) * C, :], ysb[:, ci, :, :], ysem, reads=[Bysb], writes=[By])
    P.finish([By, Baux])
    print("phaseC insts", P.ninst)
    return P.close()

def ref_dir(xs6, Wd, nh):
    T = xs6.shape[1]
    r = xs6[0] @ Wd["wr"]; k = xs6[1] @ Wd["wk"]; v = xs6[2] @ Wd["wv"]
    lw = np.tanh(xs6[3] @ Wd["w1"]) @ Wd["w2"]
    z = Wd["w0"] + lw
    w_log = -np.log1p(np.exp(-z)) - 0.5
    decay = np.exp(-np.exp(w_log))
    a = 1 / (1 + np.exp(-(Wd["a0"] + (xs6[4] @ Wd["a1"]) @ Wd["a2"])))
    g = (1 / (1 + np.exp(-(xs6[5] @ Wd["g1"])))) @ Wd["g2"]
    kk = (k * Wd["k_k"]).reshape(T, nh, 64)
    kk = kk / np.maximum(np.sqrt((kk * kk).sum(-1, keepdims=True)), 1e-12)
    kd = k * (1 + (a - 1) * Wd["k_a"])
    rh = r.reshape(T, nh, 64); wh = decay.reshape(T, nh, 64); kdh = kd.reshape(T, nh, 64); vh = v.reshape(T, nh, 64); ah = a.reshape(T, nh, 64)
    S = np.zeros((nh, 64, 64)); ys = np.zeros((T, nh, 64))
    for t in range(T):
        sa = np.einsum('hvk,hk->hv', S, kk[t])
        S = S * wh[t][:, None, :] - sa[:, :, None] * (kk[t] * ah[t])[:, None, :] + vh[t][:, :, None] * kdh[t][:, None, :]
        ys[t] = np.einsum('hvk,hk->hv', S, rh[t])
    return ys.reshape(T, nh * 64), r, kd, v, g


LNX_EPS = 64e-5

def build_phaseD(ntiles, FE=3584, E=8, D=1024, FW=256, PART=11):
    P = Prog(); nc = P.nc
    T = ntiles * 128
    NHh = D // 64
    names = ["yf", "yb", "r", "kdf", "kdb", "v", "g", "h1"]
    ind = {n: P.dram(n, [T, D], F32, "ExternalInput") for n in names}
    wo_d = P.dram("wo", [D, D], F32, "ExternalInput")
    vec_d = P.dram("vecs", [7, D], F32, "ExternalInput")
    wr_d = P.dram("wrt", [E, D], F32, "ExternalInput")
    br_d = P.dram("brt", [1, E], F32, "ExternalInput")
    wg_d = P.dram("wg", [E, D, FE], F32, "ExternalInput")
    wu_d = P.dram("wu", [E, D, FE], F32, "ExternalInput")
    wd_d = P.dram("wd", [E, FE, D], F32, "ExternalInput")
    id_d = P.dram("ident", [128, 128], F32, "ExternalInput")
    out_d = P.dram("out", [T, D], F32, "ExternalOutput")
    Bout_d = Buf("out_d")
    ident = P.sb("ident_sb", [128, 128], BF16); Bid = Buf("ident"); P.dma("pool", ident[:], id_d[:, :], None, writes=[Bid])
    wo, Bwo = load_w_bf16(P, "wo_sb", wo_d, D, D, None)
    vb = []; Bvb = []
    for i in range(7):
        t, B = load_bcast(P, "vec%d" % i, vec_d[i:i + 1, :], D, None); vb.append(t); Bvb.append(B)
    wrb = P.sb("wrb", [128, E, D], F32); Bwrb = Buf("wrb")
    for e_ in range(E):
        P.dma("sp", wrb[:, e_, :], wr_d[e_:e_ + 1, :].to_broadcast([128, D]), None, writes=[Bwrb])
    brb, Bbrb = load_bcast(P, "brb", br_d[0:1, :], E, None)
    halves = [list(range(a, min(ntiles, a + PART))) for a in range(0, ntiles, PART)]
    maxh = max(len(h) for h in halves)
    hT = P.sb("hT", [128, 8, maxh * 128], BF16); BhT = Buf("hT")
    yacc = P.sb("yacc", [128, maxh, D], F32); Byacc = [Buf("yacc%d" % i) for i in range(maxh)]
    gates = P.sb("gates", [128, maxh, E], F32); Bgates = Buf("gates")
    st = P.sb("lnst", [128, 12], F32); mv = P.sb("lnmv", [128, 4], F32); Bscr = Buf("lnscr")
    pt = [P.ps("pt%d" % i, [128, 8, 128], BF16) for i in range(2)]; Bpt = [Buf("pt") for i in range(2)]
    pm = [P.ps("pm%d" % i, [128, 512], F32) for i in range(2)]; Bpm = [Buf("pm") for i in range(2)]
    pg = [P.ps("pg%d" % i, [128, 512], F32) for i in range(2)]; Bpg = [Buf("pg") for i in range(2)]
    pu = [P.ps("pu%d" % i, [128, 512], F32) for i in range(2)]; Bpu = [Buf("pu") for i in range(2)]
    NFC = FW // 128
    outt = P.sb("outt", [128, D], F32); Boutt = Buf("outt"); outsem = P.newsem()
    ptc = 0; wcc = 0; hc = 0
    for half in halves:
        if not half: continue
        with P.scope():
            tin = {n: P.sb("in_" + n, [128, D], F32) for n in names}; Bin = {n: Buf("in_" + n) for n in names}
            s16 = P.sb("s16", [128, 8, NHh], F32); Bs16 = Buf("s16")
            tmp = P.sb("tmp", [128, D], F32); Btmp = Buf("tmp")
            obf = P.sb("obf", [128, D], BF16); Bobf = Buf("obf")
            oT = P.sb("oT", [128, 8, 128], BF16); BoT = Buf("oT")
            hbf = P.sb("hbf", [128, D], BF16); Bhbf = Buf("hbf")
            lg = P.sb("lg", [128, 4, E], F32); Blg = Buf("lg")
            for j, t in enumerate(half):
                for n in names:
                    P.dma("sp", tin[n][:], ind[n][t * 128:(t + 1) * 128, :], None, writes=[Bin[n]])
                y = tin["yf"]; By_ = Bin["yf"]
                v3 = lambda a: a[:, :].rearrange("p (h c) -> p h c", c=64)
                P.op("dve", lambda e: e.tensor_tensor(y[:, :], y[:, :], tin["yb"][:, :], ALU.add), reads=[By_, Bin["yb"]], writes=[By_])
                P.op("dve", lambda e: e.tensor_reduce(s16[:, 0, :], v3(y), AX.X, ALU.add), reads=[By_], writes=[Bs16])
                P.op("pool", lambda e: e.tensor_tensor(tmp[:, :], y[:, :], y[:, :], ALU.mult), reads=[By_], writes=[Btmp])
                P.op("dve", lambda e: e.tensor_reduce(s16[:, 1, :], v3(tmp), AX.X, ALU.add), reads=[Btmp], writes=[Bs16])
                P.op("dve", lambda e: e.tensor_scalar(s16[:, 0, :], s16[:, 0, :], 1.0 / 64, None, ALU.mult), reads=[Bs16], writes=[Bs16])
                P.op("dve", lambda e: e.tensor_tensor(s16[:, 2, :], s16[:, 0, :], s16[:, 0, :], ALU.mult), reads=[Bs16], writes=[Bs16])
                P.op("dve", lambda e: e.scalar_tensor_tensor(s16[:, 3, :], s16[:, 1, :], 1.0 / 64, s16[:, 2, :], ALU.mult, ALU.subtract), reads=[Bs16], writes=[Bs16])
                P.op("act", lambda e: e.activation(s16[:, 4, :], s16[:, 3, :], AF.Ln, bias=LNX_EPS, scale=1.0), reads=[Bs16], writes=[Bs16])
                P.op("act", lambda e: e.activation(s16[:, 5, :], s16[:, 4, :], AF.Exp, scale=-0.5), reads=[Bs16], writes=[Bs16])
                P.op("dve", lambda e: e.tensor_tensor(v3(y), v3(y), s16[:, 0, :].to_broadcast([128, NHh, 64]) if False else s16[:, 0:1, :].rearrange("p o h -> p h o").to_broadcast([128, NHh, 64]), ALU.subtract), reads=[By_, Bs16], writes=[By_])
                P.op("dve", lambda e: e.tensor_tensor(v3(y), v3(y), s16[:, 5:6, :].rearrange("p o h -> p h o").to_broadcast([128, NHh, 64]), ALU.mult), reads=[By_, Bs16], writes=[By_])
                P.op("pool", lambda e: e.tensor_tensor(y[:, :], y[:, :], vb[0][:, :], ALU.mult), reads=[By_, Bvb[0]], writes=[By_])
                P.op("pool", lambda e: e.tensor_tensor(y[:, :], y[:, :], vb[1][:, :], ALU.add), reads=[By_, Bvb[1]], writes=[By_])
                kd = tin["kdf"]
                P.op("dve", lambda e: e.tensor_tensor(kd[:, :], kd[:, :], tin["kdb"][:, :], ALU.add), reads=[Bin["kdf"], Bin["kdb"]], writes=[Bin["kdf"]])
                P.op("pool", lambda e: e.tensor_tensor(kd[:, :], kd[:, :], vb[2][:, :], ALU.mult), reads=[Bin["kdf"], Bvb[2]], writes=[Bin["kdf"]])
                P.op("dve", lambda e: e.tensor_tensor(kd[:, :], kd[:, :], tin["r"][:, :], ALU.mult), reads=[Bin["kdf"], Bin["r"]], writes=[Bin["kdf"]])
                P.op("dve", lambda e: e.tensor_reduce(s16[:, 6, :], v3(kd), AX.X, ALU.add), reads=[Bin["kdf"]], writes=[Bs16])
                P.op("dve", lambda e: e.tensor_tensor(v3(tmp), v3(tin["v"]), s16[:, 6:7, :].rearrange("p o h -> p h o").to_broadcast([128, NHh, 64]), ALU.mult), reads=[Bin["v"], Bs16], writes=[Btmp])
                P.op("dve", lambda e: e.tensor_tensor(y[:, :], y[:, :], tmp[:, :], ALU.add), reads=[By_, Btmp], writes=[By_])
                P.op("dve", lambda e: e.tensor_tensor(obf[:, :], y[:, :], tin["g"][:, :], ALU.mult), reads=[By_, Bin["g"]], writes=[Bobf])
                transpose_tile(P, obf, Bobf, ident, Bid, pt[ptc % 2], Bpt[ptc % 2], oT, BoT, 0); ptc += 1
                res = tin["h1"]; Bres = Bin["h1"]
                for nh in range(2):
                    for c in range(8):
                        P.op("pe", lambda e, c=c, nh=nh: e.matmul(pm[nh][:, :], oT[:, c, :], wo[:, c, nh * 512:(nh + 1) * 512], start=(c == 0), stop=(c == 7)), reads=[BoT, Bwo], writes=[Bpm[nh]])
                    P.op("dve", lambda e, nh=nh: e.scalar_tensor_tensor(res[:, nh * 512:(nh + 1) * 512], res[:, nh * 512:(nh + 1) * 512], ALPHA, pm[nh][:, :], ALU.mult, ALU.add), reads=[Bres, Bpm[nh]], writes=[Bres])
                layer_norm(P, res, Bres, vb[3], Bvb[3], vb[4], Bvb[4], (st, mv), Bscr, res, Bres, hbf, Bhbf)
                transpose_tile(P, hbf, Bhbf, ident, Bid, pt[ptc % 2], Bpt[ptc % 2], hT, BhT, j); ptc += 1
                P.op("act", lambda e, j=j: e.activation(yacc[:, j, :], res[:, :], AF.Identity, scale=ALPHA), reads=[Bres], writes=[Byacc[j]])
                for e_ in range(E):
                    P.op("pool" if e_ % 2 else "dve", lambda e, e_=e_: e.tensor_tensor(tmp[:, :], res[:, :], wrb[:, e_, :], ALU.mult), reads=[Bres, Bwrb], writes=[Btmp])
                    P.op("dve", lambda e, e_=e_: e.reduce_sum(lg[:, 0, e_:e_ + 1], tmp[:, :], AX.X), reads=[Btmp], writes=[Blg])
                P.op("dve", lambda e: e.tensor_tensor(lg[:, 0, :], lg[:, 0, :], brb[:, :], ALU.add), reads=[Blg, Bbrb], writes=[Blg])
                P.op("dve", lambda e: e.reduce_max(lg[:, 1, 0:1], lg[:, 0, :], AX.X), reads=[Blg], writes=[Blg])
                P.op("dve", lambda e: e.tensor_scalar(lg[:, 2, :], lg[:, 0, :], lg[:, 1, 0:1], -1e30, ALU.is_equal, ALU.mult), reads=[Blg], writes=[Blg])
                P.op("dve", lambda e: e.tensor_tensor(lg[:, 2, :], lg[:, 2, :], lg[:, 0, :], ALU.add), reads=[Blg], writes=[Blg])
                P.op("dve", lambda e: e.reduce_max(lg[:, 1, 1:2], lg[:, 2, :], AX.X), reads=[Blg], writes=[Blg])
                P.op("dve", lambda e: e.tensor_scalar(lg[:, 2, :], lg[:, 0, :], lg[:, 1, 1:2], None, ALU.is_ge), reads=[Blg], writes=[Blg])
                P.op("dve", lambda e: e.tensor_scalar(lg[:, 3, :], lg[:, 0, :], lg[:, 1, 0:1], None, ALU.subtract), reads=[Blg], writes=[Blg])
                P.op("act", lambda e: e.activation(lg[:, 3, :], lg[:, 3, :], AF.Exp), reads=[Blg], writes=[Blg])
                P.op("dve", lambda e: e.tensor_tensor(lg[:, 3, :], lg[:, 3, :], lg[:, 2, :], ALU.mult), reads=[Blg], writes=[Blg])
                P.op("dve", lambda e: e.reduce_sum(lg[:, 1, 2:3], lg[:, 3, :], AX.X), reads=[Blg], writes=[Blg])
                P.op("dve", lambda e: e.reciprocal(lg[:, 1, 3:4], lg[:, 1, 2:3]), reads=[Blg], writes=[Blg])
                P.op("dve", lambda e, j=j: e.tensor_scalar(gates[:, j, :], lg[:, 3, :], lg[:, 1, 3:4], None, ALU.mult), reads=[Blg], writes=[Bgates])
        P.barrier()
        nT = len(half) * 128
        _sc = P.scope(); _sc.__enter__()
        wgc = [P.sb("wgc%d" % i, [128, 8, FW], BF16) for i in range(2)]; wuc = [P.sb("wuc%d" % i, [128, 8, FW], BF16) for i in range(2)]
        wdc = [P.sb("wdc%d" % i, [128, NFC, D], BF16) for i in range(2)]
        Bwc = [Buf("wc%d" % i) for i in range(2)]
        hid = [P.sb("hid%d" % i, [128, NFC, 512], BF16) for i in range(2)]; Bhid = [Buf("hid") for i in range(2)]
        sg = [P.sb("sg%d" % i, [128, 512], F32) for i in range(2)]; Bsg = [Buf("sg") for i in range(2)]
        tgroups = [(a, min(512, nT - a)) for a in range(0, nT, 512)]
        for e_ in range(E):
            for fg in range(FE // FW):
                k = wcc % 2; wcc += 1
                f0 = fg * FW
                for c in range(8):
                    P.dma("pool", wgc[k][:, c, :], wg_d[e_, c * 128:(c + 1) * 128, f0:f0 + FW], None, writes=[Bwc[k]])
                    P.dma("pool", wuc[k][:, c, :], wu_d[e_, c * 128:(c + 1) * 128, f0:f0 + FW], None, writes=[Bwc[k]])
                for fc in range(NFC):
                    P.dma("pool", wdc[k][:, fc, :], wd_d[e_, f0 + fc * 128:f0 + (fc + 1) * 128, :], None, writes=[Bwc[k]])
                for (a, w) in tgroups:
                    hk = hc % 2; hc += 1
                    for fc in range(NFC):
                        for c in range(8):
                            P.op("pe", lambda e, c=c, fc=fc: e.matmul(pg[hk][:, 0:w], wgc[k][:, c, fc * 128:(fc + 1) * 128], hT[:, c, a:a + w], start=(c == 0), stop=(c == 7)), reads=[Bwc[k], BhT], writes=[Bpg[hk]])
                        for c in range(8):
                            P.op("pe", lambda e, c=c, fc=fc: e.matmul(pu[hk][:, 0:w], wuc[k][:, c, fc * 128:(fc + 1) * 128], hT[:, c, a:a + w], start=(c == 0), stop=(c == 7)), reads=[Bwc[k], BhT], writes=[Bpu[hk]])
                        P.op("act", lambda e: e.activation(sg[hk][:, 0:w], pg[hk][:, 0:w], AF.Silu), reads=[Bpg[hk]], writes=[Bsg[hk]])
                        P.op("dve", lambda e, fc=fc: e.tensor_tensor(hid[hk][:, fc, 0:w], sg[hk][:, 0:w], pu[hk][:, 0:w], ALU.mult), reads=[Bsg[hk], Bpu[hk]], writes=[Bhid[hk]])
                    for jj in range(w // 128):
                        j = a // 128 + jj
                        for nh in range(2):
                            for fc in range(NFC):
                                P.op("pe", lambda e, fc=fc, nh=nh, jj=jj: e.matmul(pm[nh][:, :], hid[hk][:, fc, jj * 128:(jj + 1) * 128], wdc[k][:, fc, nh * 512:(nh + 1) * 512], start=(fc == 0), stop=(fc == NFC - 1)), reads=[Bhid[hk], Bwc[k]], writes=[Bpm[nh]])
                            P.op("dve" if nh else "pool", lambda e, nh=nh, j=j: e.scalar_tensor_tensor(yacc[:, j, nh * 512:(nh + 1) * 512], pm[nh][:, :], gates[:, j, e_:e_ + 1], yacc[:, j, nh * 512:(nh + 1) * 512], ALU.mult, ALU.add) if nh else e.tensor_copy(sg[hk][:, :], sg[hk][:, :]), reads=[Bpm[nh], Bgates, Byacc[j]], writes=[Byacc[j]]) if False else \
                            P.op("dve", lambda e, nh=nh, j=j: e.scalar_tensor_tensor(yacc[:, j, nh * 512:(nh + 1) * 512], pm[nh][:, :], gates[:, j, e_:e_ + 1], yacc[:, j, nh * 512:(nh + 1) * 512], ALU.mult, ALU.add), reads=[Bpm[nh], Bgates, Byacc[j]], writes=[Byacc[j]])
        P.barrier()
        _sc.__exit__(None, None, None)
        for j, t in enumerate(half):
            layer_norm(P, yacc[:, j, :], Byacc[j], vb[5], Bvb[5], vb[6], Bvb[6], (st, mv), Bscr, outt, Boutt)
            P.dma("sp", out_d[t * 128:(t + 1) * 128, :], outt[:], outsem, reads=[Boutt], writes=[Bout_d])
        P.barrier()
    P.finish([Bout_d])
    print("phaseD insts", P.ninst)
    return P.close()


_CACHE = {}

def _run(nc, maps):
    res = run_bass_kernel_spmd(nc, maps, core_ids=list(range(8)))
    return res.results

def kernel(**inp):
    f32 = np.float32
    g = lambda k: np.asarray(inp[k], dtype=f32)
    x = g("x"); meta = g("meta")
    Bn, S, D = x.shape
    NM = meta.shape[0]; L = S + NM
    ident = np.eye(128, dtype=f32)
    nxt = S // 128
    w_in = g("attn_w_in")[0]
    lamv = np.stack([g("attn_lam_q1")[0], g("attn_lam_k1")[0], g("attn_lam_q2")[0], g("attn_lam_k2")[0]])
    subg = g("attn_subln_g")[0][None, :]
    ncA, mults = build_phaseA(2, nxt)
    maps = []
    for c in range(8):
        b = c // 4; heads = [2 * (c % 4), 2 * (c % 4) + 1]
        xp = np.concatenate([x[b], meta, np.zeros((128 - NM, D), f32)], 0)
        qaug, kaug = attn_consts(heads, nxt)
        tab = np.zeros((2, 1, 1024), f32)
        for hi, h in enumerate(heads):
            sl = 2.0 ** (-(h + 1))
            for m, k in mults[hi].items():
                tab[hi, 0, k] = sl * m
        hw = lambda off: np.ascontiguousarray(np.stack([w_in[:, off + h * 128: off + (h + 1) * 128] for h in heads]))
        maps.append(dict(xp=xp, wq=hw(0), wk=hw(D), wv=hw(2 * D), lamv=lamv, subg=subg, qaug=qaug, kaug=kaug, ctab=tab, ident=ident))
    resA = _run(ncA, maps)
    o_full = np.zeros((Bn, (nxt + 1) * 128, D), f32)
    for c in range(8):
        b = c // 4; h0 = 2 * (c % 4)
        o_full[b][:, h0 * 128:(h0 + 2) * 128] = resA[c]["o"]
    del resA, maps
    TQ = S // 4; MQ = NM // 4
    ntB = TQ // 128 + 1
    ncB = build_phaseB(ntB)
    maps = []
    for c in range(8):
        b = c // 4; q = c % 4
        o_rows = np.zeros((ntB * 128, D), f32); h_rows = np.zeros((ntB * 128, D), f32)
        o_rows[:TQ] = o_full[b][q * TQ:(q + 1) * TQ]; o_rows[TQ:TQ + MQ] = o_full[b][S + q * MQ:S + (q + 1) * MQ]
        h_rows[:TQ] = x[b][q * TQ:(q + 1) * TQ]; h_rows[TQ:TQ + MQ] = meta[q * MQ:(q + 1) * MQ]
        maps.append(dict(o=o_rows, h0=h_rows, wo=g("attn_w_o")[0], wg=g("ffn_w_gate")[0], wu=g("ffn_w_up")[0], wd=g("ffn_w_down")[0],
                         lng=g("ln_g")[0], lnb=g("ln_b")[0], ident=ident))
    resB = _run(ncB, maps)
    seq = np.zeros((Bn, L, D), f32)
    for c in range(8):
        b = c // 4; q = c % 4
        seq[b][NM + q * TQ:NM + (q + 1) * TQ] = resB[c]["h1"][:TQ]
        seq[b][q * MQ:(q + 1) * MQ] = resB[c]["h1"][TQ:TQ + MQ]
    del resB, maps, o_full
    nch = (L + 63) // 64; Tp = nch * 64
    ncC = build_phaseC(nch, 8)
    mu = g("rw_mu")[0]
    seg = np.ones((1, 512), f32); seg[0, ::64] = 0
    maps = []
    for c in range(8):
        b = c // 4; d = (c % 4) // 2; hg = c % 2
        cs = slice(hg * 512, (hg + 1) * 512)
        sq = seq[b] if d == 0 else seq[b][::-1]
        xpad = np.zeros((Tp + 2, D), f32); xpad[1:1 + L] = sq
        mu_in = np.concatenate([mu[d], mu[1 - d]], 0)
        chv = np.zeros((64, 8, 8), f32)
        for i, v in enumerate((g("rw_w0")[0][d], g("rw_a0")[0][d], g("rw_k_k")[0], g("rw_k_a")[0])):
            chv[:, :, i] = v[cs].reshape(8, 64).T
        wrkv = g("rw_w_rkv")[0]
        maps.append(dict(xT=np.ascontiguousarray(xpad.T), mu=np.ascontiguousarray(mu_in.T),
                         wr=np.ascontiguousarray(wrkv[0][:, cs]), wk=np.ascontiguousarray(wrkv[1][:, cs]), wv=np.ascontiguousarray(wrkv[2][:, cs]),
                         w1=g("rw_w1")[0][d], w2=np.ascontiguousarray(g("rw_w2")[0][d][:, cs]),
                         a1=g("rw_a1")[0][d], a2=np.ascontiguousarray(g("rw_a2")[0][d][:, cs]),
                         g1=g("rw_g1")[0], g2=np.ascontiguousarray(g("rw_g2")[0][:, cs]),
                         chv=chv, msk=rw_consts(), seg=seg, ident=ident))
    resC = _run(ncC, maps)
    Y = np.zeros((Bn, 2, L, D), f32); AUX = np.zeros((Bn, 2, 4, L, D), f32)
    for c in range(8):
        b = c // 4; d = (c % 4) // 2; hg = c % 2
        cs = slice(hg * 512, (hg + 1) * 512)
        yy = resC[c]["y"][:L]; ax = resC[c]["aux"][:, :, :L]
        if d == 1:
            yy = yy[::-1]; ax = ax[:, :, ::-1]
        Y[b, d][:, cs] = yy
        for i in range(4):
            AUX[b, d, i][:, cs] = ax[i].T
    del resC, maps
    ncD = build_phaseD(TQ // 128)
    vecs = np.stack([g("rw_lnx_g")[0], g("rw_lnx_b")[0], g("rw_r_k")[0].reshape(D), g("ln_g")[1, 0], g("ln_b")[1, 0], g("ln_g")[1, 1], g("ln_b")[1, 1]])
    wrt = np.ascontiguousarray(g("moe_w_router")[0].T); brt = g("moe_b_router")[0][None, :]
    wgE = g("moe_w_gate")[0]; wuE = g("moe_w_up")[0]; wdE = g("moe_w_down")[0]; woR = g("rw_w_o")[0]
    maps = []
    for c in range(8):
        b = c // 4; q = c % 4
        rows = slice(NM + q * TQ, NM + (q + 1) * TQ)
        cp = lambda a: np.ascontiguousarray(a[rows])
        maps.append(dict(yf=cp(Y[b, 0]), yb=cp(Y[b, 1]), r=cp(AUX[b, 0, 0]), kdf=cp(AUX[b, 0, 1]), kdb=cp(AUX[b, 1, 1]), v=cp(AUX[b, 0, 2]), g=cp(AUX[b, 0, 3]),
                         h1=cp(seq[b]), wo=woR, vecs=vecs, wrt=wrt, brt=brt, wg=wgE, wu=wuE, wd=wdE, ident=ident))
    resD = _run(ncD, maps)
    out = np.zeros((Bn, S, D), f32)
    for c in range(8):
        b = c // 4; q = c % 4
        out[b, q * TQ:(q + 1) * TQ] = resD[c]["out"]
    return out
```

```python
import numpy as np
from contextlib import ExitStack
import concourse.bass as bass
import concourse.mybir as mybir
from concourse.bass_utils import run_bass_kernel_spmd

F32 = mybir.dt.float32
BF16 = mybir.dt.bfloat16
ALU = mybir.AluOpType
AF = mybir.ActivationFunctionType
AX = mybir.AxisListType


NUM_DEVICES = None


class Sem:
    def __init__(self, h, is_dma=False):
        self.h = h
        self.cnt = 0
        self.is_dma = is_dma


class Buf:
    def __init__(self, name):
        self.name = name
        self.w = None
        self.r = []


class Prog:
    ENG = ("pe", "act", "dve", "pool", "sp")

    def __init__(self):
        self.nc = bass.Bass("TRN2", target_bir_lowering=False, num_devices=NUM_DEVICES)
        self.es = ExitStack()
        nc = self.nc
        self.eng = {"pe": nc.tensor, "act": nc.scalar, "dve": nc.vector, "pool": nc.gpsimd, "sp": nc.sync}
        self.esem = {e: Sem(self.es.enter_context(nc.semaphore("sem_" + e))) for e in self.ENG}
        self.waited = {e: {} for e in self.ENG}
        self.nsem = 0
        self.dsems = []
        self.root_es = self.es
        self.ninst = 0

    def dram(self, name, shape, dt, kind):
        return self.nc.dram_tensor(name, list(shape), dt, kind=kind).ap()

    def _uniq(self, name):
        self.nalloc = getattr(self, "nalloc", 0) + 1
        return "%s_u%d" % (name, self.nalloc)

    def sb(self, name, shape, dt):
        return self.es.enter_context(self.nc.sbuf_tensor(self._uniq(name), list(shape), dt))

    def ps(self, name, shape, dt):
        return self.es.enter_context(self.nc.psum_tensor(self._uniq(name), list(shape), dt))

    def newsem(self, name=None):
        self.nsem += 1
        sm = self._mksem(name)
        sm.is_dma = True
        self.dsems.append(sm)
        return sm

    def _mksem(self, name):
        return Sem(self.root_es.enter_context(self.nc.semaphore(name or ("ds%d" % self.nsem))))

    def _wait(self, e, ev, raw=True):
        sem, val = ev
        if sem is self.esem[e] and (e == "pe" or not raw):
            return
        if sem.is_dma:
            val = sem.cnt
        if self.waited[e].get(sem, 0) >= val:
            return
        self.waited[e][sem] = val
        self.eng[e].wait_ge(sem.h, val)

    def _deps(self, e, reads, writes):
        for b in reads:
            if b.w is not None:
                self._wait(e, b.w)
        for b in writes:
            if b.w is not None:
                self._wait(e, b.w, raw=False)
            for ev in b.r:
                self._wait(e, ev, raw=False)

    def _commit(self, ev, reads, writes):
        for b in reads:
            b.r.append(ev)
            if len(b.r) > 64:
                b.r = b.r[-64:]
        for b in writes:
            b.w = ev
            b.r = []

    def op(self, e, fn, reads=(), writes=(), accum=False):
        self._deps(e, reads, writes)
        s = self.esem[e]
        inst = fn(self.eng[e])
        s.cnt += 1
        inst.then_inc(s.h, 1)
        self._commit((s, s.cnt), reads, writes)
        self.ninst += 1
        return inst

    def dma(self, q, out, in_, sem, reads=(), writes=(), **kw):
        if sem is None:
            b0 = writes[0]
            if getattr(b0, "dsem", None) is None:
                b0.dsem = self.newsem()
            sem = b0.dsem
        self._deps(q, reads, writes)
        inst = self.eng[q].dma_start(out=out, in_=in_, **kw)
        sem.cnt += 16
        inst.then_inc(sem.h, 16)
        self._commit((sem, sem.cnt), reads, writes)
        self.ninst += 1
        return inst

    def barrier(self):
        evs = [(s, s.cnt) for s in list(self.esem.values()) + self.dsems if s.cnt > 0]
        for e in self.ENG:
            for ev in evs:
                self._wait(e, ev)

    def scope(self):
        prog = self
        class _S:
            def __enter__(s2):
                s2.old = prog.es
                prog.es = ExitStack()
                return prog
            def __exit__(s2, *a):
                prog.es.close()
                prog.es = s2.old
                return False
        return _S()

    def finish(self, bufs, e="sp"):
        for b in bufs:
            if b.w is not None:
                self._wait(e, b.w)
            for ev in b.r:
                self._wait(e, ev)

    def close(self):
        self.es.close()
        return self.nc

import math
ALPHA = 4.0 ** 0.25
LN_EPS = 1e-5

class Common:
    def __init__(self, P):
        self.P = P

def load_w_bf16(P, name, w_ap, K, N, sem):
    kc = K // 128
    t = P.sb(name, [128, kc, N], BF16)
    B = Buf(name)
    for c in range(kc):
        P.dma("pool", t[:, c, :], w_ap[c * 128:(c + 1) * 128, :], sem, writes=[B])
    return t, B

def load_bcast(P, name, v_ap, N, sem):
    t = P.sb(name, [128, N], F32)
    B = Buf(name)
    P.dma("sp", t[:], v_ap.to_broadcast([128, N]), sem, writes=[B])
    return t, B

def layer_norm(P, x, Bx, g, Bg, b, Bb, scr, Bscr, out, Bout, obf=None, Bobf=None, D=1024):
    nh = D // 512
    st, mv = scr
    for i in range(nh):
        P.op("dve", lambda e, i=i: e.bn_stats(st[:, i * 6:(i + 1) * 6], x[:, i * 512:(i + 1) * 512]), reads=[Bx], writes=[Bscr])
    P.op("dve", lambda e: e.bn_aggr(mv[:, 0:2], st[:, 0:6 * nh]), reads=[Bscr], writes=[Bscr])
    P.op("act", lambda e: e.activation(mv[:, 2:3], mv[:, 1:2], AF.Ln, bias=LN_EPS, scale=1.0), reads=[Bscr], writes=[Bscr])
    P.op("act", lambda e: e.activation(mv[:, 3:4], mv[:, 2:3], AF.Exp, scale=-0.5), reads=[Bscr], writes=[Bscr])
    P.op("dve", lambda e: e.tensor_scalar(out[:, :], x[:, :], mv[:, 0:1], mv[:, 3:4], ALU.subtract, ALU.mult), reads=[Bx, Bscr], writes=[Bout])
    P.op("pool", lambda e: e.tensor_tensor(out[:, :], out[:, :], g[:, :], ALU.mult), reads=[Bout, Bg], writes=[Bout])
    P.op("pool", lambda e: e.tensor_tensor(out[:, :], out[:, :], b[:, :], ALU.add), reads=[Bout, Bb], writes=[Bout])
    if obf is not None:
        P.op("act", lambda e: e.activation(obf[:, :], out[:, :], AF.Identity), reads=[Bout], writes=[Bobf])

def transpose_tile(P, src_bf, Bsrc, ident, Bid, pt, Bpt, dst, Bdst, tslot, nchunk=8, evac="act"):
    for c in range(nchunk):
        P.op("pe", lambda e, c=c: e.transpose(pt[:, c, :], src_bf[:, c * 128:(c + 1) * 128], ident[:, :]), reads=[Bsrc, Bid], writes=[Bpt])
    if evac == "act":
        P.op("act", lambda e: e.activation(dst[:, 0:nchunk, tslot * 128:(tslot + 1) * 128], pt[:, 0:nchunk, :], AF.Identity), reads=[Bpt], writes=[Bdst])
    else:
        P.op("dve", lambda e: e.tensor_copy(dst[:, 0:nchunk, tslot * 128:(tslot + 1) * 128], pt[:, 0:nchunk, :]), reads=[Bpt], writes=[Bdst])

def build_phaseB(ntiles, F=2816, D=1024):
    P = Prog(); nc = P.nc
    T = ntiles * 128
    FC = F // 128
    o_d = P.dram("o", [T, D], F32, "ExternalInput")
    h0_d = P.dram("h0", [T, D], F32, "ExternalInput")
    wo_d = P.dram("wo", [D, D], F32, "ExternalInput")
    wg_d = P.dram("wg", [D, F], F32, "ExternalInput")
    wu_d = P.dram("wu", [D, F], F32, "ExternalInput")
    wd_d = P.dram("wd", [F, D], F32, "ExternalInput")
    lng_d = P.dram("lng", [2, D], F32, "ExternalInput")
    lnb_d = P.dram("lnb", [2, D], F32, "ExternalInput")
    id_d = P.dram("ident", [128, 128], F32, "ExternalInput")
    out_d = P.dram("h1", [T, D], F32, "ExternalOutput")
    wsem = P.newsem("wsem")
    wo, Bwo = load_w_bf16(P, "wo_sb", wo_d, D, D, wsem)
    wg, Bwg = load_w_bf16(P, "wg_sb", wg_d, D, F, wsem)
    wu, Bwu = load_w_bf16(P, "wu_sb", wu_d, D, F, wsem)
    wd, Bwd = load_w_bf16(P, "wd_sb", wd_d, F, D, wsem)
    csem = P.newsem("csem")
    g1, Bg1 = load_bcast(P, "g1", lng_d[0:1, :], D, csem)
    b1, Bb1 = load_bcast(P, "b1", lnb_d[0:1, :], D, csem)
    g2, Bg2 = load_bcast(P, "g2", lng_d[1:2, :], D, csem)
    b2, Bb2 = load_bcast(P, "b2", lnb_d[1:2, :], D, csem)
    ident = P.sb("ident_sb", [128, 128], BF16); Bid = Buf("ident")
    P.dma("pool", ident[:], id_d[:, :], csem, writes=[Bid])
    GT = 2
    W = GT * 128
    o32 = P.sb("o32", [128, D], F32); Bo32 = Buf("o32"); o32sem = P.newsem()
    obf = P.sb("obf", [128, D], BF16); Bobf = Buf("obf")
    res = [P.sb("res%d" % i, [128, D], F32) for i in range(GT)]; Bres = [Buf("res%d" % i) for i in range(GT)]
    ressem = [P.newsem() for i in range(GT)]
    hbf = P.sb("hbf", [128, D], BF16); Bhbf = Buf("hbf")
    xT = P.sb("xT", [128, 8, W], BF16); BxT = Buf("xT")
    hidT = P.sb("hidT", [128, FC, W], BF16); BhidT = Buf("hidT")
    sg = [P.sb("sg%d" % i, [128, W], F32) for i in range(2)]; Bsg = [Buf("sg%d" % i) for i in range(2)]
    st = P.sb("lnst", [128, 12], F32); mv = P.sb("lnmv", [128, 4], F32); Bscr = Buf("lnscr")
    outt = P.sb("outt", [128, D], F32); Boutt = Buf("outt"); outsem = P.newsem()
    pt = [P.ps("pt%d" % i, [128, 8, 128], BF16) for i in range(2)]; Bpt = [Buf("pt%d" % i) for i in range(2)]
    pm = [P.ps("pm%d" % i, [128, 512], F32) for i in range(2)]; Bpm = [Buf("pm%d" % i) for i in range(2)]
    pg = [P.ps("pg%d" % i, [128, 512], F32) for i in range(2)]; Bpg = [Buf("pg%d" % i) for i in range(2)]
    pu = [P.ps("pu%d" % i, [128, 512], F32) for i in range(2)]; Bpu = [Buf("pu%d" % i) for i in range(2)]
    Bout_d = Buf("out_d")
    ptc = 0
    ngroups = (ntiles + GT - 1) // GT
    for gi in range(ngroups):
        tiles = list(range(gi * GT, min(ntiles, (gi + 1) * GT)))
        w = len(tiles) * 128
        for j, t in enumerate(tiles):
            P.dma("sp", o32[:], o_d[t * 128:(t + 1) * 128, :], o32sem, writes=[Bo32])
            P.dma("sp", res[j][:], h0_d[t * 128:(t + 1) * 128, :], ressem[j], writes=[Bres[j]])
            P.op("act", lambda e: e.activation(obf[:, :], o32[:, :], AF.Identity), reads=[Bo32], writes=[Bobf])
            transpose_tile(P, obf, Bobf, ident, Bid, pt[ptc % 2], Bpt[ptc % 2], xT, BxT, j); ptc += 1
        for j, t in enumerate(tiles):
            for nh in range(2):
                for c in range(8):
                    P.op("pe", lambda e, c=c, nh=nh, j=j: e.matmul(pm[nh][:, :], xT[:, c, j * 128:(j + 1) * 128], wo[:, c, nh * 512:(nh + 1) * 512], start=(c == 0), stop=(c == 7)),
                         reads=[BxT, Bwo], writes=[Bpm[nh]])
                P.op("dve", lambda e, nh=nh, j=j: e.scalar_tensor_tensor(res[j][:, nh * 512:(nh + 1) * 512], res[j][:, nh * 512:(nh + 1) * 512], ALPHA, pm[nh][:, :], ALU.mult, ALU.add),
                     reads=[Bres[j], Bpm[nh]], writes=[Bres[j]])
            layer_norm(P, res[j], Bres[j], g1, Bg1, b1, Bb1, (st, mv), Bscr, res[j], Bres[j], hbf, Bhbf)
            transpose_tile(P, hbf, Bhbf, ident, Bid, pt[ptc % 2], Bpt[ptc % 2], xT, BxT, j); ptc += 1
        for fc in range(FC):
            k = fc % 2
            for c in range(8):
                P.op("pe", lambda e, c=c, fc=fc, k=k: e.matmul(pg[k][:, 0:w], wg[:, c, fc * 128:(fc + 1) * 128], xT[:, c, 0:w], start=(c == 0), stop=(c == 7)),
                     reads=[BxT, Bwg], writes=[Bpg[k]])
            for c in range(8):
                P.op("pe", lambda e, c=c, fc=fc, k=k: e.matmul(pu[k][:, 0:w], wu[:, c, fc * 128:(fc + 1) * 128], xT[:, c, 0:w], start=(c == 0), stop=(c == 7)),
                     reads=[BxT, Bwu], writes=[Bpu[k]])
            P.op("act", lambda e, k=k: e.activation(sg[k][:, 0:w], pg[k][:, 0:w], AF.Silu), reads=[Bpg[k]], writes=[Bsg[k]])
            P.op("dve", lambda e, k=k, fc=fc: e.tensor_tensor(hidT[:, fc, 0:w], sg[k][:, 0:w], pu[k][:, 0:w], ALU.mult), reads=[Bsg[k], Bpu[k]], writes=[BhidT])
        for j, t in enumerate(tiles):
            for nh in range(2):
                for fc in range(FC):
                    P.op("pe", lambda e, fc=fc, nh=nh, j=j: e.matmul(pm[nh][:, :], hidT[:, fc, j * 128:(j + 1) * 128], wd[:, fc, nh * 512:(nh + 1) * 512], start=(fc == 0), stop=(fc == FC - 1)),
                         reads=[BhidT, Bwd], writes=[Bpm[nh]])
                P.op("dve", lambda e, nh=nh, j=j: e.scalar_tensor_tensor(res[j][:, nh * 512:(nh + 1) * 512], res[j][:, nh * 512:(nh + 1) * 512], ALPHA, pm[nh][:, :], ALU.mult, ALU.add),
                     reads=[Bres[j], Bpm[nh]], writes=[Bres[j]])
            layer_norm(P, res[j], Bres[j], g2, Bg2, b2, Bb2, (st, mv), Bscr, outt, Boutt)
            P.dma("sp", out_d[t * 128:(t + 1) * 128, :], outt[:], outsem, reads=[Boutt], writes=[Bout_d])
    P.finish([Bout_d])
    print("phaseB insts", P.ninst)
    return P.close()


import math
SUBLN_EPS = 1e-5
NEG = -30000.0

def attn_consts(heads, nxt):
    T = (nxt + 1) * 128
    qaug = np.zeros((2, 5, 512), np.float32)
    u = np.arange(512)
    qaug[0] = np.stack([u // 16, u % 16, np.ones(512), np.ones(512), np.ones(512)])
    qaug[1] = np.stack([-(u // 16), -(u % 16), -np.ones(512), -np.ones(512), np.ones(512)])
    kaug = np.zeros((len(heads), 5, T), np.float32)
    v = np.arange(T) % 128
    for i, h in enumerate(heads):
        sl = 2.0 ** (-(h + 1))
        kaug[i, 0] = -16 * sl
        kaug[i, 1] = -sl
        kaug[i, 2] = 16 * sl * (v // 16)
        kaug[i, 3] = sl * (v % 16)
        kaug[i, 4, nxt * 128 + 16:] = NEG
    return qaug, kaug

def build_phaseA(NH, nxt, NCT=1024, D=1024):
    P = Prog(); nc = P.nc
    NT = nxt + 1
    T = NT * 128
    x_d = P.dram("xp", [T, D], F32, "ExternalInput")
    wq_d = P.dram("wq", [NH, D, 128], F32, "ExternalInput")
    wk_d = P.dram("wk", [NH, D, 128], F32, "ExternalInput")
    wv_d = P.dram("wv", [NH, D, 128], F32, "ExternalInput")
    lam_d = P.dram("lamv", [4, 64], F32, "ExternalInput")
    sg_d = P.dram("subg", [1, 128], F32, "ExternalInput")
    qaug_d = P.dram("qaug", [2, 5, 512], F32, "ExternalInput")
    kaug_d = P.dram("kaug", [NH, 5, T], F32, "ExternalInput")
    ctab_d = P.dram("ctab", [NH, 1, NCT], F32, "ExternalInput")
    id_d = P.dram("ident", [128, 128], F32, "ExternalInput")
    o_d = P.dram("o", [T, NH * 128], F32, "ExternalOutput")
    Bout_d = Buf("o_d")
    csem = None
    ident = P.sb("ident_sb", [128, 128], BF16); Bid = Buf("ident")
    P.dma("pool", ident[:], id_d[:, :], csem, writes=[Bid])
    lv = P.sb("lv", [128, 4, 64], F32); Blv = Buf("lv")
    for i in range(4):
        P.dma("sp", lv[:, i, :], lam_d[i:i + 1, :].to_broadcast([128, 64]), csem, writes=[Blv])
    lsc = P.sb("lsc", [128, 8], F32); Blsc = Buf("lsc")
    lt = P.sb("lt", [128, 2, 64], F32)
    P.op("dve", lambda e: e.tensor_tensor(lt[:, 0, :], lv[:, 0, :], lv[:, 1, :], ALU.mult), reads=[Blv], writes=[Blsc])
    P.op("dve", lambda e: e.tensor_tensor(lt[:, 1, :], lv[:, 2, :], lv[:, 3, :], ALU.mult), reads=[Blv], writes=[Blsc])
    P.op("dve", lambda e: e.reduce_sum(lsc[:, 0:1], lt[:, 0, :], AX.X), reads=[Blsc], writes=[Blsc])
    P.op("dve", lambda e: e.reduce_sum(lsc[:, 1:2], lt[:, 1, :], AX.X), reads=[Blsc], writes=[Blsc])
    P.op("act", lambda e: e.activation(lsc[:, 2:4], lsc[:, 0:2], AF.Exp), reads=[Blsc], writes=[Blsc])
    P.op("dve", lambda e: e.tensor_tensor(lsc[:, 4:5], lsc[:, 3:4], lsc[:, 2:3], ALU.subtract), reads=[Blsc], writes=[Blsc])
    P.op("dve", lambda e: e.tensor_scalar(lsc[:, 5:6], lsc[:, 4:5], -0.2, None, ALU.add), reads=[Blsc], writes=[Blsc])
    neglam = lsc[:, 5:6]
    gsc, Bgsc = load_bcast(P, "gsc", sg_d[0:1, :], 128, csem)
    P.op("dve", lambda e: e.tensor_scalar(gsc[:, :], gsc[:, :], 0.8, None, ALU.mult), reads=[Bgsc], writes=[Bgsc])
    QT = P.sb("QT", [128, T], BF16); BQT = Buf("QT")
    KT = [P.sb("KT%d" % c, [69, T], BF16) for c in range(2)]; BKT = [Buf("KT%d" % c) for c in range(2)]
    VA = P.sb("VA", [128, NT, 129], BF16); BVA = Buf("VA")
    P.op("pool", lambda e: e.memset(VA[:, :, 128:129], 1.0), writes=[BVA])
    ctab = P.sb("ctab_sb", [128, NCT], F32); Bctab = Buf("ctab")
    QA = [[P.sb("QA%d%d" % (c, lr), [69, 512], BF16) for lr in range(2)] for c in range(2)]
    BQA = [[Buf("QA%d%d" % (c, lr)) for lr in range(2)] for c in range(2)]
    for c in range(2):
        for lr in range(2):
            P.dma("pool", QA[c][lr][64:69, :], qaug_d[lr, :, :], csem, writes=[BQA[c][lr]])
    wq = P.sb("wq_sb", [128, 8, 128], BF16); wk = P.sb("wk_sb", [128, 8, 128], BF16); wv = P.sb("wv_sb", [128, 8, 128], BF16)
    Bw = Buf("w_head"); wsem = None
    ctab_vals = [dict() for _ in range(NH)]
    def ccol(hi, val):
        d = ctab_vals[hi]
        if val not in d:
            d[val] = len(d)
            assert len(d) <= NCT
        return d[val]
    base = lambda tile: (0 if tile == nxt else 16 + 128 * tile)
    for hi in range(NH):
        for (dst, src) in ((wq, wq_d), (wk, wk_d), (wv, wv_d)):
            for c in range(8):
                P.dma("pool", dst[:, c, :], src[hi, c * 128:(c + 1) * 128, :], wsem, writes=[Bw])
        P.dma("sp", ctab[:], ctab_d[hi, 0:1, :].to_broadcast([128, NCT]), csem, writes=[Bctab])
        for c in range(2):
            P.dma("pool", KT[c][64:69, :], kaug_d[hi, :, :], csem, writes=[BKT[c]])
        P.barrier()
        with P.scope():
            x32 = [P.sb("x32_%d" % i, [128, D], F32) for i in range(2)]; Bx32 = [Buf("x32") for i in range(2)]; xsem = [P.newsem() for i in range(2)]
            xbf = [P.sb("xbf_%d" % i, [128, D], BF16) for i in range(2)]; Bxbf = [Buf("xbf") for i in range(2)]
            xT = [P.sb("xT_%d" % i, [128, 8, 512], BF16) for i in range(2)]; BxT = [Buf("xT") for i in range(2)]
            pt = [P.ps("pt%d" % i, [128, 8, 128], BF16) for i in range(2)]; Bpt = [Buf("pt") for i in range(2)]
            pq = [P.ps("pq%d" % i, [128, 512], F32) for i in range(2)]; Bpq = [Buf("pq") for i in range(2)]
            pk = [P.ps("pk%d" % i, [128, 512], F32) for i in range(2)]; Bpk = [Buf("pk") for i in range(2)]
            pv = [P.ps("pv%d" % i, [128, 512], F32) for i in range(2)]; Bpv = [Buf("pv") for i in range(2)]
            tcnt = 0
            ngr = (NT + 3) // 4
            for gi in range(ngr):
                tiles = list(range(gi * 4, min(NT, gi * 4 + 4)))
                w = len(tiles) * 128
                k2 = gi % 2
                for j, t in enumerate(tiles):
                    s = tcnt % 2; tcnt += 1
                    P.dma("sp", x32[s][:], x_d[t * 128:(t + 1) * 128, :], xsem[s], writes=[Bx32[s]])
                    P.op("dve" if j % 2 else "pool", lambda e, s=s: e.tensor_copy(xbf[s][:, :], x32[s][:, :]), reads=[Bx32[s]], writes=[Bxbf[s]])
                    transpose_tile(P, xbf[s], Bxbf[s], ident, Bid, pt[s], Bpt[s], xT[k2], BxT[k2], j, evac="act" if j % 2 else "dve")
                tok0 = tiles[0] * 128
                for c in range(8):
                    P.op("pe", lambda e, c=c: e.matmul(pq[k2][:, 0:w], wq[:, c, :], xT[k2][:, c, 0:w], start=(c == 0), stop=(c == 7)), reads=[Bw, BxT[k2]], writes=[Bpq[k2]])
                P.op("act", lambda e: e.activation(QT[:, tok0:tok0 + w], pq[k2][:, 0:w], AF.Identity, scale=0.125), reads=[Bpq[k2]], writes=[BQT])
                for cc in range(2):
                    for c in range(8):
                        P.op("pe", lambda e, c=c, cc=cc: e.matmul(pk[k2][0:64, cc * 0 + 0:w] if False else pk[k2][0:64, 0:w], wk[:, c, cc * 64:(cc + 1) * 64], xT[k2][:, c, 0:w], start=(c == 0), stop=(c == 7)), reads=[Bw, BxT[k2]], writes=[Bpk[k2]])
                    P.op("dve", lambda e, cc=cc: e.tensor_copy(KT[cc][0:64, tok0:tok0 + w], pk[k2][0:64, 0:w]), reads=[Bpk[k2]], writes=[BKT[cc]])
                for j, t in enumerate(tiles):
                    for c in range(8):
                        P.op("pe", lambda e, c=c, j=j: e.matmul(pv[k2][:, j * 128:(j + 1) * 128], xT[k2][:, c, j * 128:(j + 1) * 128], wv[:, c, :], start=(c == 0), stop=(c == 7)), reads=[Bw, BxT[k2]], writes=[Bpv[k2]])
                P.op("act", lambda e: e.activation(VA[:, tiles[0]:tiles[0] + len(tiles), 0:128], pv[k2][:, 0:w].rearrange("p (t d) -> p t d", d=128), AF.Identity), reads=[Bpv[k2]], writes=[BVA])
        P.barrier()
        with P.scope():
            psS = [[P.ps("psS%d%d" % (c, i), [128, 512], F32) for i in range(2)] for c in range(2)]
            BpsS = [[Buf("psS") for i in range(2)] for c in range(2)]
            oz = [[P.ps("oz%d%d" % (c, i), [128, 512], F32) for i in range(2)] for c in range(2)]
            Boz = [Buf("oz%d" % c) for c in range(2)]
            PT = [[P.sb("PT%d%d" % (c, i), [128, 512], BF16) for i in range(3)] for c in range(2)]
            BPT = [[Buf("PT") for i in range(3)] for c in range(2)]
            srt = [P.sb("srt%d" % c, [128, 512], F32) for c in range(2)]; Bsrt = [Buf("srt") for c in range(2)]
            fz = P.sb("fz", [128, 16], F32); Bfz = Buf("fz")
            fo = [P.sb("fo%d" % i, [128, 128], F32) for i in range(2)]; Bfo = [Buf("fo") for i in range(2)]
            fo2 = P.sb("fo2", [128, 128], F32); Bfo2 = Buf("fo2")
            osb = [P.sb("osb%d" % i, [128, 128], F32) for i in range(2)]; Bosb = [Buf("osb") for i in range(2)]; osem = [P.newsem() for i in range(2)]
            qtiles = [(j * 4, 4) for j in range(nxt // 4)] + [(nxt, 1)]
            assert nxt % 4 == 0
            it = 0; fcnt = 0
            for (qt0, qn) in qtiles:
                Wq = qn * 128
                qbase = base(qt0)
                for c in range(2):
                    for lr in range(2):
                        P.op("pool" if lr else "dve", lambda e, c=c, lr=lr: e.tensor_copy(QA[c][lr][0:64, 0:Wq], QT[c * 64:(c + 1) * 64, qt0 * 128:qt0 * 128 + Wq]), reads=[BQT], writes=[BQA[c][lr]])
                order = list(range(NT))
                def mk(ki, i, it):
                    Dq = qbase - base(i)
                    if Dq >= 127: typ = 0
                    elif Dq + Wq - 1 <= 0: typ = 1
                    else: typ = 2
                    return dict(ki=ki, i=i, it=it, Dq=Dq, typ=typ)
                def issue_S(inf):
                    i, typ, it_ = inf["i"], inf["typ"], inf["it"]
                    for c in range(2):
                        s2 = it_ % 2
                        ps = psS[c][s2]; Bps = BpsS[c][s2]
                        if typ < 2:
                            P.op("pe", lambda e, c=c, i=i, typ=typ, ps=ps: e.matmul(ps[:, 0:Wq], KT[c][0:69, i * 128:(i + 1) * 128], QA[c][typ][0:69, 0:Wq], start=True, stop=True), reads=[BKT[c], BQA[c][typ]], writes=[Bps])
                        else:
                            ps2 = psS[c][1 - s2]; Bps2 = BpsS[c][1 - s2]
                            P.op("pe", lambda e, c=c, i=i, ps=ps: e.matmul(ps[:, 0:Wq], KT[c][0:69, i * 128:(i + 1) * 128], QA[c][0][0:69, 0:Wq], start=True, stop=True), reads=[BKT[c], BQA[c][0]], writes=[Bps])
                            P.op("pe", lambda e, c=c, i=i, ps2=ps2: e.matmul(ps2[:, 0:Wq], KT[c][0:69, i * 128:(i + 1) * 128], QA[c][1][0:69, 0:Wq], start=True, stop=True), reads=[BKT[c], BQA[c][1]], writes=[Bps2])
                def issue_rest(inf):
                    i, typ, it_, Dq, ki = inf["i"], inf["typ"], inf["it"], inf["Dq"], inf["ki"]
                    for c in range(2):
                        s2 = it_ % 2; s3 = it_ % 3
                        ps = psS[c][s2]; Bps = BpsS[c][s2]
                        pt_ = PT[c][s3]; Bp = BPT[c][s3]
                        if typ < 2:
                            col = ccol(hi, -abs(Dq))
                            P.op("act", lambda e, ps=ps, pt_=pt_, col=col: e.activation(pt_[:, 0:Wq], ps[:, 0:Wq], AF.Exp, bias=ctab[:, col:col + 1], scale=1.0), reads=[Bps, Bctab], writes=[Bp])
                        else:
                            ps2 = psS[c][1 - s2]; Bps2 = BpsS[c][1 - s2]
                            colL = ccol(hi, -Dq); colR = ccol(hi, Dq)
                            P.op("act", lambda e, c=c, ps2=ps2, colR=colR: e.activation(srt[c][:, 0:Wq], ps2[:, 0:Wq], AF.Identity, bias=ctab[:, colR:colR + 1], scale=1.0), reads=[Bps2, Bctab], writes=[Bsrt[c]])
                            P.op("dve", lambda e, c=c, ps=ps, colL=colL: e.scalar_tensor_tensor(srt[c][:, 0:Wq], ps[:, 0:Wq], ctab[:, colL:colL + 1], srt[c][:, 0:Wq], ALU.add, ALU.min), reads=[Bps, Bsrt[c], Bctab], writes=[Bsrt[c]])
                            P.op("act", lambda e, c=c, pt_=pt_: e.activation(pt_[:, 0:Wq], srt[c][:, 0:Wq], AF.Exp), reads=[Bsrt[c]], writes=[Bp])
                    for c in range(2):
                        s3 = it_ % 3
                        pt_ = PT[c][s3]; Bp = BPT[c][s3]
                        for sub in range(qn):
                            P.op("pe", lambda e, c=c, sub=sub, i=i, pt_=pt_, ki=ki: e.matmul(oz[c][sub // 2][:, (sub % 2) * 129:(sub % 2) * 129 + 129], pt_[:, sub * 128:(sub + 1) * 128], VA[:, i, :], start=(ki == 0 and sub % 2 == 0), stop=(ki == NT - 1), skip_group_check=True), reads=[Bp, BVA], writes=[Boz[c]])
                infos = []
                for ki, i in enumerate(order):
                    infos.append(mk(ki, i, it)); it += 1
                def can_ahead(a, b):
                    return a["typ"] < 2 and b["typ"] < 2
                issued = set()
                for n, inf in enumerate(infos):
                    if n not in issued:
                        issue_S(inf); issued.add(n)
                    if n + 1 < len(infos) and can_ahead(inf, infos[n + 1]):
                        issue_S(infos[n + 1]); issued.add(n + 1)
                    issue_rest(inf)
                for sub in range(qn):
                    f = fcnt % 2; fcnt += 1
                    o0 = oz[0][sub // 2][:, (sub % 2) * 129:(sub % 2) * 129 + 129]; o1 = oz[1][sub // 2][:, (sub % 2) * 129:(sub % 2) * 129 + 129]
                    P.op("dve", lambda e, o0=o0: e.reciprocal(fz[:, 0:1], o0[:, 128:129]), reads=[Boz[0]], writes=[Bfz])
                    P.op("dve", lambda e, o1=o1: e.reciprocal(fz[:, 1:2], o1[:, 128:129]), reads=[Boz[1]], writes=[Bfz])
                    P.op("dve", lambda e: e.tensor_tensor(fz[:, 2:3], fz[:, 1:2], neglam, ALU.mult), reads=[Bfz, Blsc], writes=[Bfz])
                    P.op("dve", lambda e, o0=o0, f=f: e.tensor_scalar(fo[f][:, :], o0[:, 0:128], fz[:, 0:1], None, ALU.mult), reads=[Boz[0], Bfz], writes=[Bfo[f]])
                    P.op("dve", lambda e, o1=o1, f=f: e.scalar_tensor_tensor(fo[f][:, :], o1[:, 0:128], fz[:, 2:3], fo[f][:, :], ALU.mult, ALU.add), reads=[Boz[1], Bfz, Bfo[f]], writes=[Bfo[f]])
                    P.op("act", lambda e, f=f: e.activation(fo2[:, :], fo[f][:, :], AF.Square, accum_out=fz[:, 3:4]), reads=[Bfo[f]], writes=[Bfo2, Bfz])
                    P.op("act", lambda e: e.activation(fz[:, 4:5], fz[:, 3:4], AF.Ln, bias=SUBLN_EPS, scale=1.0 / 128), reads=[Bfz], writes=[Bfz])
                    P.op("act", lambda e: e.activation(fz[:, 5:6], fz[:, 4:5], AF.Exp, scale=-0.5), reads=[Bfz], writes=[Bfz])
                    P.op("dve", lambda e, f=f: e.scalar_tensor_tensor(osb[f][:, :], fo[f][:, :], fz[:, 5:6], gsc[:, :], ALU.mult, ALU.mult), reads=[Bfo[f], Bfz, Bgsc], writes=[Bosb[f]])
                    tt = qt0 + sub
                    P.dma("sp", o_d[tt * 128:(tt + 1) * 128, hi * 128:(hi + 1) * 128], osb[f][:], osem[f], reads=[Bosb[f]], writes=[Bout_d])
        P.barrier()
    P.finish([Bout_d])
    print("phaseA insts", P.ninst)
    nc = P.close()
    return nc, ctab_vals

def ref_attn(xp, pos, valid, w_in, lam4, subg, heads):
    T = xp.shape[0]
    D = 1024
    outs = []
    lam = np.exp((lam4[0] * lam4[1]).sum()) - np.exp((lam4[2] * lam4[3]).sum()) + 0.2
    for h in heads:
        q = xp @ w_in[:, h * 128:(h + 1) * 128]
        k = xp @ w_in[:, D + h * 128:D + (h + 1) * 128]
        v = xp @ w_in[:, 2 * D + h * 128:2 * D + (h + 1) * 128]
        sl = 2.0 ** (-(h + 1))
        dist = np.abs(pos[:, None] - pos[None, :]).astype(np.float32)
        ps = []
        for c in range(2):
            s = q[:, c * 64:(c + 1) * 64] @ k[:, c * 64:(c + 1) * 64].T / 8 - sl * dist
            s = np.where(valid[None, :], s, -np.inf)
            s = s - s.max(-1, keepdims=True)
            p = np.exp(s); p /= p.sum(-1, keepdims=True)
            ps.append(p)
        a = ps[0] - lam * ps[1]
        o = a @ v
        o = o / np.sqrt((o * o).mean(-1, keepdims=True) + 1e-5) * subg * 0.8
        outs.append(o)
    return np.concatenate(outs, 1)


import math
STAGE = 9
SKIP = ''
C = 64
NEGH = -math.exp(-0.5)

def rw_consts():
    m = np.zeros((64, 128), np.float32)
    s = np.arange(64)[:, None]; t = np.arange(64)[None, :]
    m[:, 0:64] = (s < t); m[:, 64:128] = (s <= t)
    return m

def build_phaseC(nch, NHD=8, D=1024, GW=512):
    P = Prog(); nc = P.nc
    Tp = nch * C
    NCH = NHD * 64
    xT_d = P.dram("xT", [D, Tp + 2], F32, "ExternalInput")
    mu_d = P.dram("mu", [D, 12], F32, "ExternalInput")
    wr_d = P.dram("wr", [D, NCH], F32, "ExternalInput"); wk_d = P.dram("wk", [D, NCH], F32, "ExternalInput"); wv_d = P.dram("wv", [D, NCH], F32, "ExternalInput")
    w1_d = P.dram("w1", [D, 64], F32, "ExternalInput"); w2_d = P.dram("w2", [64, NCH], F32, "ExternalInput")
    a1_d = P.dram("a1", [D, 64], F32, "ExternalInput"); a2_d = P.dram("a2", [64, NCH], F32, "ExternalInput")
    g1_d = P.dram("g1", [D, 160], F32, "ExternalInput"); g2_d = P.dram("g2", [160, NCH], F32, "ExternalInput")
    chv_d = P.dram("chv", [64, NHD, 8], F32, "ExternalInput")
    msk_d = P.dram("msk", [64, 128], F32, "ExternalInput")
    seg_d = P.dram("seg", [1, GW], F32, "ExternalInput")
    id_d = P.dram("ident", [128, 128], F32, "ExternalInput")
    y_d = P.dram("y", [Tp, NCH], F32, "ExternalOutput")
    aux_d = P.dram("aux", [4, NCH, Tp], F32, "ExternalOutput")
    By = Buf("y_d"); Baux = Buf("aux_d")
    ident = P.sb("ident_sb", [128, 128], BF16); Bid = Buf("ident"); P.dma("pool", ident[:], id_d[:, :], None, writes=[Bid])
    identf = P.sb("identf", [128, 128], F32); Bidf = Buf("identf"); P.dma("sp", identf[:], id_d[:, :], None, writes=[Bidf])
    def wload(name, src, K, N):
        kc = (K + 127) // 128
        t = P.sb(name, [128, kc, N], BF16); B = Buf(name)
        for c in range(kc):
            rows = min(128, K - c * 128)
            P.dma("pool", t[0:rows, c, :], src[c * 128:c * 128 + rows, :], None, writes=[B])
        return t, B
    wr, Bwr = wload("wr", wr_d, D, NCH); wk, Bwk = wload("wk", wk_d, D, NCH); wv, Bwv = wload("wv", wv_d, D, NCH)
    w1, Bw1 = wload("w1", w1_d, D, 64); a1, Ba1 = wload("a1", a1_d, D, 64); g1, Bg1 = wload("g1", g1_d, D, 160)
    w2, Bw2 = wload("w2", w2_d, 64, NCH); a2, Ba2 = wload("a2", a2_d, 64, NCH); g2, Bg2 = wload("g2", g2_d, 160, NCH)
    mu = P.sb("mu", [128, 8, 12], F32); Bmu = Buf("mu")
    P.dma("sp", mu[:], mu_d.rearrange("(c p) m -> p c m", p=128), None, writes=[Bmu])
    muc = P.sb("muc", [128, 8, 6], F32)
    P.op("dve", lambda e: e.tensor_tensor(muc[:, :, :], mu[:, :, 0:6], mu[:, :, 6:12], ALU.add), reads=[Bmu], writes=[Bmu])
    P.op("dve", lambda e: e.tensor_scalar(muc[:, :, :], muc[:, :, :], -1.0, 1.0, ALU.mult, ALU.add), reads=[Bmu], writes=[Bmu])
    chv = P.sb("chv", [64, NHD, 8], F32); Bchv = Buf("chv"); P.dma("sp", chv[:], chv_d[:, :, :], None, writes=[Bchv])
    P.op("dve", lambda e: e.tensor_scalar(chv[:, :, 4:5], chv[:, :, 3:4], -1.0, 1.0, ALU.mult, ALU.add), reads=[Bchv], writes=[Bchv])
    msk = P.sb("msk", [64, 128], F32); Bmsk = Buf("msk"); P.dma("sp", msk[:], msk_d[:, :], None, writes=[Bmsk])
    seg = P.sb("seg", [64, GW], F32); Bseg = Buf("seg"); P.dma("sp", seg[:], seg_d[0:1, :].to_broadcast([64, GW]), None, writes=[Bseg])
    ones64 = P.sb("ones64", [64, 64], F32); Bones = Buf("ones"); P.op("pool", lambda e: e.memset(ones64[:, :], 1.0), writes=[Bones])
    ST = P.sb("ST", [64, NHD, 64], F32); STb = P.sb("STb", [64, NHD, 64], BF16); BST = [Buf("ST%d" % h) for h in range(NHD)]
    P.op("pool", lambda e: e.memset(ST[:, :, :], 0.0), writes=BST)
    P.op("pool", lambda e: e.memset(STb[:, :, :], 0.0), writes=BST)
    x32 = P.sb("x32", [128, 8, GW + 2], F32); Bx32 = Buf("x32")
    xs = [P.sb("xs%d" % i, [128, 8, GW], BF16) for i in range(6)]; Bxs = [Buf("xs%d" % i) for i in range(6)]
    hw = P.sb("hw", [64, GW], BF16); Bhw = Buf("hw"); ha = P.sb("ha", [64, GW], BF16); Bha = Buf("ha")
    hg = P.sb("hg", [128, 2, GW], BF16); Bhg = Buf("hg")
    names = "r k v a lw kk ss t1 t2 Lc KRk".split()
    F = {n: P.sb("f_" + n, [64, GW], F32) for n in ["r", "k", "v", "a", "lw", "kk", "t1", "t2", "Lc", "kd", "g"]}
    BF = {n: Buf("f_" + n) for n in F}
    HB = 4
    NS = 4
    KRl = [P.sb("KR%d" % i, [64, GW // C, 2, C], BF16) for i in range(HB)]; BKRl = [Buf("KR") for i in range(HB)]
    KBl = [P.sb("KB%d" % i, [64, 2, GW], BF16) for i in range(HB)]; BKBl = [Buf("KB") for i in range(HB)]
    HTl = [P.sb("HT%d" % i, [64, 3, GW], BF16) for i in range(HB)]; BHTl = [Buf("HT") for i in range(HB)]
    gCl = [P.sb("gC%d" % i, [64, GW // C], F32) for i in range(HB)]; BgCl = [Buf("gC") for i in range(HB)]
    mskT = P.sb("mskT", [64, 64], F32)
    P.op("pool", lambda e: e.tensor_scalar(mskT[:, :], msk[:, 64:128], -1.0, 1.0, ALU.mult, ALU.add), reads=[Bmsk], writes=[Bmsk])
    class Slot: pass
    slots = []
    for si in range(NS):
        sl = Slot()
        sl.TM = P.sb("TM%d" % si, [64, 3, 64], BF16); sl.BTM = Buf("TM")
        sl.MA = P.sb("MA%d" % si, [64, 128], BF16); sl.BMA = Buf("MA")
        sl.MB = P.sb("MB%d" % si, [64, 128], BF16); sl.BMB = Buf("MB")
        sl.NT = P.sb("NT%d" % si, [64, 64], BF16); sl.BNT = Buf("NT")
        sl.X = [P.sb("X%d_%d" % (si, i), [64, 64], BF16) for i in range(2)]; sl.XT = [P.sb("XT%d_%d" % (si, i), [64, 64], BF16) for i in range(2)]
        sl.BX = [Buf("X") for i in range(2)]; sl.BXT = [Buf("XT") for i in range(2)]
        sl.R = P.sb("R%d" % si, [64, 64], F32); sl.Rb = P.sb("Rb%d" % si, [64, 64], BF16); sl.BR = Buf("R")
        sl.Wt = P.sb("Wt%d" % si, [64, 64], BF16); sl.BWt = Buf("Wt")
        sl.Ut = P.sb("Ut%d" % si, [64, 64], BF16); sl.BUt = Buf("Ut")
        slots.append(sl)
    ysb = P.sb("ysb", [64, GW // C, NHD, 64], F32); Bysb = Buf("ysb")
    ysem = P.newsem()
    banks = [P.ps("bank%d" % i, [128, 512], F32) for i in range(7)]; Bbanks = [Buf("bank%d" % i) for i in range(7)]
    pt = P.ps("ptr", [64, 3, 128], BF16); Bptr = Buf("ptr")
    for si in range(NS):
        slots[si].pa = banks[si]; slots[si].Bpa = Bbanks[si]
        slots[si].pw = banks[si]; slots[si].Bpw = Bbanks[si]
    pp = [banks[4], banks[5]]; Bpp = [Bbanks[4], Bbanks[5]]
    pl = banks[6]; Bpl = Bbanks[6]
    psq = banks[6]; Bpsq = Bbanks[6]
    ngr = (Tp + GW - 1) // GW
    ppi = 0
    for gi in range(ngr):
        t0 = gi * GW
        W = min(GW, Tp - t0)
        ncg = W // C
        for c in range(8):
            P.dma("sp", x32[:, c, 0:W + 2], xT_d[c * 128:(c + 1) * 128, t0:t0 + W + 2], None, writes=[Bx32])
        for i in range(6):
            for c in range(8):
                eng = "dve" if (i * 8 + c) % 2 == 0 else "pool"
                P.op("dve", lambda e, i=i, c=c: e.tensor_scalar(xs[i][:, c, 0:W], x32[:, c, 1:W + 1], muc[:, c, i:i + 1], None, ALU.mult), reads=[Bx32, Bmu], writes=[Bxs[i]])
                P.op("dve", lambda e, i=i, c=c: e.scalar_tensor_tensor(xs[i][:, c, 0:W], x32[:, c, 0:W], mu[:, c, i:i + 1], xs[i][:, c, 0:W], ALU.mult, ALU.add), reads=[Bx32, Bmu, Bxs[i]], writes=[Bxs[i]])
                P.op("dve", lambda e, i=i, c=c: e.scalar_tensor_tensor(xs[i][:, c, 0:W], x32[:, c, 2:W + 2], mu[:, c, 6 + i:7 + i], xs[i][:, c, 0:W], ALU.mult, ALU.add), reads=[Bx32, Bmu, Bxs[i]], writes=[Bxs[i]])
        for c in range(8):
            P.op("pe", lambda e, c=c: e.matmul(pl[0:64, 0:W], w1[:, c, :], xs[3][:, c, 0:W], start=(c == 0), stop=(c == 7)), reads=[Bw1, Bxs[3]], writes=[Bpl])
        P.op("act", lambda e: e.activation(hw[:, 0:W], pl[0:64, 0:W], AF.Tanh), reads=[Bpl], writes=[Bhw])
        for c in range(8):
            P.op("pe", lambda e, c=c: e.matmul(pl[0:64, 0:W], a1[:, c, :], xs[4][:, c, 0:W], start=(c == 0), stop=(c == 7)), reads=[Ba1, Bxs[4]], writes=[Bpl])
        P.op("act", lambda e: e.activation(ha[:, 0:W], pl[0:64, 0:W], AF.Identity), reads=[Bpl], writes=[Bha])
        for part, (lo, n) in enumerate(((0, 128), (128, 32))):
            for c in range(8):
                P.op("pe", lambda e, c=c, lo=lo, n=n: e.matmul(pl[0:n, 0:W], g1[:, c, lo:lo + n], xs[5][:, c, 0:W], start=(c == 0), stop=(c == 7)), reads=[Bg1, Bxs[5]], writes=[Bpl])
            P.op("act", lambda e, part=part, n=n: e.activation(hg[0:n, part, 0:W], pl[0:n, 0:W], AF.Sigmoid), reads=[Bpl], writes=[Bhg])
        for h in range(NHD):
            hb = h % HB
            KR, BKR, KB, BKB, HT, BHT, gC, BgC = KRl[hb], BKRl[hb], KBl[hb], BKBl[hb], HTl[hb], BHTl[hb], gCl[hb], BgCl[hb]
            cs = slice(h * 64, (h + 1) * 64)
            cw0, ca0, ckk, cka, c1ka = [chv[:, h, i:i + 1] for i in range(5)]
            def proj(wt, Bw, xi, dst, func=AF.Identity, **kw):
                nonlocal ppi
                p = pp[ppi % 2]; Bp = Bpp[ppi % 2]; ppi += 1
                for c in range(8):
                    P.op("pe", lambda e, c=c: e.matmul(p[0:64, 0:W], wt[:, c, cs], xs[xi][:, c, 0:W], start=(c == 0), stop=(c == 7)), reads=[Bw, Bxs[xi]], writes=[Bp])
                P.op("act", lambda e: e.activation(F[dst][:, 0:W], p[0:64, 0:W], func, **kw), reads=[Bp], writes=[BF[dst]])
            proj(wr, Bwr, 0, "r"); proj(wk, Bwk, 1, "k"); proj(wv, Bwv, 2, "v")
            def lora(w2t, Bw2_, hsrc, Bh, dst, bias):
                nonlocal ppi
                p = pp[ppi % 2]; Bp = Bpp[ppi % 2]; ppi += 1
                P.op("pe", lambda e: e.matmul(p[0:64, 0:W], w2t[0:64, 0, cs], hsrc[0:64, 0:W], start=True, stop=True), reads=[Bw2_, Bh], writes=[Bp])
                P.op("act", lambda e: e.activation(F[dst][:, 0:W], p[0:64, 0:W], AF.Sigmoid, bias=bias, scale=1.0), reads=[Bp, Bchv], writes=[BF[dst]])
            lora(w2, Bw2, hw, Bhw, "lw", cw0)
            lora(a2, Ba2, ha, Bha, "a", ca0)
            p = pp[ppi % 2]; Bp = Bpp[ppi % 2]; ppi += 1
            P.op("pe", lambda e: e.matmul(p[0:64, 0:W], g2[:, 0, cs], hg[:, 0, 0:W], start=True, stop=False), reads=[Bg2, Bhg], writes=[Bp])
            P.op("pe", lambda e: e.matmul(p[0:64, 0:W], g2[0:32, 1, cs], hg[0:32, 1, 0:W], start=False, stop=True), reads=[Bg2, Bhg], writes=[Bp])
            P.op("act", lambda e: e.activation(F["g"][:, 0:W], p[0:64, 0:W], AF.Identity), reads=[Bp], writes=[BF["g"]])
            P.op("dve", lambda e: e.tensor_scalar(F["kk"][:, 0:W], F["k"][:, 0:W], ckk, None, ALU.mult), reads=[BF["k"], Bchv], writes=[BF["kk"]])
            P.op("dve", lambda e: e.tensor_tensor(F["t1"][:, 0:W], F["kk"][:, 0:W], F["kk"][:, 0:W], ALU.mult), reads=[BF["kk"]], writes=[BF["t1"]])
            if 'a' not in SKIP:
                P.op("pe", lambda e: e.matmul(psq[0:64, 0:W], ones64[:, :], F["t1"][:, 0:W], start=True, stop=True), reads=[Bones, BF["t1"]], writes=[Bpsq])
            P.op("act", lambda e: e.activation(F["t2"][:, 0:W], psq[0:64, 0:W], AF.Ln, bias=1e-24, scale=1.0), reads=[Bpsq], writes=[BF["t2"]])
            P.op("act", lambda e: e.activation(F["t2"][:, 0:W], F["t2"][:, 0:W], AF.Exp, scale=-0.5), reads=[BF["t2"]], writes=[BF["t2"]])
            P.op("dve", lambda e: e.tensor_tensor(F["kk"][:, 0:W], F["kk"][:, 0:W], F["t2"][:, 0:W], ALU.mult), reads=[BF["kk"], BF["t2"]], writes=[BF["kk"]])
            P.op("dve", lambda e: e.tensor_scalar(F["t1"][:, 0:W], F["a"][:, 0:W], cka, c1ka, ALU.mult, ALU.add), reads=[BF["a"], Bchv], writes=[BF["t1"]])
            P.op("dve", lambda e: e.tensor_tensor(F["kd"][:, 0:W], F["k"][:, 0:W], F["t1"][:, 0:W], ALU.mult), reads=[BF["k"], BF["t1"]], writes=[BF["kd"]])
            P.op("pool", lambda e: e.tensor_tensor(F["a"][:, 0:W], F["a"][:, 0:W], F["kk"][:, 0:W], ALU.mult), reads=[BF["a"], BF["kk"]], writes=[BF["a"]])
            P.op("pool", lambda e: e.tensor_scalar(F["lw"][:, 0:W], F["lw"][:, 0:W], NEGH, None, ALU.mult), reads=[BF["lw"]], writes=[BF["lw"]])
            if 'c' not in SKIP:
              P.op("dve", lambda e: e.tensor_tensor_scan(F["Lc"][:, 0:W], seg[:, 0:W], F["lw"][:, 0:W], 0.0, ALU.mult, ALU.add), reads=[Bseg, BF["lw"]], writes=[BF["Lc"]])
            Lc3 = F["Lc"][:, 0:W].rearrange("p (n c) -> p n c", c=C)
            P.op("act", lambda e: e.activation(gC[:, 0:ncg], Lc3[:, :, C - 1], AF.Exp), reads=[BF["Lc"]], writes=[BgC])
            P.op("act", lambda e: e.activation(F["t1"][:, 0:W], F["Lc"][:, 0:W], AF.Exp), reads=[BF["Lc"]], writes=[BF["t1"]])
            P.op("dve", lambda e: e.tensor_tensor(KR[:, 0:ncg, 1, :], F["r"][:, 0:W].rearrange("p (n c) -> p n c", c=C), F["t1"][:, 0:W].rearrange("p (n c) -> p n c", c=C), ALU.mult), reads=[BF["r"], BF["t1"]], writes=[BKR])
            P.op("pool", lambda e: e.tensor_tensor(F["t2"][:, 0:W], F["Lc"][:, 0:W], F["lw"][:, 0:W], ALU.subtract), reads=[BF["Lc"], BF["lw"]], writes=[BF["t2"]])
            P.op("act", lambda e: e.activation(F["t2"][:, 0:W], F["t2"][:, 0:W], AF.Exp), reads=[BF["t2"]], writes=[BF["t2"]])
            P.op("dve", lambda e: e.tensor_tensor(KR[:, 0:ncg, 0, :], F["kk"][:, 0:W].rearrange("p (n c) -> p n c", c=C), F["t2"][:, 0:W].rearrange("p (n c) -> p n c", c=C), ALU.mult), reads=[BF["kk"], BF["t2"]], writes=[BKR])
            P.op("act", lambda e: e.activation(F["t1"][:, 0:W], F["Lc"][:, 0:W], AF.Exp, scale=-1.0), reads=[BF["Lc"]], writes=[BF["t1"]])
            P.op("dve", lambda e: e.tensor_tensor(KB[:, 0, 0:W], F["kd"][:, 0:W], F["t1"][:, 0:W], ALU.mult), reads=[BF["kd"], BF["t1"]], writes=[BKB])
            P.op("pool", lambda e: e.tensor_tensor(KB[:, 1, 0:W], F["a"][:, 0:W], F["t1"][:, 0:W], ALU.mult), reads=[BF["a"], BF["t1"]], writes=[BKB])
            if 'b' not in SKIP:
              P.op("dve", lambda e: e.tensor_tensor(F["t2"][:, 0:W].rearrange("p (n c) -> p n c", c=C), Lc3[:, :, C - 1:C].to_broadcast([64, ncg, C]), Lc3, ALU.subtract), reads=[BF["Lc"]], writes=[BF["t2"]])
            P.op("act", lambda e: e.activation(F["t2"][:, 0:W], F["t2"][:, 0:W], AF.Exp), reads=[BF["t2"]], writes=[BF["t2"]])
            P.op("dve", lambda e: e.tensor_tensor(HT[:, 0, 0:W], F["kd"][:, 0:W], F["t2"][:, 0:W], ALU.mult), reads=[BF["kd"], BF["t2"]], writes=[BHT])
            P.op("dve", lambda e: e.scalar_tensor_tensor(HT[:, 1, 0:W], F["a"][:, 0:W], -1.0, F["t2"][:, 0:W], ALU.mult, ALU.mult), reads=[BF["a"], BF["t2"]], writes=[BHT])
            P.op("pool", lambda e: e.tensor_copy(HT[:, 2, 0:W], F["v"][:, 0:W]), reads=[BF["v"]], writes=[BHT])
            for ai, nm in enumerate(("r", "kd", "v", "g")):
                P.dma("sp", aux_d[ai, h * 64:(h + 1) * 64, t0:t0 + W], F[nm][:, 0:W], None, reads=[BF[nm]], writes=[Baux])
            if hb == HB - 1 or h == NHD - 1:
                batch = list(range(h - hb, h + 1))
                def unit(hh, ci, sl):
                    hb2 = hh % HB
                    KR, BKR, KB, BKB, HT, BHT, gC, BgC = KRl[hb2], BKRl[hb2], KBl[hb2], BKBl[hb2], HTl[hb2], BHTl[hb2], gCl[hb2], BgCl[hb2]
                    cc = slice(ci * C, (ci + 1) * C)
                    pa, Bpa, pw, Bpw = sl.pa, sl.Bpa, sl.pw, sl.Bpw
                    TM, BTM, MA, BMA, MB, BMB, NT_, BNT, R, Rb, BR, Wt, BWt, Ut, BUt = sl.TM, sl.BTM, sl.MA, sl.BMA, sl.MB, sl.BMB, sl.NT, sl.BNT, sl.R, sl.Rb, sl.BR, sl.Wt, sl.BWt, sl.Ut, sl.BUt
                    for q in range(3):
                        P.op("pe", lambda e, q=q: e.transpose(pt[:, q, 0:64], HT[:, q, cc], ident[0:64, 0:64]), reads=[BHT, Bid], writes=[Bptr])
                    P.op("dve", lambda e: e.tensor_copy(TM[:, :, :], pt[:, :, 0:64]), reads=[Bptr], writes=[BTM])
                    P.op("pe", lambda e: e.matmul(pa[0:64, 0:128], KB[:, 0, cc], KR[:, ci, :, :], start=True, stop=True), reads=[BKB, BKR], writes=[Bpa])
                    P.op("pe", lambda e: e.matmul(pa[0:64, 128:256], KB[:, 1, cc], KR[:, ci, :, :], start=False, stop=True, skip_group_check=True), reads=[BKB, BKR], writes=[Bpa])
                    P.op("pe", lambda e: e.matmul(pa[0:64, 256:320], KR[:, ci, 0, :], KB[:, 1, cc], start=False, stop=True, skip_group_check=True), reads=[BKB, BKR], writes=[Bpa])
                    yield
                    P.op("dve", lambda e: e.tensor_tensor(MA[:, :], pa[0:64, 0:128], msk[:, :], ALU.mult), reads=[Bpa, Bmsk], writes=[BMA])
                    P.op("dve", lambda e: e.tensor_tensor(MB[:, 0:64], pa[0:64, 128:192], msk[:, 0:64], ALU.mult), reads=[Bpa, Bmsk], writes=[BMB])
                    P.op("dve", lambda e: e.scalar_tensor_tensor(MB[:, 64:128], pa[0:64, 192:256], -1.0, msk[:, 64:128], ALU.mult, ALU.mult), reads=[Bpa, Bmsk], writes=[BMB])
                    P.op("dve", lambda e: e.tensor_tensor(NT_[:, :], pa[0:64, 256:320], mskT[:, :], ALU.mult), reads=[Bpa, Bmsk], writes=[BNT])
                    P.op("dve", lambda e: e.tensor_tensor(R[:, :], identf[0:64, 0:64], MB[:, 0:64], ALU.subtract), reads=[Bidf, BMB], writes=[BR])
                    P.op("act", lambda e: e.activation(Rb[:, :], R[:, :], AF.Identity), reads=[BR], writes=[BR])
                    yield
                    curX, curXT, BcX, BcXT = MB[:, 0:64], NT_[:, :], BMB, BNT
                    for lvl in range(5):
                        k2 = lvl % 2
                        P.op("pe", lambda e, curX=curX, curXT=curXT: e.matmul(pw[0:64, 0:64], curXT, curX, start=True, stop=True), reads=[BcX, BcXT], writes=[Bpw])
                        P.op("pe", lambda e, curX=curX, curXT=curXT: e.matmul(pw[0:64, 64:128], curX, curXT, start=False, stop=True, skip_group_check=True), reads=[BcX, BcXT], writes=[Bpw])
                        yield
                        P.op("dve", lambda e, k2=k2: e.tensor_copy(sl.X[k2][:, :], pw[0:64, 0:64]), reads=[Bpw], writes=[sl.BX[k2]])
                        P.op("dve", lambda e, k2=k2: e.tensor_copy(sl.XT[k2][:, :], pw[0:64, 64:128]), reads=[Bpw], writes=[sl.BXT[k2]])
                        P.op("pe", lambda e, k2=k2: e.matmul(pw[0:64, 128:192], sl.XT[k2][:, :], Rb[:, :], start=False, stop=True, skip_group_check=True), reads=[sl.BXT[k2], BR], writes=[Bpw])
                        yield
                        P.op("dve", lambda e: e.tensor_tensor(R[:, :], R[:, :], pw[0:64, 128:192], ALU.add), reads=[BR, Bpw], writes=[BR])
                        P.op("act", lambda e: e.activation(Rb[:, :], R[:, :], AF.Identity), reads=[BR], writes=[BR])
                        yield
                        curX, curXT, BcX, BcXT = sl.X[k2][:, :], sl.XT[k2][:, :], sl.BX[k2], sl.BXT[k2]
                    P.op("pe", lambda e: e.matmul(pw[0:64, 192:256], KR[:, ci, 0, :], STb[:, hh, :], start=False, stop=False, skip_group_check=True), reads=[BKR, BST[hh]], writes=[Bpw])
                    P.op("pe", lambda e: e.matmul(pw[0:64, 192:256], MA[:, 0:64], TM[:, 2, :], start=False, stop=True, skip_group_check=True), reads=[BMA, BTM], writes=[Bpw])
                    yield
                    P.op("dve", lambda e: e.tensor_copy(Wt[:, :], pw[0:64, 192:256]), reads=[Bpw], writes=[BWt])
                    P.op("pe", lambda e: e.matmul(pw[0:64, 256:320], Rb[:, :], Wt[:, :], start=False, stop=True, skip_group_check=True), reads=[BR, BWt], writes=[Bpw])
                    yield
                    P.op("dve", lambda e: e.tensor_copy(Ut[:, :], pw[0:64, 256:320]), reads=[Bpw], writes=[BUt])
                    P.op("pe", lambda e: e.matmul(pw[0:64, 320:384], KR[:, ci, 1, :], STb[:, hh, :], start=False, stop=False, skip_group_check=True), reads=[BKR, BST[hh]], writes=[Bpw])
                    P.op("pe", lambda e: e.matmul(pw[0:64, 320:384], MA[:, 64:128], TM[:, 2, :], start=False, stop=False, skip_group_check=True), reads=[BMA, BTM], writes=[Bpw])
                    P.op("pe", lambda e: e.matmul(pw[0:64, 320:384], MB[:, 64:128], Ut[:, :], start=False, stop=True, skip_group_check=True), reads=[BMB, BUt], writes=[Bpw])
                    P.op("pe", lambda e: e.matmul(pw[0:64, 384:448], TM[:, 0, :], TM[:, 2, :], start=False, stop=False, skip_group_check=True), reads=[BTM], writes=[Bpw])
                    P.op("pe", lambda e: e.matmul(pw[0:64, 384:448], TM[:, 1, :], Ut[:, :], start=False, stop=True, skip_group_check=True), reads=[BTM, BUt], writes=[Bpw])
                    yield
                    P.op("dve", lambda e: e.tensor_copy(ysb[:, ci, hh, :], pw[0:64, 320:384]), reads=[Bpw], writes=[Bysb])
                    P.op("dve", lambda e: e.scalar_tensor_tensor(ST[:, hh, :], ST[:, hh, :], gC[:, ci:ci + 1], pw[0:64, 384:448], ALU.mult, ALU.add), reads=[BST[hh], BgC, Bpw], writes=[BST[hh]])
                    P.op("act", lambda e: e.activation(STb[:, hh, :], ST[:, hh, :], AF.Identity), reads=[BST[hh]], writes=[BST[hh]])
                    yield
                todo = [(hh, ci) for ci in range(ncg) for hh in batch]
                active = []
                free = list(range(NS))
                while todo or active:
                    while todo and free:
                        hh, ci = todo.pop(0); si = free.pop(0)
                        active.append((unit(hh, ci, slots[si]), si))
                    nxt_active = []
                    for gen, si in active:
                        try:
                            next(gen); nxt_active.append((gen, si))
                        except StopIteration:
                            free.append(si)
                    active = nxt_active
        for ci in range(ncg):
            P.dma("sp", y_d[t0 + ci * C:t0 + (ci + 1) * C, :], ysb[:, ci, :, :], ysem, reads=[Bysb], writes=[By])
    P.finish([By, Baux])
    print("phaseC insts", P.ninst)
    return P.close()

def ref_dir(xs6, Wd, nh):
    T = xs6.shape[1]
    r = xs6[0] @ Wd["wr"]; k = xs6[1] @ Wd["wk"]; v = xs6[2] @ Wd["wv"]
    lw = np.tanh(xs6[3] @ Wd["w1"]) @ Wd["w2"]
    z = Wd["w0"] + lw
    w_log = -np.log1p(np.exp(-z)) - 0.5
    decay = np.exp(-np.exp(w_log))
    a = 1 / (1 + np.exp(-(Wd["a0"] + (xs6[4] @ Wd["a1"]) @ Wd["a2"])))
    g = (1 / (1 + np.exp(-(xs6[5] @ Wd["g1"])))) @ Wd["g2"]
    kk = (k * Wd["k_k"]).reshape(T, nh, 64)
    kk = kk / np.maximum(np.sqrt((kk * kk).sum(-1, keepdims=True)), 1e-12)
    kd = k * (1 + (a - 1) * Wd["k_a"])
    rh = r.reshape(T, nh, 64); wh = decay.reshape(T, nh, 64); kdh = kd.reshape(T, nh, 64); vh = v.reshape(T, nh, 64); ah = a.reshape(T, nh, 64)
    S = np.zeros((nh, 64, 64)); ys = np.zeros((T, nh, 64))
    for t in range(T):
        sa = np.einsum('hvk,hk->hv', S, kk[t])
        S = S * wh[t][:, None, :] - sa[:, :, None] * (kk[t] * ah[t])[:, None, :] + vh[t][:, :, None] * kdh[t][:, None, :]
        ys[t] = np.einsum('hvk,hk->hv', S, rh[t])
    return ys.reshape(T, nh * 64), r, kd, v, g


LNX_EPS = 64e-5

def build_phaseD(ntiles, FE=3584, E=8, D=1024, FW=256, PART=11):
    P = Prog(); nc = P.nc
    T = ntiles * 128
    NHh = D // 64
    names = ["yf", "yb", "r", "kdf", "kdb", "v", "g", "h1"]
    ind = {n: P.dram(n, [T, D], F32, "ExternalInput") for n in names}
    wo_d = P.dram("wo", [D, D], F32, "ExternalInput")
    vec_d = P.dram("vecs", [7, D], F32, "ExternalInput")
    wr_d = P.dram("wrt", [E, D], F32, "ExternalInput")
    br_d = P.dram("brt", [1, E], F32, "ExternalInput")
    wg_d = P.dram("wg", [E, D, FE], F32, "ExternalInput")
    wu_d = P.dram("wu", [E, D, FE], F32, "ExternalInput")
    wd_d = P.dram("wd", [E, FE, D], F32, "ExternalInput")
    id_d = P.dram("ident", [128, 128], F32, "ExternalInput")
    out_d = P.dram("out", [T, D], F32, "ExternalOutput")
    Bout_d = Buf("out_d")
    ident = P.sb("ident_sb", [128, 128], BF16); Bid = Buf("ident"); P.dma("pool", ident[:], id_d[:, :], None, writes=[Bid])
    wo, Bwo = load_w_bf16(P, "wo_sb", wo_d, D, D, None)
    vb = []; Bvb = []
    for i in range(7):
        t, B = load_bcast(P, "vec%d" % i, vec_d[i:i + 1, :], D, None); vb.append(t); Bvb.append(B)
    brb, Bbrb = load_bcast(P, "brb", br_d[0:1, :], E, None)
    halves = [list(range(a, min(ntiles, a + PART))) for a in range(0, ntiles, PART)]
    maxh = max(len(h) for h in halves)
    hT = P.sb("hT", [128, 8, maxh * 128], BF16); BhT = Buf("hT")
    yacc = P.sb("yacc", [128, maxh, D], F32); Byacc = [Buf("yacc%d" % i) for i in range(maxh)]
    gates = P.sb("gates", [128, maxh, E], F32); Bgates = Buf("gates")
    st = P.sb("lnst", [128, 12], F32); mv = P.sb("lnmv", [128, 4], F32); Bscr = Buf("lnscr")
    pt = [P.ps("pt%d" % i, [128, 8, 128], BF16) for i in range(2)]; Bpt = [Buf("pt") for i in range(2)]
    pm = [P.ps("pm%d" % i, [128, 512], F32) for i in range(2)]; Bpm = [Buf("pm") for i in range(2)]
    pg = [P.ps("pg%d" % i, [128, 512], F32) for i in range(2)]; Bpg = [Buf("pg") for i in range(2)]
    pu = [P.ps("pu%d" % i, [128, 512], F32) for i in range(2)]; Bpu = [Buf("pu") for i in range(2)]
    NFC = FW // 128
    outt = P.sb("outt", [128, D], F32); Boutt = Buf("outt"); outsem = P.newsem()
    ptc = 0; wcc = 0; hc = 0
    for half in halves:
        if not half: continue
        with P.scope():
            tin = {n: P.sb("in_" + n, [128, D], F32) for n in names}; Bin = {n: Buf("in_" + n) for n in names}
            s16 = P.sb("s16", [128, 8, NHh], F32); Bs16 = Buf("s16")
            tmp = P.sb("tmp", [128, D], F32); Btmp = Buf("tmp")
            obf = P.sb("obf", [128, D], BF16); Bobf = Buf("obf")
            oT = P.sb("oT", [128, 8, 128], BF16); BoT = Buf("oT")
            hbf = P.sb("hbf", [128, D], BF16); Bhbf = Buf("hbf")
            lg = P.sb("lg", [128, 4, E], F32); Blg = Buf("lg")
            wrb = P.sb("wrb", [128, E, D], F32); Bwrb = Buf("wrb")
            for e_ in range(E):
                P.dma("sp", wrb[:, e_, :], wr_d[e_:e_ + 1, :].to_broadcast([128, D]), None, writes=[Bwrb])
            for j, t in enumerate(half):
                for n in names:
                    P.dma("sp", tin[n][:], ind[n][t * 128:(t + 1) * 128, :], None, writes=[Bin[n]])
                y = tin["yf"]; By_ = Bin["yf"]
                v3 = lambda a: a[:, :].rearrange("p (h c) -> p h c", c=64)
                P.op("dve", lambda e: e.tensor_tensor(y[:, :], y[:, :], tin["yb"][:, :], ALU.add), reads=[By_, Bin["yb"]], writes=[By_])
                P.op("dve", lambda e: e.tensor_reduce(s16[:, 0, :], v3(y), AX.X, ALU.add), reads=[By_], writes=[Bs16])
                P.op("pool", lambda e: e.tensor_tensor(tmp[:, :], y[:, :], y[:, :], ALU.mult), reads=[By_], writes=[Btmp])
                P.op("dve", lambda e: e.tensor_reduce(s16[:, 1, :], v3(tmp), AX.X, ALU.add), reads=[Btmp], writes=[Bs16])
                P.op("dve", lambda e: e.tensor_scalar(s16[:, 0, :], s16[:, 0, :], 1.0 / 64, None, ALU.mult), reads=[Bs16], writes=[Bs16])
                P.op("dve", lambda e: e.tensor_tensor(s16[:, 2, :], s16[:, 0, :], s16[:, 0, :], ALU.mult), reads=[Bs16], writes=[Bs16])
                P.op("dve", lambda e: e.scalar_tensor_tensor(s16[:, 3, :], s16[:, 1, :], 1.0 / 64, s16[:, 2, :], ALU.mult, ALU.subtract), reads=[Bs16], writes=[Bs16])
                P.op("act", lambda e: e.activation(s16[:, 4, :], s16[:, 3, :], AF.Ln, bias=LNX_EPS, scale=1.0), reads=[Bs16], writes=[Bs16])
                P.op("act", lambda e: e.activation(s16[:, 5, :], s16[:, 4, :], AF.Exp, scale=-0.5), reads=[Bs16], writes=[Bs16])
                P.op("dve", lambda e: e.tensor_tensor(v3(y), v3(y), s16[:, 0, :].to_broadcast([128, NHh, 64]) if False else s16[:, 0:1, :].rearrange("p o h -> p h o").to_broadcast([128, NHh, 64]), ALU.subtract), reads=[By_, Bs16], writes=[By_])
                P.op("dve", lambda e: e.tensor_tensor(v3(y), v3(y), s16[:, 5:6, :].rearrange("p o h -> p h o").to_broadcast([128, NHh, 64]), ALU.mult), reads=[By_, Bs16], writes=[By_])
                P.op("pool", lambda e: e.tensor_tensor(y[:, :], y[:, :], vb[0][:, :], ALU.mult), reads=[By_, Bvb[0]], writes=[By_])
                P.op("pool", lambda e: e.tensor_tensor(y[:, :], y[:, :], vb[1][:, :], ALU.add), reads=[By_, Bvb[1]], writes=[By_])
                kd = tin["kdf"]
                P.op("dve", lambda e: e.tensor_tensor(kd[:, :], kd[:, :], tin["kdb"][:, :], ALU.add), reads=[Bin["kdf"], Bin["kdb"]], writes=[Bin["kdf"]])
                P.op("pool", lambda e: e.tensor_tensor(kd[:, :], kd[:, :], vb[2][:, :], ALU.mult), reads=[Bin["kdf"], Bvb[2]], writes=[Bin["kdf"]])
                P.op("dve", lambda e: e.tensor_tensor(kd[:, :], kd[:, :], tin["r"][:, :], ALU.mult), reads=[Bin["kdf"], Bin["r"]], writes=[Bin["kdf"]])
                P.op("dve", lambda e: e.tensor_reduce(s16[:, 6, :], v3(kd), AX.X, ALU.add), reads=[Bin["kdf"]], writes=[Bs16])
                P.op("dve", lambda e: e.tensor_tensor(v3(tmp), v3(tin["v"]), s16[:, 6:7, :].rearrange("p o h -> p h o").to_broadcast([128, NHh, 64]), ALU.mult), reads=[Bin["v"], Bs16], writes=[Btmp])
                P.op("dve", lambda e: e.tensor_tensor(y[:, :], y[:, :], tmp[:, :], ALU.add), reads=[By_, Btmp], writes=[By_])
                P.op("dve", lambda e: e.tensor_tensor(obf[:, :], y[:, :], tin["g"][:, :], ALU.mult), reads=[By_, Bin["g"]], writes=[Bobf])
                transpose_tile(P, obf, Bobf, ident, Bid, pt[ptc % 2], Bpt[ptc % 2], oT, BoT, 0); ptc += 1
                res = tin["h1"]; Bres = Bin["h1"]
                for nh in range(2):
                    for c in range(8):
                        P.op("pe", lambda e, c=c, nh=nh: e.matmul(pm[nh][:, :], oT[:, c, :], wo[:, c, nh * 512:(nh + 1) * 512], start=(c == 0), stop=(c == 7)), reads=[BoT, Bwo], writes=[Bpm[nh]])
                    P.op("dve", lambda e, nh=nh: e.scalar_tensor_tensor(res[:, nh * 512:(nh + 1) * 512], res[:, nh * 512:(nh + 1) * 512], ALPHA, pm[nh][:, :], ALU.mult, ALU.add), reads=[Bres, Bpm[nh]], writes=[Bres])
                layer_norm(P, res, Bres, vb[3], Bvb[3], vb[4], Bvb[4], (st, mv), Bscr, res, Bres, hbf, Bhbf)
                transpose_tile(P, hbf, Bhbf, ident, Bid, pt[ptc % 2], Bpt[ptc % 2], hT, BhT, j); ptc += 1
                P.op("act", lambda e, j=j: e.activation(yacc[:, j, :], res[:, :], AF.Identity, scale=ALPHA), reads=[Bres], writes=[Byacc[j]])
                for e_ in range(E):
                    P.op("pool" if e_ % 2 else "dve", lambda e, e_=e_: e.tensor_tensor(tmp[:, :], res[:, :], wrb[:, e_, :], ALU.mult), reads=[Bres, Bwrb], writes=[Btmp])
                    P.op("dve", lambda e, e_=e_: e.reduce_sum(lg[:, 0, e_:e_ + 1], tmp[:, :], AX.X), reads=[Btmp], writes=[Blg])
                P.op("dve", lambda e: e.tensor_tensor(lg[:, 0, :], lg[:, 0, :], brb[:, :], ALU.add), reads=[Blg, Bbrb], writes=[Blg])
                P.op("dve", lambda e: e.reduce_max(lg[:, 1, 0:1], lg[:, 0, :], AX.X), reads=[Blg], writes=[Blg])
                P.op("dve", lambda e: e.tensor_scalar(lg[:, 2, :], lg[:, 0, :], lg[:, 1, 0:1], -1e30, ALU.is_equal, ALU.mult), reads=[Blg], writes=[Blg])
                P.op("dve", lambda e: e.tensor_tensor(lg[:, 2, :], lg[:, 2, :], lg[:, 0, :], ALU.add), reads=[Blg], writes=[Blg])
                P.op("dve", lambda e: e.reduce_max(lg[:, 1, 1:2], lg[:, 2, :], AX.X), reads=[Blg], writes=[Blg])
                P.op("dve", lambda e: e.tensor_scalar(lg[:, 2, :], lg[:, 0, :], lg[:, 1, 1:2], None, ALU.is_ge), reads=[Blg], writes=[Blg])
                P.op("dve", lambda e: e.tensor_scalar(lg[:, 3, :], lg[:, 0, :], lg[:, 1, 0:1], None, ALU.subtract), reads=[Blg], writes=[Blg])
                P.op("act", lambda e: e.activation(lg[:, 3, :], lg[:, 3, :], AF.Exp), reads=[Blg], writes=[Blg])
                P.op("dve", lambda e: e.tensor_tensor(lg[:, 3, :], lg[:, 3, :], lg[:, 2, :], ALU.mult), reads=[Blg], writes=[Blg])
                P.op("dve", lambda e: e.reduce_sum(lg[:, 1, 2:3], lg[:, 3, :], AX.X), reads=[Blg], writes=[Blg])
                P.op("dve", lambda e: e.reciprocal(lg[:, 1, 3:4], lg[:, 1, 2:3]), reads=[Blg], writes=[Blg])
                P.op("dve", lambda e, j=j: e.tensor_scalar(gates[:, j, :], lg[:, 3, :], lg[:, 1, 3:4], None, ALU.mult), reads=[Blg], writes=[Bgates])
        P.barrier()
        nT = len(half) * 128
        _sc = P.scope(); _sc.__enter__()
        wgc = [P.sb("wgc%d" % i, [128, 8, FW], BF16) for i in range(2)]; wuc = [P.sb("wuc%d" % i, [128, 8, FW], BF16) for i in range(2)]
        wdc = [P.sb("wdc%d" % i, [128, NFC, D], BF16) for i in range(2)]
        Bwc = [Buf("wc%d" % i) for i in range(2)]
        swg = [P.sb("swg%d" % i, [128, 8, FW], F32) for i in range(2)]; swu = [P.sb("swu%d" % i, [128, 8, FW], F32) for i in range(2)]
        swd = [P.sb("swd%d" % i, [128, NFC, D], F32) for i in range(2)]
        Bsw = [[Buf("sw") for j in range(3)] for i in range(2)]
        hid = [P.sb("hid%d" % i, [128, NFC, 512], BF16) for i in range(2)]; Bhid = [Buf("hid") for i in range(2)]
        sg = [P.sb("sg%d" % i, [128, 512], F32) for i in range(2)]; Bsg = [Buf("sg") for i in range(2)]
        tgroups = [(a, min(512, nT - a)) for a in range(0, nT, 512)]
        for e_ in range(E):
            for fg in range(FE // FW):
                k = wcc % 2; wcc += 1
                f0 = fg * FW
                P.dma("sp", swg[k][:, :, :], wg_d[e_, :, f0:f0 + FW].rearrange("(c p) f -> p c f", p=128), None, writes=[Bsw[k][0]])
                P.dma("sp", swu[k][:, :, :], wu_d[e_, :, f0:f0 + FW].rearrange("(c p) f -> p c f", p=128), None, writes=[Bsw[k][1]])
                P.dma("sp", swd[k][:, :, :], wd_d[e_, f0:f0 + FW, :].rearrange("(c p) d -> p c d", p=128), None, writes=[Bsw[k][2]])
                P.op("pool", lambda e: e.tensor_copy(wgc[k][:, :, :], swg[k][:, :, :]), reads=[Bsw[k][0]], writes=[Bwc[k]])
                P.op("act", lambda e: e.activation(wuc[k][:, :, :], swu[k][:, :, :], AF.Identity), reads=[Bsw[k][1]], writes=[Bwc[k]])
                P.op("pool", lambda e: e.tensor_copy(wdc[k][:, :, :], swd[k][:, :, :]), reads=[Bsw[k][2]], writes=[Bwc[k]])
                for (a, w) in tgroups:
                    hk = hc % 2; hc += 1
                    for fc in range(NFC):
                        for c in range(8):
                            P.op("pe", lambda e, c=c, fc=fc: e.matmul(pg[hk][:, 0:w], wgc[k][:, c, fc * 128:(fc + 1) * 128], hT[:, c, a:a + w], start=(c == 0), stop=(c == 7)), reads=[Bwc[k], BhT], writes=[Bpg[hk]])
                        for c in range(8):
                            P.op("pe", lambda e, c=c, fc=fc: e.matmul(pu[hk][:, 0:w], wuc[k][:, c, fc * 128:(fc + 1) * 128], hT[:, c, a:a + w], start=(c == 0), stop=(c == 7)), reads=[Bwc[k], BhT], writes=[Bpu[hk]])
                        P.op("act", lambda e: e.activation(sg[hk][:, 0:w], pg[hk][:, 0:w], AF.Silu), reads=[Bpg[hk]], writes=[Bsg[hk]])
                        P.op("dve", lambda e, fc=fc: e.tensor_tensor(hid[hk][:, fc, 0:w], sg[hk][:, 0:w], pu[hk][:, 0:w], ALU.mult), reads=[Bsg[hk], Bpu[hk]], writes=[Bhid[hk]])
                    for jj in range(w // 128):
                        j = a // 128 + jj
                        for nh in range(2):
                            for fc in range(NFC):
                                P.op("pe", lambda e, fc=fc, nh=nh, jj=jj: e.matmul(pm[nh][:, :], hid[hk][:, fc, jj * 128:(jj + 1) * 128], wdc[k][:, fc, nh * 512:(nh + 1) * 512], start=(fc == 0), stop=(fc == NFC - 1)), reads=[Bhid[hk], Bwc[k]], writes=[Bpm[nh]])
                            P.op("dve" if nh else "pool", lambda e, nh=nh, j=j: e.scalar_tensor_tensor(yacc[:, j, nh * 512:(nh + 1) * 512], pm[nh][:, :], gates[:, j, e_:e_ + 1], yacc[:, j, nh * 512:(nh + 1) * 512], ALU.mult, ALU.add) if nh else e.tensor_copy(sg[hk][:, :], sg[hk][:, :]), reads=[Bpm[nh], Bgates, Byacc[j]], writes=[Byacc[j]]) if False else \
                            P.op("dve", lambda e, nh=nh, j=j: e.scalar_tensor_tensor(yacc[:, j, nh * 512:(nh + 1) * 512], pm[nh][:, :], gates[:, j, e_:e_ + 1], yacc[:, j, nh * 512:(nh + 1) * 512], ALU.mult, ALU.add), reads=[Bpm[nh], Bgates, Byacc[j]], writes=[Byacc[j]])
        P.barrier()
        _sc.__exit__(None, None, None)
        for j, t in enumerate(half):
            layer_norm(P, yacc[:, j, :], Byacc[j], vb[5], Bvb[5], vb[6], Bvb[6], (st, mv), Bscr, outt, Boutt)
            P.dma("sp", out_d[t * 128:(t + 1) * 128, :], outt[:], outsem, reads=[Boutt], writes=[Bout_d])
        P.barrier()
    P.finish([Bout_d])
    print("phaseD insts", P.ninst)
    return P.close()


_CACHE = {}

def _run(nc, maps):
    res = run_bass_kernel_spmd(nc, maps, core_ids=list(range(8)))
    return res.results

def kernel(**inp):
    f32 = np.float32
    g = lambda k: np.asarray(inp[k], dtype=f32)
    x = g("x"); meta = g("meta")
    Bn, S, D = x.shape
    NM = meta.shape[0]; L = S + NM
    ident = np.eye(128, dtype=f32)
    nxt = S // 128
    w_in = g("attn_w_in")[0]
    lamv = np.stack([g("attn_lam_q1")[0], g("attn_lam_k1")[0], g("attn_lam_q2")[0], g("attn_lam_k2")[0]])
    subg = g("attn_subln_g")[0][None, :]
    ncA, mults = build_phaseA(2, nxt)
    maps = []
    for c in range(8):
        b = c // 4; heads = [2 * (c % 4), 2 * (c % 4) + 1]
        xp = np.concatenate([x[b], meta, np.zeros((128 - NM, D), f32)], 0)
        qaug, kaug = attn_consts(heads, nxt)
        tab = np.zeros((2, 1, 1024), f32)
        for hi, h in enumerate(heads):
            sl = 2.0 ** (-(h + 1))
            for m, k in mults[hi].items():
                tab[hi, 0, k] = sl * m
        hw = lambda off: np.ascontiguousarray(np.stack([w_in[:, off + h * 128: off + (h + 1) * 128] for h in heads]))
        maps.append(dict(xp=xp, wq=hw(0), wk=hw(D), wv=hw(2 * D), lamv=lamv, subg=subg, qaug=qaug, kaug=kaug, ctab=tab, ident=ident))
    resA = _run(ncA, maps)
    o_full = np.zeros((Bn, (nxt + 1) * 128, D), f32)
    for c in range(8):
        b = c // 4; h0 = 2 * (c % 4)
        o_full[b][:, h0 * 128:(h0 + 2) * 128] = resA[c]["o"]
    del resA, maps
    TQ = S // 4; MQ = NM // 4
    ntB = TQ // 128 + 1
    ncB = build_phaseB(ntB)
    maps = []
    for c in range(8):
        b = c // 4; q = c % 4
        o_rows = np.zeros((ntB * 128, D), f32); h_rows = np.zeros((ntB * 128, D), f32)
        o_rows[:TQ] = o_full[b][q * TQ:(q + 1) * TQ]; o_rows[TQ:TQ + MQ] = o_full[b][S + q * MQ:S + (q + 1) * MQ]
        h_rows[:TQ] = x[b][q * TQ:(q + 1) * TQ]; h_rows[TQ:TQ + MQ] = meta[q * MQ:(q + 1) * MQ]
        maps.append(dict(o=o_rows, h0=h_rows, wo=g("attn_w_o")[0], wg=g("ffn_w_gate")[0], wu=g("ffn_w_up")[0], wd=g("ffn_w_down")[0],
                         lng=g("ln_g")[0], lnb=g("ln_b")[0], ident=ident))
    resB = _run(ncB, maps)
    seq = np.zeros((Bn, L, D), f32)
    for c in range(8):
        b = c // 4; q = c % 4
        seq[b][NM + q * TQ:NM + (q + 1) * TQ] = resB[c]["h1"][:TQ]
        seq[b][q * MQ:(q + 1) * MQ] = resB[c]["h1"][TQ:TQ + MQ]
    del resB, maps, o_full
    nch = (L + 63) // 64; Tp = nch * 64
    ncC = build_phaseC(nch, 8)
    mu = g("rw_mu")[0]
    seg = np.ones((1, 512), f32); seg[0, ::64] = 0
    maps = []
    for c in range(8):
        b = c // 4; d = (c % 4) // 2; hg = c % 2
        cs = slice(hg * 512, (hg + 1) * 512)
        sq = seq[b] if d == 0 else seq[b][::-1]
        xpad = np.zeros((Tp + 2, D), f32); xpad[1:1 + L] = sq
        mu_in = np.concatenate([mu[d], mu[1 - d]], 0)
        chv = np.zeros((64, 8, 8), f32)
        for i, v in enumerate((g("rw_w0")[0][d], g("rw_a0")[0][d], g("rw_k_k")[0], g("rw_k_a")[0])):
            chv[:, :, i] = v[cs].reshape(8, 64).T
        wrkv = g("rw_w_rkv")[0]
        maps.append(dict(xT=np.ascontiguousarray(xpad.T), mu=np.ascontiguousarray(mu_in.T),
                         wr=np.ascontiguousarray(wrkv[0][:, cs]), wk=np.ascontiguousarray(wrkv[1][:, cs]), wv=np.ascontiguousarray(wrkv[2][:, cs]),
                         w1=g("rw_w1")[0][d], w2=np.ascontiguousarray(g("rw_w2")[0][d][:, cs]),
                         a1=g("rw_a1")[0][d], a2=np.ascontiguousarray(g("rw_a2")[0][d][:, cs]),
                         g1=g("rw_g1")[0], g2=np.ascontiguousarray(g("rw_g2")[0][:, cs]),
                         chv=chv, msk=rw_consts(), seg=seg, ident=ident))
    resC = _run(ncC, maps)
    Y = np.zeros((Bn, 2, L, D), f32); AUX = np.zeros((Bn, 2, 4, L, D), f32)
    for c in range(8):
        b = c // 4; d = (c % 4) // 2; hg = c % 2
        cs = slice(hg * 512, (hg + 1) * 512)
        yy = resC[c]["y"][:L]; ax = resC[c]["aux"][:, :, :L]
        if d == 1:
            yy = yy[::-1]; ax = ax[:, :, ::-1]
        Y[b, d][:, cs] = yy
        for i in range(4):
            AUX[b, d, i][:, cs] = ax[i].T
    del resC, maps
    ncD = build_phaseD(TQ // 128)
    vecs = np.stack([g("rw_lnx_g")[0], g("rw_lnx_b")[0], g("rw_r_k")[0].reshape(D), g("ln_g")[1, 0], g("ln_b")[1, 0], g("ln_g")[1, 1], g("ln_b")[1, 1]])
    wrt = np.ascontiguousarray(g("moe_w_router")[0].T); brt = g("moe_b_router")[0][None, :]
    wgE = g("moe_w_gate")[0]; wuE = g("moe_w_up")[0]; wdE = g("moe_w_down")[0]; woR = g("rw_w_o")[0]
    maps = []
    for c in range(8):
        b = c // 4; q = c % 4
        rows = slice(NM + q * TQ, NM + (q + 1) * TQ)
        cp = lambda a: np.ascontiguousarray(a[rows])
        maps.append(dict(yf=cp(Y[b, 0]), yb=cp(Y[b, 1]), r=cp(AUX[b, 0, 0]), kdf=cp(AUX[b, 0, 1]), kdb=cp(AUX[b, 1, 1]), v=cp(AUX[b, 0, 2]), g=cp(AUX[b, 0, 3]),
                         h1=cp(seq[b]), wo=woR, vecs=vecs, wrt=wrt, brt=brt, wg=wgE, wu=wuE, wd=wdE, ident=ident))
    resD = _run(ncD, maps)
    out = np.zeros((Bn, S, D), f32)
    for c in range(8):
        b = c // 4; q = c % 4
        out[b, q * TQ:(q + 1) * TQ] = resD[c]["out"]
    return out
```

```python
import numpy as np
from contextlib import ExitStack
import concourse.bass as bass
import concourse.mybir as mybir
from concourse.bass_utils import run_bass_kernel_spmd

F32 = mybir.dt.float32
BF16 = mybir.dt.bfloat16
ALU = mybir.AluOpType
AF = mybir.ActivationFunctionType
AX = mybir.AxisListType


NUM_DEVICES = None


class Sem:
    def __init__(self, h, is_dma=False):
        self.h = h
        self.cnt = 0
        self.is_dma = is_dma


class Buf:
    def __init__(self, name):
        self.name = name
        self.w = None
        self.r = []


class Prog:
    ENG = ("pe", "act", "dve", "pool", "sp")

    def __init__(self):
        self.nc = bass.Bass("TRN2", target_bir_lowering=False, num_devices=NUM_DEVICES)
        self.es = ExitStack()
        nc = self.nc
        self.eng = {"pe": nc.tensor, "act": nc.scalar, "dve": nc.vector, "pool": nc.gpsimd, "sp": nc.sync}
        self.esem = {e: Sem(self.es.enter_context(nc.semaphore("sem_" + e))) for e in self.ENG}
        self.waited = {e: {} for e in self.ENG}
        self.nsem = 0
        self.dsems = []
        self.root_es = self.es
        self.ninst = 0

    def dram(self, name, shape, dt, kind):
        return self.nc.dram_tensor(name, list(shape), dt, kind=kind).ap()

    def _uniq(self, name):
        self.nalloc = getattr(self, "nalloc", 0) + 1
        return "%s_u%d" % (name, self.nalloc)

    def sb(self, name, shape, dt):
        return self.es.enter_context(self.nc.sbuf_tensor(self._uniq(name), list(shape), dt))

    def ps(self, name, shape, dt):
        return self.es.enter_context(self.nc.psum_tensor(self._uniq(name), list(shape), dt))

    def newsem(self, name=None):
        self.nsem += 1
        sm = self._mksem(name)
        sm.is_dma = True
        self.dsems.append(sm)
        return sm

    def _mksem(self, name):
        return Sem(self.root_es.enter_context(self.nc.semaphore(name or ("ds%d" % self.nsem))))

    def _wait(self, e, ev, raw=True):
        sem, val = ev
        if sem is self.esem[e] and (e == "pe" or not raw):
            return
        if sem.is_dma:
            val = sem.cnt
        if self.waited[e].get(sem, 0) >= val:
            return
        self.waited[e][sem] = val
        self.eng[e].wait_ge(sem.h, val)

    def _deps(self, e, reads, writes):
        for b in reads:
            if b.w is not None:
                self._wait(e, b.w)
        for b in writes:
            if b.w is not None:
                self._wait(e, b.w, raw=False)
            for ev in b.r:
                self._wait(e, ev, raw=False)

    def _commit(self, ev, reads, writes):
        for b in reads:
            b.r.append(ev)
            if len(b.r) > 64:
                b.r = b.r[-64:]
        for b in writes:
            b.w = ev
            b.r = []

    def op(self, e, fn, reads=(), writes=(), accum=False):
        self._deps(e, reads, writes)
        s = self.esem[e]
        inst = fn(self.eng[e])
        s.cnt += 1
        inst.then_inc(s.h, 1)
        self._commit((s, s.cnt), reads, writes)
        self.ninst += 1
        return inst

    def dma(self, q, out, in_, sem, reads=(), writes=(), **kw):
        if sem is None:
            b0 = writes[0]
            if getattr(b0, "dsem", None) is None:
                b0.dsem = self.newsem()
            sem = b0.dsem
        self._deps(q, reads, writes)
        inst = self.eng[q].dma_start(out=out, in_=in_, **kw)
        sem.cnt += 16
        inst.then_inc(sem.h, 16)
        self._commit((sem, sem.cnt), reads, writes)
        self.ninst += 1
        return inst

    def barrier(self):
        evs = [(s, s.cnt) for s in list(self.esem.values()) + self.dsems if s.cnt > 0]
        for e in self.ENG:
            for ev in evs:
                self._wait(e, ev)

    def scope(self):
        prog = self
        class _S:
            def __enter__(s2):
                s2.old = prog.es
                prog.es = ExitStack()
                return prog
            def __exit__(s2, *a):
                prog.es.close()
                prog.es = s2.old
                return False
        return _S()

    def finish(self, bufs, e="sp"):
        for b in bufs:
            if b.w is not None:
                self._wait(e, b.w)
            for ev in b.r:
                self._wait(e, ev)

    def close(self):
        self.es.close()
        return self.nc

import math
ALPHA = 4.0 ** 0.25
LN_EPS = 1e-5

class Common:
    def __init__(self, P):
        self.P = P

def load_w_bf16(P, name, w_ap, K, N, sem):
    kc = K // 128
    t = P.sb(name, [128, kc, N], BF16)
    B = Buf(name)
    for c in range(kc):
        P.dma("pool", t[:, c, :], w_ap[c * 128:(c + 1) * 128, :], sem, writes=[B])
    return t, B

def load_bcast(P, name, v_ap, N, sem):
    t = P.sb(name, [128, N], F32)
    B = Buf(name)
    P.dma("sp", t[:], v_ap.to_broadcast([128, N]), sem, writes=[B])
    return t, B

def layer_norm(P, x, Bx, g, Bg, b, Bb, scr, Bscr, out, Bout, obf=None, Bobf=None, D=1024):
    nh = D // 512
    st, mv = scr
    for i in range(nh):
        P.op("dve", lambda e, i=i: e.bn_stats(st[:, i * 6:(i + 1) * 6], x[:, i * 512:(i + 1) * 512]), reads=[Bx], writes=[Bscr])
    P.op("dve", lambda e: e.bn_aggr(mv[:, 0:2], st[:, 0:6 * nh]), reads=[Bscr], writes=[Bscr])
    P.op("act", lambda e: e.activation(mv[:, 2:3], mv[:, 1:2], AF.Ln, bias=LN_EPS, scale=1.0), reads=[Bscr], writes=[Bscr])
    P.op("act", lambda e: e.activation(mv[:, 3:4], mv[:, 2:3], AF.Exp, scale=-0.5), reads=[Bscr], writes=[Bscr])
    P.op("dve", lambda e: e.tensor_scalar(out[:, :], x[:, :], mv[:, 0:1], mv[:, 3:4], ALU.subtract, ALU.mult), reads=[Bx, Bscr], writes=[Bout])
    P.op("pool", lambda e: e.tensor_tensor(out[:, :], out[:, :], g[:, :], ALU.mult), reads=[Bout, Bg], writes=[Bout])
    P.op("pool", lambda e: e.tensor_tensor(out[:, :], out[:, :], b[:, :], ALU.add), reads=[Bout, Bb], writes=[Bout])
    if obf is not None:
        P.op("act", lambda e: e.activation(obf[:, :], out[:, :], AF.Identity), reads=[Bout], writes=[Bobf])

def transpose_tile(P, src_bf, Bsrc, ident, Bid, pt, Bpt, dst, Bdst, tslot, nchunk=8, evac="act"):
    for c in range(nchunk):
        P.op("pe", lambda e, c=c: e.transpose(pt[:, c, :], src_bf[:, c * 128:(c + 1) * 128], ident[:, :]), reads=[Bsrc, Bid], writes=[Bpt])
    if evac == "act":
        P.op("act", lambda e: e.activation(dst[:, 0:nchunk, tslot * 128:(tslot + 1) * 128], pt[:, 0:nchunk, :], AF.Identity), reads=[Bpt], writes=[Bdst])
    else:
        P.op("dve", lambda e: e.tensor_copy(dst[:, 0:nchunk, tslot * 128:(tslot + 1) * 128], pt[:, 0:nchunk, :]), reads=[Bpt], writes=[Bdst])

def build_phaseB(ntiles, F=2816, D=1024):
    P = Prog(); nc = P.nc
    T = ntiles * 128
    FC = F // 128
    o_d = P.dram("o", [T, D], F32, "ExternalInput")
    h0_d = P.dram("h0", [T, D], F32, "ExternalInput")
    wo_d = P.dram("wo", [D, D], F32, "ExternalInput")
    wg_d = P.dram("wg", [D, F], F32, "ExternalInput")
    wu_d = P.dram("wu", [D, F], F32, "ExternalInput")
    wd_d = P.dram("wd", [F, D], F32, "ExternalInput")
    lng_d = P.dram("lng", [2, D], F32, "ExternalInput")
    lnb_d = P.dram("lnb", [2, D], F32, "ExternalInput")
    id_d = P.dram("ident", [128, 128], F32, "ExternalInput")
    out_d = P.dram("h1", [T, D], F32, "ExternalOutput")
    wsem = P.newsem("wsem")
    wo, Bwo = load_w_bf16(P, "wo_sb", wo_d, D, D, wsem)
    wg, Bwg = load_w_bf16(P, "wg_sb", wg_d, D, F, wsem)
    wu, Bwu = load_w_bf16(P, "wu_sb", wu_d, D, F, wsem)
    wd, Bwd = load_w_bf16(P, "wd_sb", wd_d, F, D, wsem)
    csem = P.newsem("csem")
    g1, Bg1 = load_bcast(P, "g1", lng_d[0:1, :], D, csem)
    b1, Bb1 = load_bcast(P, "b1", lnb_d[0:1, :], D, csem)
    g2, Bg2 = load_bcast(P, "g2", lng_d[1:2, :], D, csem)
    b2, Bb2 = load_bcast(P, "b2", lnb_d[1:2, :], D, csem)
    ident = P.sb("ident_sb", [128, 128], BF16); Bid = Buf("ident")
    P.dma("pool", ident[:], id_d[:, :], csem, writes=[Bid])
    GT = 2
    W = GT * 128
    o32 = P.sb("o32", [128, D], F32); Bo32 = Buf("o32"); o32sem = P.newsem()
    obf = P.sb("obf", [128, D], BF16); Bobf = Buf("obf")
    res = [P.sb("res%d" % i, [128, D], F32) for i in range(GT)]; Bres = [Buf("res%d" % i) for i in range(GT)]
    ressem = [P.newsem() for i in range(GT)]
    hbf = P.sb("hbf", [128, D], BF16); Bhbf = Buf("hbf")
    xT = P.sb("xT", [128, 8, W], BF16); BxT = Buf("xT")
    hidT = P.sb("hidT", [128, FC, W], BF16); BhidT = Buf("hidT")
    sg = [P.sb("sg%d" % i, [128, W], F32) for i in range(2)]; Bsg = [Buf("sg%d" % i) for i in range(2)]
    st = P.sb("lnst", [128, 12], F32); mv = P.sb("lnmv", [128, 4], F32); Bscr = Buf("lnscr")
    outt = P.sb("outt", [128, D], F32); Boutt = Buf("outt"); outsem = P.newsem()
    pt = [P.ps("pt%d" % i, [128, 8, 128], BF16) for i in range(2)]; Bpt = [Buf("pt%d" % i) for i in range(2)]
    pm = [P.ps("pm%d" % i, [128, 512], F32) for i in range(2)]; Bpm = [Buf("pm%d" % i) for i in range(2)]
    pg = [P.ps("pg%d" % i, [128, 512], F32) for i in range(2)]; Bpg = [Buf("pg%d" % i) for i in range(2)]
    pu = [P.ps("pu%d" % i, [128, 512], F32) for i in range(2)]; Bpu = [Buf("pu%d" % i) for i in range(2)]
    Bout_d = Buf("out_d")
    ptc = 0
    ngroups = (ntiles + GT - 1) // GT
    for gi in range(ngroups):
        tiles = list(range(gi * GT, min(ntiles, (gi + 1) * GT)))
        w = len(tiles) * 128
        for j, t in enumerate(tiles):
            P.dma("sp", o32[:], o_d[t * 128:(t + 1) * 128, :], o32sem, writes=[Bo32])
            P.dma("sp", res[j][:], h0_d[t * 128:(t + 1) * 128, :], ressem[j], writes=[Bres[j]])
            P.op("act", lambda e: e.activation(obf[:, :], o32[:, :], AF.Identity), reads=[Bo32], writes=[Bobf])
            transpose_tile(P, obf, Bobf, ident, Bid, pt[ptc % 2], Bpt[ptc % 2], xT, BxT, j); ptc += 1
        for j, t in enumerate(tiles):
            for nh in range(2):
                for c in range(8):
                    P.op("pe", lambda e, c=c, nh=nh, j=j: e.matmul(pm[nh][:, :], xT[:, c, j * 128:(j + 1) * 128], wo[:, c, nh * 512:(nh + 1) * 512], start=(c == 0), stop=(c == 7)),
                         reads=[BxT, Bwo], writes=[Bpm[nh]])
                P.op("dve", lambda e, nh=nh, j=j: e.scalar_tensor_tensor(res[j][:, nh * 512:(nh + 1) * 512], res[j][:, nh * 512:(nh + 1) * 512], ALPHA, pm[nh][:, :], ALU.mult, ALU.add),
                     reads=[Bres[j], Bpm[nh]], writes=[Bres[j]])
            layer_norm(P, res[j], Bres[j], g1, Bg1, b1, Bb1, (st, mv), Bscr, res[j], Bres[j], hbf, Bhbf)
            transpose_tile(P, hbf, Bhbf, ident, Bid, pt[ptc % 2], Bpt[ptc % 2], xT, BxT, j); ptc += 1
        for fc in range(FC):
            k = fc % 2
            for c in range(8):
                P.op("pe", lambda e, c=c, fc=fc, k=k: e.matmul(pg[k][:, 0:w], wg[:, c, fc * 128:(fc + 1) * 128], xT[:, c, 0:w], start=(c == 0), stop=(c == 7)),
                     reads=[BxT, Bwg], writes=[Bpg[k]])
            for c in range(8):
                P.op("pe", lambda e, c=c, fc=fc, k=k: e.matmul(pu[k][:, 0:w], wu[:, c, fc * 128:(fc + 1) * 128], xT[:, c, 0:w], start=(c == 0), stop=(c == 7)),
                     reads=[BxT, Bwu], writes=[Bpu[k]])
            P.op("act", lambda e, k=k: e.activation(sg[k][:, 0:w], pg[k][:, 0:w], AF.Silu), reads=[Bpg[k]], writes=[Bsg[k]])
            P.op("dve", lambda e, k=k, fc=fc: e.tensor_tensor(hidT[:, fc, 0:w], sg[k][:, 0:w], pu[k][:, 0:w], ALU.mult), reads=[Bsg[k], Bpu[k]], writes=[BhidT])
        for j, t in enumerate(tiles):
            for nh in range(2):
                for fc in range(FC):
                    P.op("pe", lambda e, fc=fc, nh=nh, j=j: e.matmul(pm[nh][:, :], hidT[:, fc, j * 128:(j + 1) * 128], wd[:, fc, nh * 512:(nh + 1) * 512], start=(fc == 0), stop=(fc == FC - 1)),
                         reads=[BhidT, Bwd], writes=[Bpm[nh]])
                P.op("dve", lambda e, nh=nh, j=j: e.scalar_tensor_tensor(res[j][:, nh * 512:(nh + 1) * 512], res[j][:, nh * 512:(nh + 1) * 512], ALPHA, pm[nh][:, :], ALU.mult, ALU.add),
                     reads=[Bres[j], Bpm[nh]], writes=[Bres[j]])
            layer_norm(P, res[j], Bres[j], g2, Bg2, b2, Bb2, (st, mv), Bscr, outt, Boutt)
            P.dma("sp", out_d[t * 128:(t + 1) * 128, :], outt[:], outsem, reads=[Boutt], writes=[Bout_d])
    P.finish([Bout_d])
    print("phaseB insts", P.ninst)
    return P.close()


import math
SUBLN_EPS = 1e-5
import os
NJUNK = int(os.environ.get('NJUNK', '0'))
NEG = -30000.0

def attn_consts(heads, nxt):
    T = (nxt + 1) * 128
    qaug = np.zeros((2, 5, 512), np.float32)
    u = np.arange(512)
    qaug[0] = np.stack([u // 16, u % 16, np.ones(512), np.ones(512), np.ones(512)])
    qaug[1] = np.stack([-(u // 16), -(u % 16), -np.ones(512), -np.ones(512), np.ones(512)])
    kaug = np.zeros((len(heads), 5, T), np.float32)
    v = np.arange(T) % 128
    for i, h in enumerate(heads):
        sl = 2.0 ** (-(h + 1))
        kaug[i, 0] = -16 * sl
        kaug[i, 1] = -sl
        kaug[i, 2] = 16 * sl * (v // 16)
        kaug[i, 3] = sl * (v % 16)
        kaug[i, 4, nxt * 128 + 16:] = NEG
    return qaug, kaug

def build_phaseA(NH, nxt, NCT=1024, D=1024):
    P = Prog(); nc = P.nc
    NT = nxt + 1
    T = NT * 128
    x_d = P.dram("xp", [T, D], F32, "ExternalInput")
    wq_d = P.dram("wq", [NH, D, 128], F32, "ExternalInput")
    wk_d = P.dram("wk", [NH, D, 128], F32, "ExternalInput")
    wv_d = P.dram("wv", [NH, D, 128], F32, "ExternalInput")
    lam_d = P.dram("lamv", [4, 64], F32, "ExternalInput")
    sg_d = P.dram("subg", [1, 128], F32, "ExternalInput")
    qaug_d = P.dram("qaug", [2, 5, 512], F32, "ExternalInput")
    kaug_d = P.dram("kaug", [NH, 5, T], F32, "ExternalInput")
    ctab_d = P.dram("ctab", [NH, 1, NCT], F32, "ExternalInput")
    id_d = P.dram("ident", [128, 128], F32, "ExternalInput")
    o_d = P.dram("o", [T, NH * 128], F32, "ExternalOutput")
    Bout_d = Buf("o_d")
    csem = None
    ident = P.sb("ident_sb", [128, 128], BF16); Bid = Buf("ident")
    P.dma("pool", ident[:], id_d[:, :], csem, writes=[Bid])
    lv = P.sb("lv", [128, 4, 64], F32); Blv = Buf("lv")
    for i in range(4):
        P.dma("sp", lv[:, i, :], lam_d[i:i + 1, :].to_broadcast([128, 64]), csem, writes=[Blv])
    lsc = P.sb("lsc", [128, 8], F32); Blsc = Buf("lsc")
    lt = P.sb("lt", [128, 2, 64], F32)
    P.op("dve", lambda e: e.tensor_tensor(lt[:, 0, :], lv[:, 0, :], lv[:, 1, :], ALU.mult), reads=[Blv], writes=[Blsc])
    P.op("dve", lambda e: e.tensor_tensor(lt[:, 1, :], lv[:, 2, :], lv[:, 3, :], ALU.mult), reads=[Blv], writes=[Blsc])
    P.op("dve", lambda e: e.reduce_sum(lsc[:, 0:1], lt[:, 0, :], AX.X), reads=[Blsc], writes=[Blsc])
    P.op("dve", lambda e: e.reduce_sum(lsc[:, 1:2], lt[:, 1, :], AX.X), reads=[Blsc], writes=[Blsc])
    P.op("act", lambda e: e.activation(lsc[:, 2:4], lsc[:, 0:2], AF.Exp), reads=[Blsc], writes=[Blsc])
    P.op("dve", lambda e: e.tensor_tensor(lsc[:, 4:5], lsc[:, 3:4], lsc[:, 2:3], ALU.subtract), reads=[Blsc], writes=[Blsc])
    P.op("dve", lambda e: e.tensor_scalar(lsc[:, 5:6], lsc[:, 4:5], -0.2, None, ALU.add), reads=[Blsc], writes=[Blsc])
    neglam = lsc[:, 5:6]
    gsc, Bgsc = load_bcast(P, "gsc", sg_d[0:1, :], 128, csem)
    P.op("dve", lambda e: e.tensor_scalar(gsc[:, :], gsc[:, :], 0.8, None, ALU.mult), reads=[Bgsc], writes=[Bgsc])
    QT = P.sb("QT", [128, T], BF16); BQT = Buf("QT")
    KT = [P.sb("KT%d" % c, [69, T], BF16) for c in range(2)]; BKT = [Buf("KT%d" % c) for c in range(2)]
    VA = P.sb("VA", [128, NT, 129], BF16); BVA = Buf("VA")
    P.op("pool", lambda e: e.memset(VA[:, :, 128:129], 1.0), writes=[BVA])
    ctab = P.sb("ctab_sb", [128, NCT], F32); Bctab = Buf("ctab")
    QA = [[P.sb("QA%d%d" % (c, lr), [69, 512], BF16) for lr in range(2)] for c in range(2)]
    BQA = [[Buf("QA%d%d" % (c, lr)) for lr in range(2)] for c in range(2)]
    for c in range(2):
        for lr in range(2):
            P.dma("pool", QA[c][lr][64:69, :], qaug_d[lr, :, :], csem, writes=[BQA[c][lr]])
    wq = P.sb("wq_sb", [128, 8, 128], BF16); wk = P.sb("wk_sb", [128, 8, 128], BF16); wv = P.sb("wv_sb", [128, 8, 128], BF16)
    Bw = Buf("w_head"); wsem = None
    ctab_vals = [dict() for _ in range(NH)]
    def ccol(hi, val):
        d = ctab_vals[hi]
        if val not in d:
            d[val] = len(d)
            assert len(d) <= NCT
        return d[val]
    base = lambda tile: (0 if tile == nxt else 16 + 128 * tile)
    for hi in range(NH):
        for (dst, src) in ((wq, wq_d), (wk, wk_d), (wv, wv_d)):
            for c in range(8):
                P.dma("pool", dst[:, c, :], src[hi, c * 128:(c + 1) * 128, :], wsem, writes=[Bw])
        P.dma("sp", ctab[:], ctab_d[hi, 0:1, :].to_broadcast([128, NCT]), csem, writes=[Bctab])
        for c in range(2):
            P.dma("pool", KT[c][64:69, :], kaug_d[hi, :, :], csem, writes=[BKT[c]])
        P.barrier()
        with P.scope():
            x32 = [P.sb("x32_%d" % i, [128, D], F32) for i in range(2)]; Bx32 = [Buf("x32") for i in range(2)]; xsem = [P.newsem() for i in range(2)]
            xbf = [P.sb("xbf_%d" % i, [128, D], BF16) for i in range(2)]; Bxbf = [Buf("xbf") for i in range(2)]
            xT = [P.sb("xT_%d" % i, [128, 8, 512], BF16) for i in range(2)]; BxT = [Buf("xT") for i in range(2)]
            pt = [P.ps("pt%d" % i, [128, 8, 128], BF16) for i in range(2)]; Bpt = [Buf("pt") for i in range(2)]
            pq = [P.ps("pq%d" % i, [128, 512], F32) for i in range(2)]; Bpq = [Buf("pq") for i in range(2)]
            pk = [P.ps("pk%d" % i, [128, 512], F32) for i in range(2)]; Bpk = [Buf("pk") for i in range(2)]
            pv = [P.ps("pv%d" % i, [128, 512], F32) for i in range(2)]; Bpv = [Buf("pv") for i in range(2)]
            tcnt = 0
            ngr = (NT + 3) // 4
            for gi in range(ngr):
                tiles = list(range(gi * 4, min(NT, gi * 4 + 4)))
                w = len(tiles) * 128
                k2 = gi % 2
                for j, t in enumerate(tiles):
                    s = tcnt % 2; tcnt += 1
                    P.dma("sp", x32[s][:], x_d[t * 128:(t + 1) * 128, :], xsem[s], writes=[Bx32[s]])
                    P.op("dve" if j % 2 else "pool", lambda e, s=s: e.tensor_copy(xbf[s][:, :], x32[s][:, :]), reads=[Bx32[s]], writes=[Bxbf[s]])
                    transpose_tile(P, xbf[s], Bxbf[s], ident, Bid, pt[s], Bpt[s], xT[k2], BxT[k2], j, evac="act" if j % 2 else "dve")
                tok0 = tiles[0] * 128
                for c in range(8):
                    P.op("pe", lambda e, c=c: e.matmul(pq[k2][:, 0:w], wq[:, c, :], xT[k2][:, c, 0:w], start=(c == 0), stop=(c == 7)), reads=[Bw, BxT[k2]], writes=[Bpq[k2]])
                P.op("act", lambda e: e.activation(QT[:, tok0:tok0 + w], pq[k2][:, 0:w], AF.Identity, scale=0.125), reads=[Bpq[k2]], writes=[BQT])
                for cc in range(2):
                    for c in range(8):
                        P.op("pe", lambda e, c=c, cc=cc: e.matmul(pk[k2][0:64, cc * 0 + 0:w] if False else pk[k2][0:64, 0:w], wk[:, c, cc * 64:(cc + 1) * 64], xT[k2][:, c, 0:w], start=(c == 0), stop=(c == 7)), reads=[Bw, BxT[k2]], writes=[Bpk[k2]])
                    P.op("dve", lambda e, cc=cc: e.tensor_copy(KT[cc][0:64, tok0:tok0 + w], pk[k2][0:64, 0:w]), reads=[Bpk[k2]], writes=[BKT[cc]])
                for j, t in enumerate(tiles):
                    for c in range(8):
                        P.op("pe", lambda e, c=c, j=j: e.matmul(pv[k2][:, j * 128:(j + 1) * 128], xT[k2][:, c, j * 128:(j + 1) * 128], wv[:, c, :], start=(c == 0), stop=(c == 7)), reads=[Bw, BxT[k2]], writes=[Bpv[k2]])
                P.op("act", lambda e: e.activation(VA[:, tiles[0]:tiles[0] + len(tiles), 0:128], pv[k2][:, 0:w].rearrange("p (t d) -> p t d", d=128), AF.Identity), reads=[Bpv[k2]], writes=[BVA])
        P.barrier()
        with P.scope():
            psS = [[P.ps("psS%d%d" % (c, i), [128, 512], F32) for i in range(2)] for c in range(2)]
            BpsS = [[Buf("psS") for i in range(2)] for c in range(2)]
            oz = [[P.ps("oz%d%d" % (c, i), [128, 512], F32) for i in range(2)] for c in range(2)]
            Boz = [Buf("oz%d" % c) for c in range(2)]
            PT = [[P.sb("PT%d%d" % (c, i), [128, 512], BF16) for i in range(3)] for c in range(2)]
            BPT = [[Buf("PT") for i in range(3)] for c in range(2)]
            srt = [P.sb("srt%d" % c, [128, 512], F32) for c in range(2)]; Bsrt = [Buf("srt") for c in range(2)]
            fz = P.sb("fz", [128, 16], F32); Bfz = Buf("fz")
            fo = [P.sb("fo%d" % i, [128, 128], F32) for i in range(2)]; Bfo = [Buf("fo") for i in range(2)]
            fo2 = P.sb("fo2", [128, 128], F32); Bfo2 = Buf("fo2")
            osb = [P.sb("osb%d" % i, [128, 128], F32) for i in range(2)]; Bosb = [Buf("osb") for i in range(2)]; osem = [P.newsem() for i in range(2)]
            Bjunk = Buf("junk")
            qtiles = [(j * 4, 4) for j in range(nxt // 4)] + [(nxt, 1)]
            assert nxt % 4 == 0
            it = 0; fcnt = 0
            for (qt0, qn) in qtiles:
                Wq = qn * 128
                qbase = base(qt0)
                for c in range(2):
                    for lr in range(2):
                        P.op("pool" if lr else "dve", lambda e, c=c, lr=lr: e.tensor_copy(QA[c][lr][0:64, 0:Wq], QT[c * 64:(c + 1) * 64, qt0 * 128:qt0 * 128 + Wq]), reads=[BQT], writes=[BQA[c][lr]])
                order = list(range(NT))
                def mk(ki, i, it):
                    Dq = qbase - base(i)
                    if Dq >= 127: typ = 0
                    elif Dq + Wq - 1 <= 0: typ = 1
                    else: typ = 2
                    return dict(ki=ki, i=i, it=it, Dq=Dq, typ=typ)
                def issue_S(inf):
                    i, typ, it_ = inf["i"], inf["typ"], inf["it"]
                    for c in range(2):
                        s2 = it_ % 2
                        ps = psS[c][s2]; Bps = BpsS[c][s2]
                        if typ < 2:
                            P.op("pe", lambda e, c=c, i=i, typ=typ, ps=ps: e.matmul(ps[:, 0:Wq], KT[c][0:69, i * 128:(i + 1) * 128], QA[c][typ][0:69, 0:Wq], start=True, stop=True), reads=[BKT[c], BQA[c][typ]], writes=[Bps])
                        else:
                            ps2 = psS[c][1 - s2]; Bps2 = BpsS[c][1 - s2]
                            P.op("pe", lambda e, c=c, i=i, ps=ps: e.matmul(ps[:, 0:Wq], KT[c][0:69, i * 128:(i + 1) * 128], QA[c][0][0:69, 0:Wq], start=True, stop=True), reads=[BKT[c], BQA[c][0]], writes=[Bps])
                            P.op("pe", lambda e, c=c, i=i, ps2=ps2: e.matmul(ps2[:, 0:Wq], KT[c][0:69, i * 128:(i + 1) * 128], QA[c][1][0:69, 0:Wq], start=True, stop=True), reads=[BKT[c], BQA[c][1]], writes=[Bps2])
                def issue_rest(inf):
                    i, typ, it_, Dq, ki = inf["i"], inf["typ"], inf["it"], inf["Dq"], inf["ki"]
                    for c in range(2):
                        s2 = it_ % 2; s3 = it_ % 3
                        ps = psS[c][s2]; Bps = BpsS[c][s2]
                        pt_ = PT[c][s3]; Bp = BPT[c][s3]
                        if typ < 2:
                            col = ccol(hi, -abs(Dq))
                            P.op("act", lambda e, ps=ps, pt_=pt_, col=col: e.activation(pt_[:, 0:Wq], ps[:, 0:Wq], AF.Exp, bias=ctab[:, col:col + 1], scale=1.0), reads=[Bps, Bctab], writes=[Bp])
                        else:
                            ps2 = psS[c][1 - s2]; Bps2 = BpsS[c][1 - s2]
                            colL = ccol(hi, -Dq); colR = ccol(hi, Dq)
                            P.op("act", lambda e, c=c, ps2=ps2, colR=colR: e.activation(srt[c][:, 0:Wq], ps2[:, 0:Wq], AF.Identity, bias=ctab[:, colR:colR + 1], scale=1.0), reads=[Bps2, Bctab], writes=[Bsrt[c]])
                            P.op("dve", lambda e, c=c, ps=ps, colL=colL: e.scalar_tensor_tensor(srt[c][:, 0:Wq], ps[:, 0:Wq], ctab[:, colL:colL + 1], srt[c][:, 0:Wq], ALU.add, ALU.min), reads=[Bps, Bsrt[c], Bctab], writes=[Bsrt[c]])
                            P.op("act", lambda e, c=c, pt_=pt_: e.activation(pt_[:, 0:Wq], srt[c][:, 0:Wq], AF.Exp), reads=[Bsrt[c]], writes=[Bp])
                    for c in range(2):
                        s3 = it_ % 3
                        pt_ = PT[c][s3]; Bp = BPT[c][s3]
                        for sub in range(qn):
                            P.op("pe", lambda e, c=c, sub=sub, i=i, pt_=pt_, ki=ki: e.matmul(oz[c][sub // 2][:, (sub % 2) * 129:(sub % 2) * 129 + 129], pt_[:, sub * 128:(sub + 1) * 128], VA[:, i, :], start=(ki == 0 and sub % 2 == 0), stop=(ki == NT - 1), skip_group_check=True), reads=[Bp, BVA], writes=[Boz[c]])
                infos = []
                for ki, i in enumerate(order):
                    infos.append(mk(ki, i, it)); it += 1
                def can_ahead(a, b):
                    return a["typ"] < 2 and b["typ"] < 2
                issued = set()
                for n, inf in enumerate(infos):
                    if n not in issued:
                        issue_S(inf); issued.add(n)
                    if n + 1 < len(infos) and can_ahead(inf, infos[n + 1]):
                        issue_S(infos[n + 1]); issued.add(n + 1)
                    issue_rest(inf)
                    for jj in range(NJUNK):
                        P.op("pe", lambda e, jj=jj: e.matmul(oz[jj % 2][1][:, 320:384], ident[:, :], ident[:, 0:64], start=False, stop=True, skip_group_check=True), reads=[Bid], writes=[Bjunk])
                for sub in range(qn):
                    f = fcnt % 2; fcnt += 1
                    o0 = oz[0][sub // 2][:, (sub % 2) * 129:(sub % 2) * 129 + 129]; o1 = oz[1][sub // 2][:, (sub % 2) * 129:(sub % 2) * 129 + 129]
                    P.op("dve", lambda e, o0=o0: e.reciprocal(fz[:, 0:1], o0[:, 128:129]), reads=[Boz[0]], writes=[Bfz])
                    P.op("dve", lambda e, o1=o1: e.reciprocal(fz[:, 1:2], o1[:, 128:129]), reads=[Boz[1]], writes=[Bfz])
                    P.op("dve", lambda e: e.tensor_tensor(fz[:, 2:3], fz[:, 1:2], neglam, ALU.mult), reads=[Bfz, Blsc], writes=[Bfz])
                    P.op("dve", lambda e, o0=o0, f=f: e.tensor_scalar(fo[f][:, :], o0[:, 0:128], fz[:, 0:1], None, ALU.mult), reads=[Boz[0], Bfz], writes=[Bfo[f]])
                    P.op("dve", lambda e, o1=o1, f=f: e.scalar_tensor_tensor(fo[f][:, :], o1[:, 0:128], fz[:, 2:3], fo[f][:, :], ALU.mult, ALU.add), reads=[Boz[1], Bfz, Bfo[f]], writes=[Bfo[f]])
                    P.op("act", lambda e, f=f: e.activation(fo2[:, :], fo[f][:, :], AF.Square, accum_out=fz[:, 3:4]), reads=[Bfo[f]], writes=[Bfo2, Bfz])
                    P.op("act", lambda e: e.activation(fz[:, 4:5], fz[:, 3:4], AF.Ln, bias=SUBLN_EPS, scale=1.0 / 128), reads=[Bfz], writes=[Bfz])
                    P.op("act", lambda e: e.activation(fz[:, 5:6], fz[:, 4:5], AF.Exp, scale=-0.5), reads=[Bfz], writes=[Bfz])
                    P.op("dve", lambda e, f=f: e.scalar_tensor_tensor(osb[f][:, :], fo[f][:, :], fz[:, 5:6], gsc[:, :], ALU.mult, ALU.mult), reads=[Bfo[f], Bfz, Bgsc], writes=[Bosb[f]])
                    tt = qt0 + sub
                    P.dma("sp", o_d[tt * 128:(tt + 1) * 128, hi * 128:(hi + 1) * 128], osb[f][:], osem[f], reads=[Bosb[f]], writes=[Bout_d])
        P.barrier()
    P.finish([Bout_d])
    print("phaseA insts", P.ninst)
    nc = P.close()
    return nc, ctab_vals

def ref_attn(xp, pos, valid, w_in, lam4, subg, heads):
    T = xp.shape[0]
    D = 1024
    outs = []
    lam = np.exp((lam4[0] * lam4[1]).sum()) - np.exp((lam4[2] * lam4[3]).sum()) + 0.2
    for h in heads:
        q = xp @ w_in[:, h * 128:(h + 1) * 128]
        k = xp @ w_in[:, D + h * 128:D + (h + 1) * 128]
        v = xp @ w_in[:, 2 * D + h * 128:2 * D + (h + 1) * 128]
        sl = 2.0 ** (-(h + 1))
        dist = np.abs(pos[:, None] - pos[None, :]).astype(np.float32)
        ps = []
        for c in range(2):
            s = q[:, c * 64:(c + 1) * 64] @ k[:, c * 64:(c + 1) * 64].T / 8 - sl * dist
            s = np.where(valid[None, :], s, -np.inf)
            s = s - s.max(-1, keepdims=True)
            p = np.exp(s); p /= p.sum(-1, keepdims=True)
            ps.append(p)
        a = ps[0] - lam * ps[1]
        o = a @ v
        o = o / np.sqrt((o * o).mean(-1, keepdims=True) + 1e-5) * subg * 0.8
        outs.append(o)
    return np.concatenate(outs, 1)


import math
STAGE = 9
SKIP = ''
C = 64
NEGH = -math.exp(-0.5)

def rw_consts():
    m = np.zeros((64, 128), np.float32)
    s = np.arange(64)[:, None]; t = np.arange(64)[None, :]
    m[:, 0:64] = (s < t); m[:, 64:128] = (s <= t)
    return m

def build_phaseC(nch, NHD=8, D=1024, GW=512):
    P = Prog(); nc = P.nc
    Tp = nch * C
    NCH = NHD * 64
    xT_d = P.dram("xT", [D, Tp + 2], F32, "ExternalInput")
    mu_d = P.dram("mu", [D, 12], F32, "ExternalInput")
    wr_d = P.dram("wr", [D, NCH], F32, "ExternalInput"); wk_d = P.dram("wk", [D, NCH], F32, "ExternalInput"); wv_d = P.dram("wv", [D, NCH], F32, "ExternalInput")
    w1_d = P.dram("w1", [D, 64], F32, "ExternalInput"); w2_d = P.dram("w2", [64, NCH], F32, "ExternalInput")
    a1_d = P.dram("a1", [D, 64], F32, "ExternalInput"); a2_d = P.dram("a2", [64, NCH], F32, "ExternalInput")
    g1_d = P.dram("g1", [D, 160], F32, "ExternalInput"); g2_d = P.dram("g2", [160, NCH], F32, "ExternalInput")
    chv_d = P.dram("chv", [64, NHD, 8], F32, "ExternalInput")
    msk_d = P.dram("msk", [64, 128], F32, "ExternalInput")
    seg_d = P.dram("seg", [1, GW], F32, "ExternalInput")
    id_d = P.dram("ident", [128, 128], F32, "ExternalInput")
    y_d = P.dram("y", [Tp, NCH], F32, "ExternalOutput")
    aux_d = P.dram("aux", [4, NCH, Tp], F32, "ExternalOutput")
    By = Buf("y_d"); Baux = Buf("aux_d")
    ident = P.sb("ident_sb", [128, 128], BF16); Bid = Buf("ident"); P.dma("pool", ident[:], id_d[:, :], None, writes=[Bid])
    identf = P.sb("identf", [128, 128], F32); Bidf = Buf("identf"); P.dma("sp", identf[:], id_d[:, :], None, writes=[Bidf])
    def wload(name, src, K, N):
        kc = (K + 127) // 128
        t = P.sb(name, [128, kc, N], BF16); B = Buf(name)
        for c in range(kc):
            rows = min(128, K - c * 128)
            P.dma("pool", t[0:rows, c, :], src[c * 128:c * 128 + rows, :], None, writes=[B])
        return t, B
    wr, Bwr = wload("wr", wr_d, D, NCH); wk, Bwk = wload("wk", wk_d, D, NCH); wv, Bwv = wload("wv", wv_d, D, NCH)
    w1, Bw1 = wload("w1", w1_d, D, 64); a1, Ba1 = wload("a1", a1_d, D, 64); g1, Bg1 = wload("g1", g1_d, D, 160)
    w2, Bw2 = wload("w2", w2_d, 64, NCH); a2, Ba2 = wload("a2", a2_d, 64, NCH); g2, Bg2 = wload("g2", g2_d, 160, NCH)
    mu = P.sb("mu", [128, 8, 12], F32); Bmu = Buf("mu")
    P.dma("sp", mu[:], mu_d.rearrange("(c p) m -> p c m", p=128), None, writes=[Bmu])
    muc = P.sb("muc", [128, 8, 6], F32)
    P.op("dve", lambda e: e.tensor_tensor(muc[:, :, :], mu[:, :, 0:6], mu[:, :, 6:12], ALU.add), reads=[Bmu], writes=[Bmu])
    P.op("dve", lambda e: e.tensor_scalar(muc[:, :, :], muc[:, :, :], -1.0, 1.0, ALU.mult, ALU.add), reads=[Bmu], writes=[Bmu])
    chv = P.sb("chv", [64, NHD, 8], F32); Bchv = Buf("chv"); P.dma("sp", chv[:], chv_d[:, :, :], None, writes=[Bchv])
    P.op("dve", lambda e: e.tensor_scalar(chv[:, :, 4:5], chv[:, :, 3:4], -1.0, 1.0, ALU.mult, ALU.add), reads=[Bchv], writes=[Bchv])
    msk = P.sb("msk", [64, 128], F32); Bmsk = Buf("msk"); P.dma("sp", msk[:], msk_d[:, :], None, writes=[Bmsk])
    seg = P.sb("seg", [64, GW], F32); Bseg = Buf("seg"); P.dma("sp", seg[:], seg_d[0:1, :].to_broadcast([64, GW]), None, writes=[Bseg])
    ones64 = P.sb("ones64", [64, 64], F32); Bones = Buf("ones"); P.op("pool", lambda e: e.memset(ones64[:, :], 1.0), writes=[Bones])
    ST = P.sb("ST", [64, NHD, 64], F32); STb = P.sb("STb", [64, NHD, 64], BF16); BST = [Buf("ST%d" % h) for h in range(NHD)]
    P.op("pool", lambda e: e.memset(ST[:, :, :], 0.0), writes=BST)
    P.op("pool", lambda e: e.memset(STb[:, :, :], 0.0), writes=BST)
    x32 = P.sb("x32", [128, 8, GW + 2], F32); Bx32 = Buf("x32")
    xs = [P.sb("xs%d" % i, [128, 8, GW], BF16) for i in range(6)]; Bxs = [Buf("xs%d" % i) for i in range(6)]
    hw = P.sb("hw", [64, GW], BF16); Bhw = Buf("hw"); ha = P.sb("ha", [64, GW], BF16); Bha = Buf("ha")
    hg = P.sb("hg", [128, 2, GW], BF16); Bhg = Buf("hg")
    names = "r k v a lw kk ss t1 t2 Lc KRk".split()
    F = {n: P.sb("f_" + n, [64, GW], F32) for n in ["r", "k", "v", "a", "lw", "kk", "t1", "t2", "Lc", "kd", "g"]}
    BF = {n: Buf("f_" + n) for n in F}
    HB = 4
    NS = 4
    KRl = [P.sb("KR%d" % i, [64, GW // C, 2, C], BF16) for i in range(HB)]; BKRl = [Buf("KR") for i in range(HB)]
    KBl = [P.sb("KB%d" % i, [64, 2, GW], BF16) for i in range(HB)]; BKBl = [Buf("KB") for i in range(HB)]
    HTl = [P.sb("HT%d" % i, [64, 3, GW], BF16) for i in range(HB)]; BHTl = [Buf("HT") for i in range(HB)]
    gCl = [P.sb("gC%d" % i, [64, GW // C], F32) for i in range(HB)]; BgCl = [Buf("gC") for i in range(HB)]
    mskT = P.sb("mskT", [64, 64], F32)
    P.op("pool", lambda e: e.tensor_scalar(mskT[:, :], msk[:, 64:128], -1.0, 1.0, ALU.mult, ALU.add), reads=[Bmsk], writes=[Bmsk])
    class Slot: pass
    slots = []
    for si in range(NS):
        sl = Slot()
        sl.TM = P.sb("TM%d" % si, [64, 3, 64], BF16); sl.BTM = Buf("TM")
        sl.MA = P.sb("MA%d" % si, [64, 128], BF16); sl.BMA = Buf("MA")
        sl.MB = P.sb("MB%d" % si, [64, 128], BF16); sl.BMB = Buf("MB")
        sl.NT = P.sb("NT%d" % si, [64, 64], BF16); sl.BNT = Buf("NT")
        sl.X = [P.sb("X%d_%d" % (si, i), [64, 64], BF16) for i in range(2)]; sl.XT = [P.sb("XT%d_%d" % (si, i), [64, 64], BF16) for i in range(2)]
        sl.BX = [Buf("X") for i in range(2)]; sl.BXT = [Buf("XT") for i in range(2)]
        sl.R = P.sb("R%d" % si, [64, 64], F32); sl.Rb = P.sb("Rb%d" % si, [64, 64], BF16); sl.BR = Buf("R")
        sl.Wt = P.sb("Wt%d" % si, [64, 64], BF16); sl.BWt = Buf("Wt")
        sl.Ut = P.sb("Ut%d" % si, [64, 64], BF16); sl.BUt = Buf("Ut")
        slots.append(sl)
    ysb = P.sb("ysb", [64, GW // C, NHD, 64], F32); Bysb = Buf("ysb")
    ysem = P.newsem()
    banks = [P.ps("bank%d" % i, [128, 512], F32) for i in range(7)]; Bbanks = [Buf("bank%d" % i) for i in range(7)]
    pt = P.ps("ptr", [64, 3, 128], BF16); Bptr = Buf("ptr")
    for si in range(NS):
        slots[si].pa = banks[si]; slots[si].Bpa = Bbanks[si]
        slots[si].pw = banks[si]; slots[si].Bpw = Bbanks[si]
    pp = [banks[4], banks[5]]; Bpp = [Bbanks[4], Bbanks[5]]
    pl = banks[6]; Bpl = Bbanks[6]
    psq = banks[6]; Bpsq = Bbanks[6]
    ngr = (Tp + GW - 1) // GW
    ppi = 0
    for gi in range(ngr):
        t0 = gi * GW
        W = min(GW, Tp - t0)
        ncg = W // C
        for c in range(8):
            P.dma("sp", x32[:, c, 0:W + 2], xT_d[c * 128:(c + 1) * 128, t0:t0 + W + 2], None, writes=[Bx32])
        for i in range(6):
            for c in range(8):
                eng = "dve" if (i * 8 + c) % 2 == 0 else "pool"
                P.op("act", lambda e, i=i, c=c: e.activation(xs[i][:, c, 0:W], x32[:, c, 1:W + 1], AF.Identity, scale=muc[:, c, i:i + 1]), reads=[Bx32, Bmu], writes=[Bxs[i]])
                P.op("dve", lambda e, i=i, c=c: e.scalar_tensor_tensor(xs[i][:, c, 0:W], x32[:, c, 0:W], mu[:, c, i:i + 1], xs[i][:, c, 0:W], ALU.mult, ALU.add), reads=[Bx32, Bmu, Bxs[i]], writes=[Bxs[i]])
                P.op("dve", lambda e, i=i, c=c: e.scalar_tensor_tensor(xs[i][:, c, 0:W], x32[:, c, 2:W + 2], mu[:, c, 6 + i:7 + i], xs[i][:, c, 0:W], ALU.mult, ALU.add), reads=[Bx32, Bmu, Bxs[i]], writes=[Bxs[i]])
        for c in range(8):
            P.op("pe", lambda e, c=c: e.matmul(pl[0:64, 0:W], w1[:, c, :], xs[3][:, c, 0:W], start=(c == 0), stop=(c == 7)), reads=[Bw1, Bxs[3]], writes=[Bpl])
        P.op("act", lambda e: e.activation(hw[:, 0:W], pl[0:64, 0:W], AF.Tanh), reads=[Bpl], writes=[Bhw])
        for c in range(8):
            P.op("pe", lambda e, c=c: e.matmul(pl[0:64, 0:W], a1[:, c, :], xs[4][:, c, 0:W], start=(c == 0), stop=(c == 7)), reads=[Ba1, Bxs[4]], writes=[Bpl])
        P.op("act", lambda e: e.activation(ha[:, 0:W], pl[0:64, 0:W], AF.Identity), reads=[Bpl], writes=[Bha])
        for part, (lo, n) in enumerate(((0, 128), (128, 32))):
            for c in range(8):
                P.op("pe", lambda e, c=c, lo=lo, n=n: e.matmul(pl[0:n, 0:W], g1[:, c, lo:lo + n], xs[5][:, c, 0:W], start=(c == 0), stop=(c == 7)), reads=[Bg1, Bxs[5]], writes=[Bpl])
            P.op("act", lambda e, part=part, n=n: e.activation(hg[0:n, part, 0:W], pl[0:n, 0:W], AF.Sigmoid), reads=[Bpl], writes=[Bhg])
        for h in range(NHD):
            hb = h % HB
            KR, BKR, KB, BKB, HT, BHT, gC, BgC = KRl[hb], BKRl[hb], KBl[hb], BKBl[hb], HTl[hb], BHTl[hb], gCl[hb], BgCl[hb]
            cs = slice(h * 64, (h + 1) * 64)
            cw0, ca0, ckk, cka, c1ka = [chv[:, h, i:i + 1] for i in range(5)]
            def proj(wt, Bw, xi, dst, func=AF.Identity, **kw):
                nonlocal ppi
                p = pp[ppi % 2]; Bp = Bpp[ppi % 2]; ppi += 1
                for c in range(8):
                    P.op("pe", lambda e, c=c: e.matmul(p[0:64, 0:W], wt[:, c, cs], xs[xi][:, c, 0:W], start=(c == 0), stop=(c == 7)), reads=[Bw, Bxs[xi]], writes=[Bp])
                P.op("act", lambda e: e.activation(F[dst][:, 0:W], p[0:64, 0:W], func, **kw), reads=[Bp], writes=[BF[dst]])
            proj(wr, Bwr, 0, "r"); proj(wk, Bwk, 1, "k"); proj(wv, Bwv, 2, "v")
            def lora(w2t, Bw2_, hsrc, Bh, dst, bias):
                nonlocal ppi
                p = pp[ppi % 2]; Bp = Bpp[ppi % 2]; ppi += 1
                P.op("pe", lambda e: e.matmul(p[0:64, 0:W], w2t[0:64, 0, cs], hsrc[0:64, 0:W], start=True, stop=True), reads=[Bw2_, Bh], writes=[Bp])
                P.op("act", lambda e: e.activation(F[dst][:, 0:W], p[0:64, 0:W], AF.Sigmoid, bias=bias, scale=1.0), reads=[Bp, Bchv], writes=[BF[dst]])
            lora(w2, Bw2, hw, Bhw, "lw", cw0)
            lora(a2, Ba2, ha, Bha, "a", ca0)
            p = pp[ppi % 2]; Bp = Bpp[ppi % 2]; ppi += 1
            P.op("pe", lambda e: e.matmul(p[0:64, 0:W], g2[:, 0, cs], hg[:, 0, 0:W], start=True, stop=False), reads=[Bg2, Bhg], writes=[Bp])
            P.op("pe", lambda e: e.matmul(p[0:64, 0:W], g2[0:32, 1, cs], hg[0:32, 1, 0:W], start=False, stop=True), reads=[Bg2, Bhg], writes=[Bp])
            P.op("act", lambda e: e.activation(F["g"][:, 0:W], p[0:64, 0:W], AF.Identity), reads=[Bp], writes=[BF["g"]])
            P.op("dve", lambda e: e.tensor_scalar(F["kk"][:, 0:W], F["k"][:, 0:W], ckk, None, ALU.mult), reads=[BF["k"], Bchv], writes=[BF["kk"]])
            P.op("dve", lambda e: e.tensor_tensor(F["t1"][:, 0:W], F["kk"][:, 0:W], F["kk"][:, 0:W], ALU.mult), reads=[BF["kk"]], writes=[BF["t1"]])
            if 'a' not in SKIP:
                P.op("pe", lambda e: e.matmul(psq[0:64, 0:W], ones64[:, :], F["t1"][:, 0:W], start=True, stop=True), reads=[Bones, BF["t1"]], writes=[Bpsq])
            P.op("act", lambda e: e.activation(F["t2"][:, 0:W], psq[0:64, 0:W], AF.Ln, bias=1e-24, scale=1.0), reads=[Bpsq], writes=[BF["t2"]])
            P.op("act", lambda e: e.activation(F["t2"][:, 0:W], F["t2"][:, 0:W], AF.Exp, scale=-0.5), reads=[BF["t2"]], writes=[BF["t2"]])
            P.op("dve", lambda e: e.tensor_tensor(F["kk"][:, 0:W], F["kk"][:, 0:W], F["t2"][:, 0:W], ALU.mult), reads=[BF["kk"], BF["t2"]], writes=[BF["kk"]])
            P.op("dve", lambda e: e.tensor_scalar(F["t1"][:, 0:W], F["a"][:, 0:W], cka, c1ka, ALU.mult, ALU.add), reads=[BF["a"], Bchv], writes=[BF["t1"]])
            P.op("dve", lambda e: e.tensor_tensor(F["kd"][:, 0:W], F["k"][:, 0:W], F["t1"][:, 0:W], ALU.mult), reads=[BF["k"], BF["t1"]], writes=[BF["kd"]])
            P.op("pool", lambda e: e.tensor_tensor(F["a"][:, 0:W], F["a"][:, 0:W], F["kk"][:, 0:W], ALU.mult), reads=[BF["a"], BF["kk"]], writes=[BF["a"]])
            P.op("pool", lambda e: e.tensor_scalar(F["lw"][:, 0:W], F["lw"][:, 0:W], NEGH, None, ALU.mult), reads=[BF["lw"]], writes=[BF["lw"]])
            if 'c' not in SKIP:
              P.op("dve", lambda e: e.tensor_tensor_scan(F["Lc"][:, 0:W], seg[:, 0:W], F["lw"][:, 0:W], 0.0, ALU.mult, ALU.add), reads=[Bseg, BF["lw"]], writes=[BF["Lc"]])
            Lc3 = F["Lc"][:, 0:W].rearrange("p (n c) -> p n c", c=C)
            P.op("act", lambda e: e.activation(gC[:, 0:ncg], Lc3[:, :, C - 1], AF.Exp), reads=[BF["Lc"]], writes=[BgC])
            P.op("act", lambda e: e.activation(F["t1"][:, 0:W], F["Lc"][:, 0:W], AF.Exp), reads=[BF["Lc"]], writes=[BF["t1"]])
            P.op("dve", lambda e: e.tensor_tensor(KR[:, 0:ncg, 1, :], F["r"][:, 0:W].rearrange("p (n c) -> p n c", c=C), F["t1"][:, 0:W].rearrange("p (n c) -> p n c", c=C), ALU.mult), reads=[BF["r"], BF["t1"]], writes=[BKR])
            P.op("pool", lambda e: e.tensor_tensor(F["t2"][:, 0:W], F["Lc"][:, 0:W], F["lw"][:, 0:W], ALU.subtract), reads=[BF["Lc"], BF["lw"]], writes=[BF["t2"]])
            P.op("act", lambda e: e.activation(F["t2"][:, 0:W], F["t2"][:, 0:W], AF.Exp), reads=[BF["t2"]], writes=[BF["t2"]])
            P.op("dve", lambda e: e.tensor_tensor(KR[:, 0:ncg, 0, :], F["kk"][:, 0:W].rearrange("p (n c) -> p n c", c=C), F["t2"][:, 0:W].rearrange("p (n c) -> p n c", c=C), ALU.mult), reads=[BF["kk"], BF["t2"]], writes=[BKR])
            P.op("act", lambda e: e.activation(F["t1"][:, 0:W], F["Lc"][:, 0:W], AF.Exp, scale=-1.0), reads=[BF["Lc"]], writes=[BF["t1"]])
            P.op("dve", lambda e: e.tensor_tensor(KB[:, 0, 0:W], F["kd"][:, 0:W], F["t1"][:, 0:W], ALU.mult), reads=[BF["kd"], BF["t1"]], writes=[BKB])
            P.op("pool", lambda e: e.tensor_tensor(KB[:, 1, 0:W], F["a"][:, 0:W], F["t1"][:, 0:W], ALU.mult), reads=[BF["a"], BF["t1"]], writes=[BKB])
            if 'b' not in SKIP:
              P.op("dve", lambda e: e.tensor_tensor(F["t2"][:, 0:W].rearrange("p (n c) -> p n c", c=C), Lc3[:, :, C - 1:C].to_broadcast([64, ncg, C]), Lc3, ALU.subtract), reads=[BF["Lc"]], writes=[BF["t2"]])
            P.op("act", lambda e: e.activation(F["t2"][:, 0:W], F["t2"][:, 0:W], AF.Exp), reads=[BF["t2"]], writes=[BF["t2"]])
            P.op("dve", lambda e: e.tensor_tensor(HT[:, 0, 0:W], F["kd"][:, 0:W], F["t2"][:, 0:W], ALU.mult), reads=[BF["kd"], BF["t2"]], writes=[BHT])
            P.op("dve", lambda e: e.scalar_tensor_tensor(HT[:, 1, 0:W], F["a"][:, 0:W], -1.0, F["t2"][:, 0:W], ALU.mult, ALU.mult), reads=[BF["a"], BF["t2"]], writes=[BHT])
            P.op("pool", lambda e: e.tensor_copy(HT[:, 2, 0:W], F["v"][:, 0:W]), reads=[BF["v"]], writes=[BHT])
            for ai, nm in enumerate(("r", "kd", "v", "g")):
                P.dma("sp", aux_d[ai, h * 64:(h + 1) * 64, t0:t0 + W], F[nm][:, 0:W], None, reads=[BF[nm]], writes=[Baux])
            if hb == HB - 1 or h == NHD - 1:
                batch = list(range(h - hb, h + 1))
                def unit(hh, ci, sl):
                    hb2 = hh % HB
                    KR, BKR, KB, BKB, HT, BHT, gC, BgC = KRl[hb2], BKRl[hb2], KBl[hb2], BKBl[hb2], HTl[hb2], BHTl[hb2], gCl[hb2], BgCl[hb2]
                    cc = slice(ci * C, (ci + 1) * C)
                    pa, Bpa, pw, Bpw = sl.pa, sl.Bpa, sl.pw, sl.Bpw
                    TM, BTM, MA, BMA, MB, BMB, NT_, BNT, R, Rb, BR, Wt, BWt, Ut, BUt = sl.TM, sl.BTM, sl.MA, sl.BMA, sl.MB, sl.BMB, sl.NT, sl.BNT, sl.R, sl.Rb, sl.BR, sl.Wt, sl.BWt, sl.Ut, sl.BUt
                    for q in range(3):
                        P.op("pe", lambda e, q=q: e.transpose(pt[:, q, 0:64], HT[:, q, cc], ident[0:64, 0:64]), reads=[BHT, Bid], writes=[Bptr])
                    P.op("dve", lambda e: e.tensor_copy(TM[:, :, :], pt[:, :, 0:64]), reads=[Bptr], writes=[BTM])
                    P.op("pe", lambda e: e.matmul(pa[0:64, 0:128], KB[:, 0, cc], KR[:, ci, :, :], start=True, stop=True), reads=[BKB, BKR], writes=[Bpa])
                    P.op("pe", lambda e: e.matmul(pa[0:64, 128:256], KB[:, 1, cc], KR[:, ci, :, :], start=False, stop=True, skip_group_check=True), reads=[BKB, BKR], writes=[Bpa])
                    P.op("pe", lambda e: e.matmul(pa[0:64, 256:320], KR[:, ci, 0, :], KB[:, 1, cc], start=False, stop=True, skip_group_check=True), reads=[BKB, BKR], writes=[Bpa])
                    yield
                    P.op("dve", lambda e: e.tensor_tensor(MA[:, :], pa[0:64, 0:128], msk[:, :], ALU.mult), reads=[Bpa, Bmsk], writes=[BMA])
                    P.op("dve", lambda e: e.tensor_tensor(MB[:, 0:64], pa[0:64, 128:192], msk[:, 0:64], ALU.mult), reads=[Bpa, Bmsk], writes=[BMB])
                    P.op("dve", lambda e: e.scalar_tensor_tensor(MB[:, 64:128], pa[0:64, 192:256], -1.0, msk[:, 64:128], ALU.mult, ALU.mult), reads=[Bpa, Bmsk], writes=[BMB])
                    P.op("dve", lambda e: e.tensor_tensor(NT_[:, :], pa[0:64, 256:320], mskT[:, :], ALU.mult), reads=[Bpa, Bmsk], writes=[BNT])
                    P.op("dve", lambda e: e.tensor_tensor(R[:, :], identf[0:64, 0:64], MB[:, 0:64], ALU.subtract), reads=[Bidf, BMB], writes=[BR])
                    P.op("act", lambda e: e.activation(Rb[:, :], R[:, :], AF.Identity), reads=[BR], writes=[BR])
                    yield
                    curX, curXT, BcX, BcXT = MB[:, 0:64], NT_[:, :], BMB, BNT
                    for lvl in range(5):
                        k2 = lvl % 2
                        P.op("pe", lambda e, curX=curX, curXT=curXT: e.matmul(pw[0:64, 0:64], curXT, curX, start=True, stop=True), reads=[BcX, BcXT], writes=[Bpw])
                        P.op("pe", lambda e, curX=curX, curXT=curXT: e.matmul(pw[0:64, 64:128], curX, curXT, start=False, stop=True, skip_group_check=True), reads=[BcX, BcXT], writes=[Bpw])
                        yield
                        P.op("dve", lambda e, k2=k2: e.tensor_copy(sl.X[k2][:, :], pw[0:64, 0:64]), reads=[Bpw], writes=[sl.BX[k2]])
                        P.op("dve", lambda e, k2=k2: e.tensor_copy(sl.XT[k2][:, :], pw[0:64, 64:128]), reads=[Bpw], writes=[sl.BXT[k2]])
                        P.op("pe", lambda e, k2=k2: e.matmul(pw[0:64, 128:192], sl.XT[k2][:, :], Rb[:, :], start=False, stop=True, skip_group_check=True), reads=[sl.BXT[k2], BR], writes=[Bpw])
                        yield
                        P.op("dve", lambda e: e.tensor_tensor(R[:, :], R[:, :], pw[0:64, 128:192], ALU.add), reads=[BR, Bpw], writes=[BR])
                        P.op("act", lambda e: e.activation(Rb[:, :], R[:, :], AF.Identity), reads=[BR], writes=[BR])
                        yield
                        curX, curXT, BcX, BcXT = sl.X[k2][:, :], sl.XT[k2][:, :], sl.BX[k2], sl.BXT[k2]
                    P.op("pe", lambda e: e.matmul(pw[0:64, 192:256], KR[:, ci, 0, :], STb[:, hh, :], start=False, stop=False, skip_group_check=True), reads=[BKR, BST[hh]], writes=[Bpw])
                    P.op("pe", lambda e: e.matmul(pw[0:64, 192:256], MA[:, 0:64], TM[:, 2, :], start=False, stop=True, skip_group_check=True), reads=[BMA, BTM], writes=[Bpw])
                    yield
                    P.op("dve", lambda e: e.tensor_copy(Wt[:, :], pw[0:64, 192:256]), reads=[Bpw], writes=[BWt])
                    P.op("pe", lambda e: e.matmul(pw[0:64, 256:320], Rb[:, :], Wt[:, :], start=False, stop=True, skip_group_check=True), reads=[BR, BWt], writes=[Bpw])
                    yield
                    P.op("dve", lambda e: e.tensor_copy(Ut[:, :], pw[0:64, 256:320]), reads=[Bpw], writes=[BUt])
                    P.op("pe", lambda e: e.matmul(pw[0:64, 320:384], KR[:, ci, 1, :], STb[:, hh, :], start=False, stop=False, skip_group_check=True), reads=[BKR, BST[hh]], writes=[Bpw])
                    P.op("pe", lambda e: e.matmul(pw[0:64, 320:384], MA[:, 64:128], TM[:, 2, :], start=False, stop=False, skip_group_check=True), reads=[BMA, BTM], writes=[Bpw])
                    P.op("pe", lambda e: e.matmul(pw[0:64, 320:384], MB[:, 64:128], Ut[:, :], start=False, stop=True, skip_group_check=True), reads=[BMB, BUt], writes=[Bpw])
                    P.op("pe", lambda e: e.matmul(pw[0:64, 384:448], TM[:, 0, :], TM[:, 2, :], start=False, stop=False, skip_group_check=True), reads=[BTM], writes=[Bpw])
                    P.op("pe", lambda e: e.matmul(pw[0:64, 384:448], TM[:, 1, :], Ut[:, :], start=False, stop=True, skip_group_check=True), reads=[BTM, BUt], writes=[Bpw])
                    yield
                    P.op("dve", lambda e: e.tensor_copy(ysb[:, ci, hh, :], pw[0:64, 320:384]), reads=[Bpw], writes=[Bysb])
                    P.op("dve", lambda e: e.scalar_tensor_tensor(ST[:, hh, :], ST[:, hh, :], gC[:, ci:ci + 1], pw[0:64, 384:448], ALU.mult, ALU.add), reads=[BST[hh], BgC, Bpw], writes=[BST[hh]])
                    P.op("act", lambda e: e.activation(STb[:, hh, :], ST[:, hh, :], AF.Identity), reads=[BST[hh]], writes=[BST[hh]])
                    yield
                todo = [(hh, ci) for ci in range(ncg) for hh in batch]
                active = []
                free = list(range(NS))
                while todo or active:
                    while todo and free:
                        hh, ci = todo.pop(0); si = free.pop(0)
                        active.append((unit(hh, ci, slots[si]), si))
                    nxt_active = []
                    for gen, si in active:
                        try:
                            next(gen); nxt_active.append((gen, si))
                        except StopIteration:
                            free.append(si)
                    active = nxt_active
        for ci in range(ncg):
            P.dma("sp", y_d[t0 + ci * C:t0 + (ci + 1) * C, :], ysb[:, ci, :, :], ysem, reads=[Bysb], writes=[By])
    P.finish([By, Baux])
    print("phaseC insts", P.ninst)
    return P.close()

def ref_dir(xs6, Wd, nh):
    T = xs6.shape[1]
    r = xs6[0] @ Wd["wr"]; k = xs6[1] @ Wd["wk"]; v = xs6[2] @ Wd["wv"]
    lw = np.tanh(xs6[3] @ Wd["w1"]) @ Wd["w2"]
    z = Wd["w0"] + lw
    w_log = -np.log1p(np.exp(-z)) - 0.5
    decay = np.exp(-np.exp(w_log))
    a = 1 / (1 + np.exp(-(Wd["a0"] + (xs6[4] @ Wd["a1"]) @ Wd["a2"])))
    g = (1 / (1 + np.exp(-(xs6[5] @ Wd["g1"])))) @ Wd["g2"]
    kk = (k * Wd["k_k"]).reshape(T, nh, 64)
    kk = kk / np.maximum(np.sqrt((kk * kk).sum(-1, keepdims=True)), 1e-12)
    kd = k * (1 + (a - 1) * Wd["k_a"])
    rh = r.reshape(T, nh, 64); wh = decay.reshape(T, nh, 64); kdh = kd.reshape(T, nh, 64); vh = v.reshape(T, nh, 64); ah = a.reshape(T, nh, 64)
    S = np.zeros((nh, 64, 64)); ys = np.zeros((T, nh, 64))
    for t in range(T):
        sa = np.einsum('hvk,hk->hv', S, kk[t])
        S = S * wh[t][:, None, :] - sa[:, :, None] * (kk[t] * ah[t])[:, None, :] + vh[t][:, :, None] * kdh[t][:, None, :]
        ys[t] = np.einsum('hvk,hk->hv', S, rh[t])
    return ys.reshape(T, nh * 64), r, kd, v, g


LNX_EPS = 64e-5

def build_phaseD(ntiles, FE=3584, E=8, D=1024, FW=256, PART=16):
    P = Prog(); nc = P.nc
    T = ntiles * 128
    NHh = D // 64
    names = ["yf", "yb", "r", "kdf", "kdb", "v", "g", "h1"]
    ind = {n: P.dram(n, [T, D], F32, "ExternalInput") for n in names}
    wo_d = P.dram("wo", [D, D], F32, "ExternalInput")
    vec_d = P.dram("vecs", [7, D], F32, "ExternalInput")
    wr_d = P.dram("wrt", [E, D], F32, "ExternalInput")
    br_d = P.dram("brt", [1, E], F32, "ExternalInput")
    wg_d = P.dram("wg", [E, FE // FW, 128, 8, FW], F32, "ExternalInput")
    wu_d = P.dram("wu", [E, FE // FW, 128, 8, FW], F32, "ExternalInput")
    wd_d = P.dram("wd", [E, FE, D], F32, "ExternalInput")
    id_d = P.dram("ident", [128, 128], F32, "ExternalInput")
    out_d = P.dram("out", [T, D], F32, "ExternalOutput")
    Bout_d = Buf("out_d")
    ident = P.sb("ident_sb", [128, 128], BF16); Bid = Buf("ident"); P.dma("pool", ident[:], id_d[:, :], None, writes=[Bid])
    wo, Bwo = load_w_bf16(P, "wo_sb", wo_d, D, D, None)
    vb = []; Bvb = []
    for i in range(5):
        t, B = load_bcast(P, "vec%d" % i, vec_d[i:i + 1, :], D, None); vb.append(t); Bvb.append(B)
    brb, Bbrb = load_bcast(P, "brb", br_d[0:1, :], E, None)
    halves = [list(range(a, min(ntiles, a + PART))) for a in range(0, ntiles, PART)]
    maxh = max(len(h) for h in halves)
    hT = P.sb("hT", [128, 8, maxh * 128], BF16); BhT = Buf("hT")
    yacc = P.sb("yacc", [128, maxh, D], F32); Byacc = [Buf("yacc%d" % i) for i in range(maxh)]
    gates = P.sb("gates", [128, maxh, E], F32); Bgates = Buf("gates")
    st = P.sb("lnst", [128, 12], F32); mv = P.sb("lnmv", [128, 4], F32); Bscr = Buf("lnscr")
    pt = [P.ps("pt%d" % i, [128, 8, 128], BF16) for i in range(2)]; Bpt = [Buf("pt") for i in range(2)]
    pm = [P.ps("pm%d" % i, [128, 512], F32) for i in range(2)]; Bpm = [Buf("pm") for i in range(2)]
    pg = [P.ps("pg%d" % i, [128, 512], F32) for i in range(2)]; Bpg = [Buf("pg") for i in range(2)]
    pu = [P.ps("pu%d" % i, [128, 512], F32) for i in range(2)]; Bpu = [Buf("pu") for i in range(2)]
    NFC = FW // 128
    outsem = P.newsem()
    ptc = 0; wcc = 0; hc = 0
    for half in halves:
        if not half: continue
        with P.scope():
            tin = {n: P.sb("in_" + n, [128, D], F32) for n in names}; Bin = {n: Buf("in_" + n) for n in names}
            s16 = P.sb("s16", [128, 8, NHh], F32); Bs16 = Buf("s16")
            tmp = tin["yb"]; Btmp = Bin["yb"]
            obf = P.sb("obf", [128, D], BF16); Bobf = Buf("obf")
            oT = P.sb("oT", [128, 8, 128], BF16); BoT = Buf("oT")
            hbf = P.sb("hbf", [128, D], BF16); Bhbf = Buf("hbf")
            lg = P.sb("lg", [128, 4, E], F32); Blg = Buf("lg")
            wrb = P.sb("wrb", [128, E, D], F32); Bwrb = Buf("wrb")
            for e_ in range(E):
                P.dma("sp", wrb[:, e_, :], wr_d[e_:e_ + 1, :].to_broadcast([128, D]), None, writes=[Bwrb])
            for j, t in enumerate(half):
                for n in names:
                    P.dma("sp", tin[n][:], ind[n][t * 128:(t + 1) * 128, :], None, writes=[Bin[n]])
                y = tin["yf"]; By_ = Bin["yf"]
                v3 = lambda a: a[:, :].rearrange("p (h c) -> p h c", c=64)
                P.op("dve", lambda e: e.tensor_tensor(y[:, :], y[:, :], tin["yb"][:, :], ALU.add), reads=[By_, Bin["yb"]], writes=[By_])
                P.op("dve", lambda e: e.tensor_reduce(s16[:, 0, :], v3(y), AX.X, ALU.add), reads=[By_], writes=[Bs16])
                P.op("pool", lambda e: e.tensor_tensor(tmp[:, :], y[:, :], y[:, :], ALU.mult), reads=[By_], writes=[Btmp])
                P.op("dve", lambda e: e.tensor_reduce(s16[:, 1, :], v3(tmp), AX.X, ALU.add), reads=[Btmp], writes=[Bs16])
                P.op("dve", lambda e: e.tensor_scalar(s16[:, 0, :], s16[:, 0, :], 1.0 / 64, None, ALU.mult), reads=[Bs16], writes=[Bs16])
                P.op("dve", lambda e: e.tensor_tensor(s16[:, 2, :], s16[:, 0, :], s16[:, 0, :], ALU.mult), reads=[Bs16], writes=[Bs16])
                P.op("dve", lambda e: e.scalar_tensor_tensor(s16[:, 3, :], s16[:, 1, :], 1.0 / 64, s16[:, 2, :], ALU.mult, ALU.subtract), reads=[Bs16], writes=[Bs16])
                P.op("act", lambda e: e.activation(s16[:, 4, :], s16[:, 3, :], AF.Ln, bias=LNX_EPS, scale=1.0), reads=[Bs16], writes=[Bs16])
                P.op("act", lambda e: e.activation(s16[:, 5, :], s16[:, 4, :], AF.Exp, scale=-0.5), reads=[Bs16], writes=[Bs16])
                P.op("dve", lambda e: e.tensor_tensor(v3(y), v3(y), s16[:, 0, :].to_broadcast([128, NHh, 64]) if False else s16[:, 0:1, :].rearrange("p o h -> p h o").to_broadcast([128, NHh, 64]), ALU.subtract), reads=[By_, Bs16], writes=[By_])
                P.op("dve", lambda e: e.tensor_tensor(v3(y), v3(y), s16[:, 5:6, :].rearrange("p o h -> p h o").to_broadcast([128, NHh, 64]), ALU.mult), reads=[By_, Bs16], writes=[By_])
                P.op("pool", lambda e: e.tensor_tensor(y[:, :], y[:, :], vb[0][:, :], ALU.mult), reads=[By_, Bvb[0]], writes=[By_])
                P.op("pool", lambda e: e.tensor_tensor(y[:, :], y[:, :], vb[1][:, :], ALU.add), reads=[By_, Bvb[1]], writes=[By_])
                kd = tin["kdf"]
                P.op("dve", lambda e: e.tensor_tensor(kd[:, :], kd[:, :], tin["kdb"][:, :], ALU.add), reads=[Bin["kdf"], Bin["kdb"]], writes=[Bin["kdf"]])
                P.op("pool", lambda e: e.tensor_tensor(kd[:, :], kd[:, :], vb[2][:, :], ALU.mult), reads=[Bin["kdf"], Bvb[2]], writes=[Bin["kdf"]])
                P.op("dve", lambda e: e.tensor_tensor(kd[:, :], kd[:, :], tin["r"][:, :], ALU.mult), reads=[Bin["kdf"], Bin["r"]], writes=[Bin["kdf"]])
                P.op("dve", lambda e: e.tensor_reduce(s16[:, 6, :], v3(kd), AX.X, ALU.add), reads=[Bin["kdf"]], writes=[Bs16])
                P.op("dve", lambda e: e.tensor_tensor(v3(tmp), v3(tin["v"]), s16[:, 6:7, :].rearrange("p o h -> p h o").to_broadcast([128, NHh, 64]), ALU.mult), reads=[Bin["v"], Bs16], writes=[Btmp])
                P.op("dve", lambda e: e.tensor_tensor(y[:, :], y[:, :], tmp[:, :], ALU.add), reads=[By_, Btmp], writes=[By_])
                P.op("dve", lambda e: e.tensor_tensor(obf[:, :], y[:, :], tin["g"][:, :], ALU.mult), reads=[By_, Bin["g"]], writes=[Bobf])
                transpose_tile(P, obf, Bobf, ident, Bid, pt[ptc % 2], Bpt[ptc % 2], oT, BoT, 0); ptc += 1
                res = tin["h1"]; Bres = Bin["h1"]
                for nh in range(2):
                    for c in range(8):
                        P.op("pe", lambda e, c=c, nh=nh: e.matmul(pm[nh][:, :], oT[:, c, :], wo[:, c, nh * 512:(nh + 1) * 512], start=(c == 0), stop=(c == 7)), reads=[BoT, Bwo], writes=[Bpm[nh]])
                    P.op("dve", lambda e, nh=nh: e.scalar_tensor_tensor(res[:, nh * 512:(nh + 1) * 512], res[:, nh * 512:(nh + 1) * 512], ALPHA, pm[nh][:, :], ALU.mult, ALU.add), reads=[Bres, Bpm[nh]], writes=[Bres])
                layer_norm(P, res, Bres, vb[3], Bvb[3], vb[4], Bvb[4], (st, mv), Bscr, res, Bres, hbf, Bhbf)
                transpose_tile(P, hbf, Bhbf, ident, Bid, pt[ptc % 2], Bpt[ptc % 2], hT, BhT, j); ptc += 1
                P.op("act", lambda e, j=j: e.activation(yacc[:, j, :], res[:, :], AF.Identity, scale=ALPHA), reads=[Bres], writes=[Byacc[j]])
                for e_ in range(E):
                    P.op("pool" if e_ % 2 else "dve", lambda e, e_=e_: e.tensor_tensor(tmp[:, :], res[:, :], wrb[:, e_, :], ALU.mult), reads=[Bres, Bwrb], writes=[Btmp])
                    P.op("dve", lambda e, e_=e_: e.reduce_sum(lg[:, 0, e_:e_ + 1], tmp[:, :], AX.X), reads=[Btmp], writes=[Blg])
                P.op("dve", lambda e: e.tensor_tensor(lg[:, 0, :], lg[:, 0, :], brb[:, :], ALU.add), reads=[Blg, Bbrb], writes=[Blg])
                P.op("dve", lambda e: e.reduce_max(lg[:, 1, 0:1], lg[:, 0, :], AX.X), reads=[Blg], writes=[Blg])
                P.op("dve", lambda e: e.tensor_scalar(lg[:, 2, :], lg[:, 0, :], lg[:, 1, 0:1], -1e30, ALU.is_equal, ALU.mult), reads=[Blg], writes=[Blg])
                P.op("dve", lambda e: e.tensor_tensor(lg[:, 2, :], lg[:, 2, :], lg[:, 0, :], ALU.add), reads=[Blg], writes=[Blg])
                P.op("dve", lambda e: e.reduce_max(lg[:, 1, 1:2], lg[:, 2, :], AX.X), reads=[Blg], writes=[Blg])
                P.op("dve", lambda e: e.tensor_scalar(lg[:, 2, :], lg[:, 0, :], lg[:, 1, 1:2], None, ALU.is_ge), reads=[Blg], writes=[Blg])
                P.op("dve", lambda e: e.tensor_scalar(lg[:, 3, :], lg[:, 0, :], lg[:, 1, 0:1], None, ALU.subtract), reads=[Blg], writes=[Blg])
                P.op("act", lambda e: e.activation(lg[:, 3, :], lg[:, 3, :], AF.Exp), reads=[Blg], writes=[Blg])
                P.op("dve", lambda e: e.tensor_tensor(lg[:, 3, :], lg[:, 3, :], lg[:, 2, :], ALU.mult), reads=[Blg], writes=[Blg])
                P.op("dve", lambda e: e.reduce_sum(lg[:, 1, 2:3], lg[:, 3, :], AX.X), reads=[Blg], writes=[Blg])
                P.op("dve", lambda e: e.reciprocal(lg[:, 1, 3:4], lg[:, 1, 2:3]), reads=[Blg], writes=[Blg])
                P.op("dve", lambda e, j=j: e.tensor_scalar(gates[:, j, :], lg[:, 3, :], lg[:, 1, 3:4], None, ALU.mult), reads=[Blg], writes=[Bgates])
        P.barrier()
        nT = len(half) * 128
        _sc = P.scope(); _sc.__enter__()
        wgc = [P.sb("wgc%d" % i, [128, 8, FW], BF16) for i in range(2)]; wuc = [P.sb("wuc%d" % i, [128, 8, FW], BF16) for i in range(2)]
        wdc = [P.sb("wdc%d" % i, [128, NFC, D], BF16) for i in range(2)]
        Bwc = [Buf("wc%d" % i) for i in range(2)]
        swg = [P.sb("swg%d" % i, [128, 8, FW], F32) for i in range(2)]; swu = [P.sb("swu%d" % i, [128, 8, FW], F32) for i in range(2)]
        _swd = P.sb("swd", [128, NFC, D], F32); swd = [_swd, _swd]
        Bsw = [[Buf("sw") for j in range(3)] for i in range(2)]
        Bsw[1][2] = Bsw[0][2]
        hid = [P.sb("hid%d" % i, [128, NFC, 512], BF16) for i in range(2)]; Bhid = [Buf("hid") for i in range(2)]
        sg = [P.sb("sg%d" % i, [128, 512], F32) for i in range(2)]; Bsg = [Buf("sg") for i in range(2)]
        tgroups = [(a, min(512, nT - a)) for a in range(0, nT, 512)]
        for e_ in range(E):
            for fg in range(FE // FW):
                k = wcc % 2; wcc += 1
                f0 = fg * FW
                P.dma("sp", swg[k][:, :, :], wg_d[e_, fg, :, :, :], None, writes=[Bsw[k][0]])
                P.dma("sp", swu[k][:, :, :], wu_d[e_, fg, :, :, :], None, writes=[Bsw[k][1]])
                P.dma("sp", swd[k][:, :, :], wd_d[e_, f0:f0 + FW, :].rearrange("(c p) d -> p c d", p=128), None, writes=[Bsw[k][2]])
                P.op("pool", lambda e: e.tensor_copy(wgc[k][:, :, :], swg[k][:, :, :]), reads=[Bsw[k][0]], writes=[Bwc[k]])
                P.op("act", lambda e: e.activation(wuc[k][:, :, :], swu[k][:, :, :], AF.Identity), reads=[Bsw[k][1]], writes=[Bwc[k]])
                P.op("pool", lambda e: e.tensor_copy(wdc[k][:, :, :], swd[k][:, :, :]), reads=[Bsw[k][2]], writes=[Bwc[k]])
                for (a, w) in tgroups:
                    hk = hc % 2; hc += 1
                    for fc in range(NFC):
                        for c in range(8):
                            P.op("pe", lambda e, c=c, fc=fc: e.matmul(pg[hk][:, 0:w], wgc[k][:, c, fc * 128:(fc + 1) * 128], hT[:, c, a:a + w], start=(c == 0), stop=(c == 7)), reads=[Bwc[k], BhT], writes=[Bpg[hk]])
                        for c in range(8):
                            P.op("pe", lambda e, c=c, fc=fc: e.matmul(pu[hk][:, 0:w], wuc[k][:, c, fc * 128:(fc + 1) * 128], hT[:, c, a:a + w], start=(c == 0), stop=(c == 7)), reads=[Bwc[k], BhT], writes=[Bpu[hk]])
                        P.op("act", lambda e: e.activation(sg[hk][:, 0:w], pg[hk][:, 0:w], AF.Silu), reads=[Bpg[hk]], writes=[Bsg[hk]])
                        P.op("dve", lambda e, fc=fc: e.tensor_tensor(hid[hk][:, fc, 0:w], sg[hk][:, 0:w], pu[hk][:, 0:w], ALU.mult), reads=[Bsg[hk], Bpu[hk]], writes=[Bhid[hk]])
                    for jj in range(w // 128):
                        j = a // 128 + jj
                        for nh in range(2):
                            for fc in range(NFC):
                                P.op("pe", lambda e, fc=fc, nh=nh, jj=jj: e.matmul(pm[nh][:, :], hid[hk][:, fc, jj * 128:(jj + 1) * 128], wdc[k][:, fc, nh * 512:(nh + 1) * 512], start=(fc == 0), stop=(fc == NFC - 1)), reads=[Bhid[hk], Bwc[k]], writes=[Bpm[nh]])
                            P.op("dve" if nh else "pool", lambda e, nh=nh, j=j: e.scalar_tensor_tensor(yacc[:, j, nh * 512:(nh + 1) * 512], pm[nh][:, :], gates[:, j, e_:e_ + 1], yacc[:, j, nh * 512:(nh + 1) * 512], ALU.mult, ALU.add) if nh else e.tensor_copy(sg[hk][:, :], sg[hk][:, :]), reads=[Bpm[nh], Bgates, Byacc[j]], writes=[Byacc[j]]) if False else \
                            P.op("dve", lambda e, nh=nh, j=j: e.scalar_tensor_tensor(yacc[:, j, nh * 512:(nh + 1) * 512], pm[nh][:, :], gates[:, j, e_:e_ + 1], yacc[:, j, nh * 512:(nh + 1) * 512], ALU.mult, ALU.add), reads=[Bpm[nh], Bgates, Byacc[j]], writes=[Byacc[j]])
        P.barrier()
        _sc.__exit__(None, None, None)
        with P.scope():
            g1t, Bg1t = load_bcast(P, "vec5", vec_d[5:6, :], D, None)
            b1t, Bb1t = load_bcast(P, "vec6", vec_d[6:7, :], D, None)
            outt = P.sb("outt", [128, D], F32); Boutt = Buf("outt")
            for j, t in enumerate(half):
                layer_norm(P, yacc[:, j, :], Byacc[j], g1t, Bg1t, b1t, Bb1t, (st, mv), Bscr, outt, Boutt)
                P.dma("sp", out_d[t * 128:(t + 1) * 128, :], outt[:], outsem, reads=[Boutt], writes=[Bout_d])
            P.barrier()
    P.finish([Bout_d])
    print("phaseD insts", P.ninst)
    return P.close()


_CACHE = {}

def _run(nc, maps):
    res = run_bass_kernel_spmd(nc, maps, core_ids=list(range(8)))
    return res.results

def kernel(**inp):
    f32 = np.float32
    g = lambda k: np.asarray(inp[k], dtype=f32)
    x = g("x"); meta = g("meta")
    Bn, S, D = x.shape
    NM = meta.shape[0]; L = S + NM
    ident = np.eye(128, dtype=f32)
    nxt = S // 128
    w_in = g("attn_w_in")[0]
    lamv = np.stack([g("attn_lam_q1")[0], g("attn_lam_k1")[0], g("attn_lam_q2")[0], g("attn_lam_k2")[0]])
    subg = g("attn_subln_g")[0][None, :]
    ncA, mults = build_phaseA(2, nxt)
    maps = []
    for c in range(8):
        b = c // 4; heads = [2 * (c % 4), 2 * (c % 4) + 1]
        xp = np.concatenate([x[b], meta, np.zeros((128 - NM, D), f32)], 0)
        qaug, kaug = attn_consts(heads, nxt)
        tab = np.zeros((2, 1, 1024), f32)
        for hi, h in enumerate(heads):
            sl = 2.0 ** (-(h + 1))
            for m, k in mults[hi].items():
                tab[hi, 0, k] = sl * m
        hw = lambda off: np.ascontiguousarray(np.stack([w_in[:, off + h * 128: off + (h + 1) * 128] for h in heads]))
        maps.append(dict(xp=xp, wq=hw(0), wk=hw(D), wv=hw(2 * D), lamv=lamv, subg=subg, qaug=qaug, kaug=kaug, ctab=tab, ident=ident))
    resA = _run(ncA, maps)
    o_full = np.zeros((Bn, (nxt + 1) * 128, D), f32)
    for c in range(8):
        b = c // 4; h0 = 2 * (c % 4)
        o_full[b][:, h0 * 128:(h0 + 2) * 128] = resA[c]["o"]
    del resA, maps
    TQ = S // 4; MQ = NM // 4
    ntB = TQ // 128 + 1
    ncB = build_phaseB(ntB)
    maps = []
    for c in range(8):
        b = c // 4; q = c % 4
        o_rows = np.zeros((ntB * 128, D), f32); h_rows = np.zeros((ntB * 128, D), f32)
        o_rows[:TQ] = o_full[b][q * TQ:(q + 1) * TQ]; o_rows[TQ:TQ + MQ] = o_full[b][S + q * MQ:S + (q + 1) * MQ]
        h_rows[:TQ] = x[b][q * TQ:(q + 1) * TQ]; h_rows[TQ:TQ + MQ] = meta[q * MQ:(q + 1) * MQ]
        maps.append(dict(o=o_rows, h0=h_rows, wo=g("attn_w_o")[0], wg=g("ffn_w_gate")[0], wu=g("ffn_w_up")[0], wd=g("ffn_w_down")[0],
                         lng=g("ln_g")[0], lnb=g("ln_b")[0], ident=ident))
    resB = _run(ncB, maps)
    seq = np.zeros((Bn, L, D), f32)
    for c in range(8):
        b = c // 4; q = c % 4
        seq[b][NM + q * TQ:NM + (q + 1) * TQ] = resB[c]["h1"][:TQ]
        seq[b][q * MQ:(q + 1) * MQ] = resB[c]["h1"][TQ:TQ + MQ]
    del resB, maps, o_full
    nch = (L + 63) // 64; Tp = nch * 64
    ncC = build_phaseC(nch, 8)
    mu = g("rw_mu")[0]
    seg = np.ones((1, 512), f32); seg[0, ::64] = 0
    maps = []
    for c in range(8):
        b = c // 4; d = (c % 4) // 2; hg = c % 2
        cs = slice(hg * 512, (hg + 1) * 512)
        sq = seq[b] if d == 0 else seq[b][::-1]
        xpad = np.zeros((Tp + 2, D), f32); xpad[1:1 + L] = sq
        mu_in = np.concatenate([mu[d], mu[1 - d]], 0)
        chv = np.zeros((64, 8, 8), f32)
        for i, v in enumerate((g("rw_w0")[0][d], g("rw_a0")[0][d], g("rw_k_k")[0], g("rw_k_a")[0])):
            chv[:, :, i] = v[cs].reshape(8, 64).T
        wrkv = g("rw_w_rkv")[0]
        maps.append(dict(xT=np.ascontiguousarray(xpad.T), mu=np.ascontiguousarray(mu_in.T),
                         wr=np.ascontiguousarray(wrkv[0][:, cs]), wk=np.ascontiguousarray(wrkv[1][:, cs]), wv=np.ascontiguousarray(wrkv[2][:, cs]),
                         w1=g("rw_w1")[0][d], w2=np.ascontiguousarray(g("rw_w2")[0][d][:, cs]),
                         a1=g("rw_a1")[0][d], a2=np.ascontiguousarray(g("rw_a2")[0][d][:, cs]),
                         g1=g("rw_g1")[0], g2=np.ascontiguousarray(g("rw_g2")[0][:, cs]),
                         chv=chv, msk=rw_consts(), seg=seg, ident=ident))
    resC = _run(ncC, maps)
    Y = np.zeros((Bn, 2, L, D), f32); AUX = np.zeros((Bn, 2, 4, L, D), f32)
    for c in range(8):
        b = c // 4; d = (c % 4) // 2; hg = c % 2
        cs = slice(hg * 512, (hg + 1) * 512)
        yy = resC[c]["y"][:L]; ax = resC[c]["aux"][:, :, :L]
        if d == 1:
            yy = yy[::-1]; ax = ax[:, :, ::-1]
        Y[b, d][:, cs] = yy
        for i in range(4):
            AUX[b, d, i][:, cs] = ax[i].T
    del resC, maps
    ncD = build_phaseD(TQ // 128)
    vecs = np.stack([g("rw_lnx_g")[0], g("rw_lnx_b")[0], g("rw_r_k")[0].reshape(D), g("ln_g")[1, 0], g("ln_b")[1, 0], g("ln_g")[1, 1], g("ln_b")[1, 1]])
    wrt = np.ascontiguousarray(g("moe_w_router")[0].T); brt = g("moe_b_router")[0][None, :]
    FWD = 256
    def _wl(w):
        E_, D_, FE_ = w.shape
        return np.ascontiguousarray(w.reshape(E_, D_ // 128, 128, FE_ // FWD, FWD).transpose(0, 3, 2, 1, 4))
    wgE = _wl(g("moe_w_gate")[0]); wuE = _wl(g("moe_w_up")[0]); wdE = g("moe_w_down")[0]; woR = g("rw_w_o")[0]
    maps = []
    for c in range(8):
        b = c // 4; q = c % 4
        rows = slice(NM + q * TQ, NM + (q + 1) * TQ)
        cp = lambda a: np.ascontiguousarray(a[rows])
        maps.append(dict(yf=cp(Y[b, 0]), yb=cp(Y[b, 1]), r=cp(AUX[b, 0, 0]), kdf=cp(AUX[b, 0, 1]), kdb=cp(AUX[b, 1, 1]), v=cp(AUX[b, 0, 2]), g=cp(AUX[b, 0, 3]),
                         h1=cp(seq[b]), wo=woR, vecs=vecs, wrt=wrt, brt=brt, wg=wgE, wu=wuE, wd=wdE, ident=ident))
    resD = _run(ncD, maps)
    out = np.zeros((Bn, S, D), f32)
    for c in range(8):
        b = c // 4; q = c % 4
        out[b, q * TQ:(q + 1) * TQ] = resD[c]["out"]
    return out
```
